# Optimizing a Trainium2 kernel written in Bass

```python
import math
import jax
import jax.numpy as jnp
from jax import lax
import numpy as np

D_MODEL = 1024
BATCH = 1
SEQ = 16384
DEPTH = 2

RW_HEADS = 8
RW_HEAD_DIM = 64
RW_WIDTH = RW_HEADS * RW_HEAD_DIM
RW_DECAY_LORA = 64
RW_AAA_LORA = 64
RW_VRES_LORA = 32
RW_GATE_LORA = 160
RW_SPLITS = (RW_WIDTH, RW_WIDTH, RW_WIDTH, RW_DECAY_LORA, RW_AAA_LORA, RW_GATE_LORA)
RW_COLS = 3 * RW_WIDTH + RW_DECAY_LORA + RW_AAA_LORA + RW_GATE_LORA
GN_EPS = 64e-5

SSD_HEADS = 16
SSD_HEAD_DIM = 64
SSD_WIDTH = SSD_HEADS * SSD_HEAD_DIM
SSD_GROUPS = 4
SSD_STATE = 128
SSD_CONV = 4
SSD_CHUNK = 128
SSD_XBC = SSD_WIDTH + 2 * SSD_GROUPS * SSD_STATE
SSD_SPLITS = (SSD_WIDTH, SSD_XBC, SSD_HEADS)
SSD_COLS = SSD_WIDTH + SSD_XBC + SSD_HEADS

FOX_HEADS = 8
FOX_HEAD_DIM = 64
FOX_WIDTH = FOX_HEADS * FOX_HEAD_DIM
FOX_BLOCK = 128
FOX_SPLITS = (FOX_WIDTH, FOX_WIDTH, FOX_WIDTH, FOX_HEADS)
FOX_COLS = 3 * FOX_WIDTH + FOX_HEADS

N_BRANCH = 3
GATE_COLS = N_BRANCH * D_MODEL
IN_SPLITS = (RW_COLS, SSD_COLS, FOX_COLS, GATE_COLS)
IN_COLS = RW_COLS + SSD_COLS + FOX_COLS + GATE_COLS

FFN_DENSE = 2816
N_EXPERTS = 8
TOP_K = 2
FFN_EXPERT = 3584
EPS = 1e-6

kernel_name = 'hybrid_rwkv7_ssd_fox_moe_adaln'


def _split(t, sizes):
    idx, acc = [], 0
    for s in sizes[:-1]:
        acc += s
        idx.append(acc)
    return jnp.split(t, idx, axis=-1)


def rms_norm(x, gain, eps=EPS):
    xf = x.astype(jnp.float32)
    y = xf * lax.rsqrt(jnp.mean(xf * xf, axis=-1, keepdims=True) + eps)
    return (y * gain.astype(jnp.float32)).astype(x.dtype)


def token_shift(p, mu):
    prev = jnp.pad(p, ((0, 0), (1, 0), (0, 0)))[:, :-1]
    return p + (prev - p) * mu


def wkv7_scan(r, decay, k, v, a_vec, b_vec):
    Bsz, L, H, N = r.shape

    def step(S, inp):
        r_t, w_t, k_t, v_t, a_t, b_t = inp
        sa = jnp.einsum('bhvk,bhk->bhv', S, a_t)
        S = S * w_t[:, :, None, :] + sa[..., None] * b_t[:, :, None, :] + v_t[..., None] * k_t[:, :, None, :]
        return S, jnp.einsum('bhvk,bhk->bhv', S, r_t)

    seq = tuple(jnp.moveaxis(t, 1, 0) for t in (r, decay, k, v, a_vec, b_vec))
    _, y = lax.scan(step, jnp.zeros((Bsz, H, N, N), jnp.float32), seq)
    return jnp.moveaxis(y, 0, 1)


def rwkv7_mixer(h, cols, mu, w0, w_up, a0, a_up, g_up, k_k, k_a, r_k, lnx_w, lnx_b, v_first, v_res):
    Bsz, L, _ = h.shape
    f32 = jnp.float32
    cols = token_shift(cols, mu)
    r, k, v, w_lo, a_lo, g_lo = _split(cols, RW_SPLITS)
    w = -jax.nn.softplus(-(w0 + jnp.tanh(w_lo) @ w_up)) - 0.5
    a = jax.nn.sigmoid(a0 + a_lo @ a_up)
    g = jax.nn.sigmoid(g_lo) @ g_up
    if v_res is None:
        v_first = v
    else:
        v0, v_down, v_up = v_res
        v = v + (v_first - v) * jax.nn.sigmoid(v0 + (h @ v_down) @ v_up)
    heads = lambda t: t.reshape(Bsz, L, RW_HEADS, RW_HEAD_DIM).astype(f32)
    kk = heads(k * k_k)
    kk = kk / jnp.maximum(jnp.sqrt(jnp.sum(kk * kk, axis=-1, keepdims=True)), 1e-12)
    k = k * (1 + (a - 1) * k_a)
    rh, kh, vh, ah = heads(r), heads(k), heads(v), heads(a)
    decay = jnp.exp(-jnp.exp(heads(w)))
    y = wkv7_scan(rh, decay, kh, vh, -kk, kk * ah)
    mean = jnp.mean(y, axis=-1, keepdims=True)
    var = jnp.mean(jnp.square(y - mean), axis=-1, keepdims=True)
    y = ((y - mean) * lax.rsqrt(var + GN_EPS)).reshape(Bsz, L, RW_WIDTH) * lnx_w + lnx_b
    bonus = jnp.sum(rh * kh * r_k, axis=-1, keepdims=True) * vh
    y = (y + bonus.reshape(Bsz, L, RW_WIDTH)).astype(h.dtype) * g
    return y, v_first


def causal_depthwise_conv(x, w, b):
    C = x.shape[-1]
    out = lax.conv_general_dilated(x, w[:, None, :].astype(x.dtype), window_strides=(1,),
                                   padding=((SSD_CONV - 1, 0),), dimension_numbers=('NWC', 'WIO', 'NWC'),
                                   feature_group_count=C)
    return out + b


def ssd_chunked(x, dt, A, Bm, Cm):
    f32 = jnp.float32
    Bsz, L = x.shape[:2]
    nc = L // SSD_CHUNK
    E = SSD_HEADS // SSD_GROUPS
    Q = SSD_CHUNK
    x = x.astype(f32).reshape(Bsz, nc, Q, SSD_GROUPS, E, SSD_HEAD_DIM)
    dt = dt.reshape(Bsz, nc, Q, SSD_GROUPS, E)
    Bm = Bm.astype(f32).reshape(Bsz, nc, Q, SSD_GROUPS, SSD_STATE)
    Cm = Cm.astype(f32).reshape(Bsz, nc, Q, SSD_GROUPS, SSD_STATE)
    a_cum = jnp.cumsum(dt * A.reshape(SSD_GROUPS, E), axis=2)
    xdt = x * dt[..., None]
    seg = a_cum[:, :, :, None] - a_cum[:, :, None, :]
    causal = jnp.tril(jnp.ones((Q, Q), dtype=bool))[None, None, :, :, None, None]
    decay_ij = jnp.exp(jnp.where(causal, seg, -jnp.inf))
    cb = jnp.einsum('bcign,bcjgn->bcijg', Cm, Bm)
    y_diag = jnp.einsum('bcijg,bcijge,bcjgep->bcigep', cb, decay_ij, xdt)
    decay_to_end = jnp.exp(a_cum[:, :, -1:] - a_cum)
    states = jnp.einsum('bcjgn,bcjge,bcjgep->bcgepn', Bm, decay_to_end, xdt)
    chunk_decay = jnp.exp(a_cum[:, :, -1])

    def step(S, inp):
        st, dec = inp
        return S * dec[..., None, None] + st, S

    S0 = jnp.zeros((Bsz, SSD_GROUPS, E, SSD_HEAD_DIM, SSD_STATE), f32)
    _, prev = lax.scan(step, S0, (jnp.moveaxis(states, 1, 0), jnp.moveaxis(chunk_decay, 1, 0)))
    prev = jnp.moveaxis(prev, 0, 1)
    y_off = jnp.einsum('bcign,bcgepn,bcige->bcigep', Cm, prev, jnp.exp(a_cum))
    return (y_diag + y_off).reshape(Bsz, L, SSD_HEADS, SSD_HEAD_DIM)


def ssd_mixer(cols, conv_w, conv_b, dt_bias, a_log, d_skip, norm_w):
    f32 = jnp.float32
    Bsz, L, _ = cols.shape
    z, xbc, dt_raw = _split(cols, SSD_SPLITS)
    xbc = jax.nn.silu(causal_depthwise_conv(xbc, conv_w, conv_b))
    xs, Bm, Cm = _split(xbc, (SSD_WIDTH, SSD_GROUPS * SSD_STATE, SSD_GROUPS * SSD_STATE))
    dt = jax.nn.softplus((dt_raw + dt_bias).astype(f32))
    A = -jnp.exp(a_log.astype(f32))
    xh = xs.reshape(Bsz, L, SSD_HEADS, SSD_HEAD_DIM)
    y = ssd_chunked(xh, dt, A,
                    Bm.reshape(Bsz, L, SSD_GROUPS, SSD_STATE), Cm.reshape(Bsz, L, SSD_GROUPS, SSD_STATE))
    y = y + d_skip.astype(f32)[:, None] * xh.astype(f32)
    y = y.reshape(Bsz, L, SSD_WIDTH) * jax.nn.silu(z.astype(f32))
    yg = y.reshape(Bsz, L, SSD_GROUPS, SSD_WIDTH // SSD_GROUPS)
    yg = yg * lax.rsqrt(jnp.mean(yg * yg, axis=-1, keepdims=True) + EPS)
    return (yg.reshape(Bsz, L, SSD_WIDTH) * norm_w.astype(f32)).astype(cols.dtype)


def fox_mixer(cols, f_bias, q_gain, k_gain):
    f32 = jnp.float32
    Bsz, L, _ = cols.shape
    q, k, v, f_logit = _split(cols, FOX_SPLITS)
    shp = (Bsz, L, FOX_HEADS, FOX_HEAD_DIM)
    q = rms_norm(q.reshape(shp), q_gain).astype(f32)
    k = rms_norm(k.reshape(shp), k_gain).astype(f32)
    v = v.reshape(shp).astype(f32)
    F = jnp.cumsum(jax.nn.log_sigmoid((f_logit + f_bias).astype(f32)), axis=1)
    Fk = jnp.transpose(F, (0, 2, 1))
    kpos = jnp.arange(L)
    scale = FOX_HEAD_DIM ** -0.5

    def block(i):
        start = i * FOX_BLOCK
        qb = lax.dynamic_slice_in_dim(q, start, FOX_BLOCK, axis=1)
        Fq = jnp.transpose(lax.dynamic_slice_in_dim(F, start, FOX_BLOCK, axis=1), (0, 2, 1))
        s = jnp.einsum('bqhd,bkhd->bhqk', qb, k) * scale + Fq[..., None] - Fk[:, :, None, :]
        qpos = start + jnp.arange(FOX_BLOCK)
        s = jnp.where(kpos[None, :] <= qpos[:, None], s, -jnp.inf)
        p = jax.nn.softmax(s, axis=-1)
        return jnp.einsum('bhqk,bkhd->bqhd', p, v)

    out = lax.map(block, jnp.arange(L // FOX_BLOCK))
    return jnp.moveaxis(out, 0, 1).reshape(Bsz, L, FOX_WIDTH).astype(cols.dtype)


def swiglu(h, w_gu, w_down):
    g, u = jnp.split(h @ w_gu, 2, axis=-1)
    return (jax.nn.silu(g) * u) @ w_down


def moe_swiglu(h, router, w_gu, w_down):
    logits = (h @ router).astype(jnp.float32)
    top_v, top_i = lax.top_k(logits, TOP_K)
    top_w = jax.nn.softmax(top_v, axis=-1)
    combine = jnp.sum(jax.nn.one_hot(top_i, N_EXPERTS, dtype=jnp.float32) * top_w[..., None], axis=-2)
    out = jnp.zeros_like(h)
    for e in range(N_EXPERTS):
        out = out + combine[..., e:e + 1].astype(h.dtype) * swiglu(h, w_gu[e], w_down[e])
    return out


def setup_inputs(seed: int = 0) -> dict:
    key = jax.random.key(seed)
    ks = iter(jax.random.split(key, 64))
    f32 = jnp.float32
    D = D_MODEL
    n_dense = (DEPTH + 1) // 2
    n_moe = DEPTH // 2
    n_vres = DEPTH - 1

    def nrm(shape, scale):
        return jax.random.normal(next(ks), shape, f32) * scale

    def uni(shape, lo, hi):
        return jax.random.uniform(next(ks), shape, f32, lo, hi)

    dt0 = jnp.exp(uni((DEPTH, SSD_HEADS), math.log(1e-3), math.log(1e-1)))
    return {
        'x': nrm((BATCH, SEQ, D), 1.0),
        'c': nrm((BATCH, D), 1.0),
        'ada_w': nrm((DEPTH, D, 6 * D), 0.5 * D ** -0.5),
        'ada_b': nrm((DEPTH, 6 * D), 0.02),
        'norm_mix': 1.0 + nrm((DEPTH, D), 0.05),
        'norm_ffn': 1.0 + nrm((DEPTH, D), 0.05),
        'w_in': nrm((DEPTH, D, IN_COLS), D ** -0.5),
        'rw_mu': uni((DEPTH, RW_COLS), 0.0, 1.0),
        'rw_w0': uni((DEPTH, RW_WIDTH), -6.0, -1.0),
        'rw_w_up': nrm((DEPTH, RW_DECAY_LORA, RW_WIDTH), 0.5 * RW_DECAY_LORA ** -0.5),
        'rw_a0': nrm((DEPTH, RW_WIDTH), 0.5),
        'rw_a_up': nrm((DEPTH, RW_AAA_LORA, RW_WIDTH), 0.5 * RW_AAA_LORA ** -0.5),
        'rw_g_up': nrm((DEPTH, RW_GATE_LORA, RW_WIDTH), RW_GATE_LORA ** -0.5),
        'rw_k_k': 0.85 + nrm((DEPTH, RW_WIDTH), 0.1),
        'rw_k_a': 1.0 + nrm((DEPTH, RW_WIDTH), 0.1),
        'rw_r_k': nrm((DEPTH, RW_HEADS, RW_HEAD_DIM), 0.1),
        'rw_lnx_w': 1.0 + nrm((DEPTH, RW_WIDTH), 0.05),
        'rw_lnx_b': nrm((DEPTH, RW_WIDTH), 0.02),
        'rw_v0': nrm((n_vres, RW_WIDTH), 0.5),
        'rw_v_down': nrm((n_vres, D, RW_VRES_LORA), D ** -0.5),
        'rw_v_up': nrm((n_vres, RW_VRES_LORA, RW_WIDTH), 0.5 * RW_VRES_LORA ** -0.5),
        'ssd_conv_w': nrm((DEPTH, SSD_CONV, SSD_XBC), SSD_CONV ** -0.5),
        'ssd_conv_b': nrm((DEPTH, SSD_XBC), 0.02),
        'ssd_dt_bias': dt0 + jnp.log(-jnp.expm1(-dt0)),
        'ssd_a_log': jnp.log(uni((DEPTH, SSD_HEADS), 1.0, 16.0)),
        'ssd_d': 1.0 + nrm((DEPTH, SSD_HEADS), 0.1),
        'ssd_norm': 1.0 + nrm((DEPTH, SSD_WIDTH), 0.05),
        'fox_f_bias': 2.0 + nrm((DEPTH, FOX_HEADS), 0.5),
        'fox_q_gain': 1.0 + nrm((DEPTH, FOX_HEAD_DIM), 0.05),
        'fox_k_gain': 1.0 + nrm((DEPTH, FOX_HEAD_DIM), 0.05),
        'gate_b': nrm((DEPTH, GATE_COLS), 0.02),
        'proj_rw': nrm((DEPTH, RW_WIDTH, D), RW_WIDTH ** -0.5),
        'proj_ssd': nrm((DEPTH, SSD_WIDTH, D), SSD_WIDTH ** -0.5),
        'proj_fox': nrm((DEPTH, FOX_WIDTH, D), FOX_WIDTH ** -0.5),
        'w_out': nrm((DEPTH, D, D), D ** -0.5),
        'ffn_w_gu': nrm((n_dense, D, 2 * FFN_DENSE), D ** -0.5),
        'ffn_w_down': nrm((n_dense, FFN_DENSE, D), FFN_DENSE ** -0.5),
        'moe_router': nrm((n_moe, D, N_EXPERTS), D ** -0.5),
        'moe_w_gu': nrm((n_moe, N_EXPERTS, D, 2 * FFN_EXPERT), D ** -0.5),
        'moe_w_down': nrm((n_moe, N_EXPERTS, FFN_EXPERT, D), FFN_EXPERT ** -0.5),
    }


def reference(x, c, ada_w, ada_b, norm_mix, norm_ffn, w_in, rw_mu, rw_w0, rw_w_up, rw_a0, rw_a_up,
              rw_g_up, rw_k_k, rw_k_a, rw_r_k, rw_lnx_w, rw_lnx_b, rw_v0, rw_v_down, rw_v_up,
              ssd_conv_w, ssd_conv_b, ssd_dt_bias, ssd_a_log, ssd_d, ssd_norm,
              fox_f_bias, fox_q_gain, fox_k_gain, gate_b, proj_rw, proj_ssd, proj_fox, w_out,
              ffn_w_gu, ffn_w_down, moe_router, moe_w_gu, moe_w_down):
    v_first = None
    for l in range(DEPTH):
        mod = jax.nn.silu(c) @ ada_w[l] + ada_b[l]
        sh_m, sc_m, g_m, sh_f, sc_f, g_f = jnp.split(mod[:, None, :], 6, axis=-1)
        h = rms_norm(x, norm_mix[l]) * (1 + sc_m) + sh_m
        proj = h @ w_in[l]
        rw_cols, ssd_cols, fox_cols, gate_logits = _split(proj, IN_SPLITS)
        v_res = None if l == 0 else (rw_v0[l - 1], rw_v_down[l - 1], rw_v_up[l - 1])
        y_rw, v_first = rwkv7_mixer(h, rw_cols, rw_mu[l], rw_w0[l], rw_w_up[l], rw_a0[l], rw_a_up[l],
                                    rw_g_up[l], rw_k_k[l], rw_k_a[l], rw_r_k[l], rw_lnx_w[l], rw_lnx_b[l],
                                    v_first, v_res)
        y_ssd = ssd_mixer(ssd_cols, ssd_conv_w[l], ssd_conv_b[l], ssd_dt_bias[l], ssd_a_log[l], ssd_d[l],
                          ssd_norm[l])
        y_fox = fox_mixer(fox_cols, fox_f_bias[l], fox_q_gain[l], fox_k_gain[l])
        g_rw, g_ssd, g_fox = jnp.split(jax.nn.sigmoid(gate_logits + gate_b[l]), N_BRANCH, axis=-1)
        merged = g_rw * (y_rw @ proj_rw[l]) + g_ssd * (y_ssd @ proj_ssd[l]) + g_fox * (y_fox @ proj_fox[l])
        x = x + g_m * (merged @ w_out[l])
        h = rms_norm(x, norm_ffn[l]) * (1 + sc_f) + sh_f
        if l % 2 == 0:
            f = swiglu(h, ffn_w_gu[l // 2], ffn_w_down[l // 2])
        else:
            f = moe_swiglu(h, moe_router[l // 2], moe_w_gu[l // 2], moe_w_down[l // 2])
        x = x + g_f * f
    return x
```

```python
from contextlib import ExitStack
import numpy as np
import concourse.bass as bass
import concourse.mybir as mybir
from concourse.bass_utils import run_bass_kernel_spmd

F32 = mybir.dt.float32
BF16 = mybir.dt.bfloat16
ALU = mybir.AluOpType
AF = mybir.ActivationFunctionType
AX = mybir.AxisListType


class Tl:
    def __init__(self, t, name):
        self.t = t
        self.name = name
        self.last_w = None
        self.readers = []
        self.alias = None

    def __getitem__(self, key):
        return V(self, self.t[key])


class V:
    def __init__(self, tl, ap):
        self.tl = tl
        self.ap = ap

    def __getitem__(self, key):
        return V(self.tl, self.ap[key])

    def bitcast(self, dt):
        return V(self.tl, self.ap.bitcast(dt))

    def rearrange(self, s, **kw):
        return V(self.tl, self.ap.rearrange(s, **kw))

    def bcast(self, shape):
        return V(self.tl, self.ap.to_broadcast(shape))

    def bmid(self, n):
        p, m = self.ap.shape
        return V(self.tl, self.ap.unsqueeze(1).to_broadcast([p, n, m]))

    def binner(self, m):
        p, n = self.ap.shape
        return V(self.tl, self.ap.unsqueeze(2).to_broadcast([p, n, m]))

    def c3(self, t=64):
        return V(self.tl, self.ap.rearrange("p (c t) -> p c t", t=t))


class Op:
    __slots__ = ("eng", "fn", "reads", "writes", "dma", "deps", "need_inc", "cnt", "slot", "slot_prev")

    def __init__(self, eng, fn, reads, writes, dma):
        self.eng = eng
        self.fn = fn
        self.reads = reads
        self.writes = writes
        self.dma = dma
        self.deps = []
        self.need_inc = False
        self.cnt = 0
        self.slot = None
        self.slot_prev = 0


NDMA_SLOTS = 8
ARENA_BYTES = 212736
ENGS = ["pe", "act", "dve", "pool", "sp"]


class Prog:
    def __init__(self, nc):
        self.nc = nc
        self.ops = []
        self.stack = ExitStack()
        self.n_t = 0
        self.arena_t = self.stack.enter_context(nc.sbuf_tensor("arena", [128, ARENA_BYTES // 2], BF16))
        self.bump = 0
        self.live = []
        self.freed = []
        self.peak = 0

    def sb(self, shape, dt, name=None):
        self.n_t += 1
        name = name or f"t{self.n_t}"
        esz = 4 if dt == F32 else 2
        n = int(np.prod(shape[1:]))
        nbytes = n * esz
        nal = (nbytes + 63) // 64 * 64
        off = self.bump
        assert off + nal <= ARENA_BYTES, f"SBUF arena overflow allocating {name}: {off}+{nal}"
        self.bump += nal
        self.peak = max(self.peak, self.bump)
        ap = self.arena_t[0:shape[0], off // 2:(off + nbytes) // 2]
        if dt == F32:
            ap = ap.bitcast(F32)
        if len(shape) == 3:
            ap = ap.rearrange("p (a b) -> p a b", b=shape[2])
        tl = Tl(ap, name)
        al = [f for (o, e, f) in self.freed if o < off + nal and e > off]
        tl.alias = al if al else None
        self.live.append((off, off + nal, tl))
        return tl

    def mark(self):
        return (self.bump, len(self.live))

    def release(self, m):
        b, n = m
        self.freed.extend(self.live[n:])
        del self.live[n:]
        self.bump = b

    def ps(self, shape, dt, name=None):
        self.n_t += 1
        name = name or f"p{self.n_t}"
        t = self.stack.enter_context(self.nc.psum_tensor(name, list(shape), dt))
        return Tl(t, name)

    def dram(self, name, shape, dt, kind):
        t = self.nc.dram_tensor(name, list(shape), dt, kind=kind).ap()
        return Tl(t, name)

    def _add(self, eng, fn, reads, writes, dma=False):
        r = []
        for v in reads:
            if isinstance(v, V) and v.tl not in r:
                r.append(v.tl)
        w = []
        for v in writes:
            if isinstance(v, V) and v.tl not in w:
                w.append(v.tl)
        self.ops.append(Op(eng, fn, r, w, dma))

    @staticmethod
    def _a(v):
        return v.ap if isinstance(v, V) else v

    def dma(self, out, in_, q="sp", **kw):
        a = self._a
        self._add(q, lambda e: e.dma_start(out=a(out), in_=a(in_), **kw), [in_], [out], dma=True)

    def matmul(self, out, lhsT, rhs, start=True, stop=True, **kw):
        a = self._a
        self._add("pe", lambda e: e.matmul(a(out), a(lhsT), a(rhs), start=start, stop=stop, **kw), [lhsT, rhs], [out])

    def transpose(self, out, in_, ident):
        a = self._a
        self._add("pe", lambda e: e.transpose(a(out), a(in_), a(ident)), [in_, ident], [out])

    def act(self, out, in_, func, bias=None, scale=None, accum_out=None, eng="act"):
        a = self._a
        kw = {}
        if bias is not None:
            kw["bias"] = a(bias)
        if scale is not None:
            kw["scale"] = a(scale)
        if accum_out is not None:
            kw["accum_out"] = a(accum_out)
        self._add(eng, lambda e: e.activation(a(out), a(in_), func, **kw), [in_, bias, scale], [out, accum_out])

    def ts(self, out, in0, s1, s2, op0, op1=None, accum_out=None, eng="dve"):
        a = self._a
        kw = {}
        if accum_out is not None:
            kw["accum_out"] = a(accum_out)
        if op1 is None:
            self._add(eng, lambda e: e.tensor_single_scalar(a(out), a(in0), a(s1), op0), [in0, s1], [out])
        else:
            self._add(eng, lambda e: e.tensor_scalar(a(out), a(in0), a(s1), a(s2), op0, op1, **kw), [in0, s1, s2], [out, accum_out])

    def tt(self, out, in0, in1, op, eng="dve"):
        a = self._a
        self._add(eng, lambda e: e.tensor_tensor(a(out), a(in0), a(in1), op), [in0, in1], [out])

    def stt(self, out, in0, scalar, in1, op0, op1, accum_out=None, eng="dve"):
        a = self._a
        kw = {}
        if accum_out is not None:
            kw["accum_out"] = a(accum_out)
        self._add(eng, lambda e: e.scalar_tensor_tensor(a(out), a(in0), a(scalar), a(in1), op0, op1, **kw), [in0, scalar, in1], [out, accum_out])

    def copy(self, out, in_, eng="dve"):
        a = self._a
        if eng == "act":
            self._add(eng, lambda e: e.activation(a(out), a(in_), AF.Copy), [in_], [out])
        else:
            self._add(eng, lambda e: e.tensor_copy(a(out), a(in_)), [in_], [out])

    def memset(self, out, val, eng="pool"):
        a = self._a
        self._add(eng, lambda e: e.memset(a(out), val), [], [out])

    def recip(self, out, in_):
        a = self._a
        self._add("dve", lambda e: e.reciprocal(a(out), a(in_)), [in_], [out])

    def reduce(self, out, in_, op, axis=AX.X, eng="dve"):
        a = self._a
        self._add(eng, lambda e: e.tensor_reduce(a(out), a(in_), axis, op), [in_], [out])

    def affine_select(self, out, in_, pattern, compare_op, fill, base, channel_multiplier):
        a = self._a
        self._add("pool", lambda e: e.affine_select(out=a(out), in_=a(in_), pattern=pattern, compare_op=compare_op, fill=fill, base=base, channel_multiplier=channel_multiplier), [in_], [out])

    def generic(self, eng, fn, reads, writes, dma=False):
        self._add(eng, fn, reads, writes, dma)

    def emit(self):
        nc = self.nc
        ops = self.ops
        for i, op in enumerate(ops):
            deps = {}
            for tl in op.reads + op.writes:
                if tl.alias:
                    for par in tl.alias:
                        tl.readers.extend(par.readers)
                        if par.last_w is not None:
                            tl.readers.append(par.last_w)
                    tl.alias = None
            for tl in op.reads:
                if tl.last_w is not None:
                    deps[tl.last_w] = "raw"
            for tl in op.writes:
                if tl.last_w is not None and tl.last_w not in deps:
                    deps[tl.last_w] = "waw"
                for r in tl.readers:
                    if r not in deps:
                        deps[r] = "war"
            for p, kind in deps.items():
                if p == i:
                    continue
                po = ops[p]
                if (not po.dma) and (not op.dma) and po.eng == op.eng and op.eng == "pe":
                    continue
                op.deps.append(p)
                po.need_inc = True
            for tl in op.reads:
                tl.readers.append(i)
            for tl in op.writes:
                tl.last_w = i
                tl.readers = []
        cnt = {e: 0 for e in ENGS}
        slot_n = {e: 0 for e in ENGS}
        slot_cnt = {}
        for op in ops:
            if op.dma:
                k = slot_n[op.eng] % NDMA_SLOTS
                slot_n[op.eng] += 1
                key = (op.eng, k)
                op.slot = key
                op.slot_prev = slot_cnt.get(key, 0)
                slot_cnt[key] = op.slot_prev + 1
                op.cnt = op.slot_prev + 1
            elif op.need_inc:
                cnt[op.eng] += 1
                op.cnt = cnt[op.eng]
        st = self.stack
        sem_e = {e: st.enter_context(nc.semaphore(f"s_{e}")) for e in ENGS}
        sem_d = {}
        for e in ENGS:
            if slot_n[e] > 0:
                for k in range(min(NDMA_SLOTS, slot_n[e])):
                    sem_d[(e, k)] = st.enter_context(nc.semaphore(f"d_{e}{k}"))
        block = st.enter_context(nc.Block())

        def run_engine(ename):
            def body(eng):
                waited = {}

                def wait(sem, key, val):
                    if waited.get(key, 0) >= val:
                        return
                    waited[key] = val
                    eng.wait_ge(sem, val)

                for op in ops:
                    if op.eng != ename:
                        continue
                    need = {}
                    for p in op.deps:
                        po = ops[p]
                        if po.dma:
                            key = ("d",) + po.slot
                            val = 16 * po.cnt
                            sem = sem_d[po.slot]
                        else:
                            key = ("e", po.eng)
                            val = po.cnt
                            sem = sem_e[po.eng]
                        if need.get(key, (None, 0))[1] < val:
                            need[key] = (sem, val)
                    if op.dma and op.slot_prev > 0:
                        key = ("d",) + op.slot
                        val = 16 * op.slot_prev
                        if need.get(key, (None, 0))[1] < val:
                            need[key] = (sem_d[op.slot], val)
                    for key, (sem, val) in need.items():
                        wait(sem, key, val)
                    ins = op.fn(eng)
                    if op.dma:
                        ins.then_inc(sem_d[op.slot], 16)
                    elif op.need_inc:
                        ins.then_inc(sem_e[ename], 1)
                for (e, k), sem in sem_d.items():
                    if e == ename:
                        wait(sem, ("d", e, k), 16 * slot_cnt[(e, k)])
            return body

        used = set(op.eng for op in ops)
        if "pe" in used:
            block.tensor(run_engine("pe"))
        if "act" in used:
            block.scalar(run_engine("act"))
        if "dve" in used:
            block.vector(run_engine("dve"))
        if "pool" in used:
            block.gpsimd(run_engine("pool"))
        if "sp" in used:
            block.sync(run_engine("sp"))
        st.close()
        return nc


D = 1024
EPS = 1e-6


class Ctx:
    pass


def mk_consts(P):
    c = Ctx()
    c.ones_bf = P.sb([128, 128], BF16, "ones_bf")
    c.ones_f = P.sb([128, 128], F32, "ones_f")
    c.ident_bf = P.sb([128, 128], BF16, "ident_bf")
    c.ident_f = P.sb([128, 128], F32, "ident_f")
    P.memset(c.ones_bf[:], 1.0)
    P.memset(c.ones_f[:], 1.0)
    P.affine_select(c.ident_bf[:], c.ones_bf[:], [[-1, 128]], ALU.is_equal, 0.0, 0, 1)
    P.affine_select(c.ident_f[:], c.ones_f[:], [[-1, 128]], ALU.is_equal, 0.0, 0, 1)
    return c


def rsqrt_inplace(P, v, eps=None):
    if eps is not None:
        P.ts(v, v, eps, None, ALU.add)
    P.act(v, v, AF.Ln)
    P.act(v, v, AF.Exp, scale=-0.5)


def emit_mod(P, cn, c_d, ada_w_d, ada_b_d, col_blocks, row_blocks, ps_col, ps_row):
    siluc = P.sb([128, 8], F32, "siluc")
    P.dma(siluc[:], c_d[0, :].rearrange("(k p) -> p k", p=128), allow_slow_non_contiguous=True)
    P.act(siluc[:], siluc[:], AF.Silu)
    aw = P.sb([128, 8, 1024], F32, "aw_stage")
    cols, rows = {}, {}
    sbc = None
    if row_blocks:
        sbc = P.sb([128, 8, 128], F32, "siluc_bc")
        for kc in range(8):
            P.ts(sbc[:, kc, :], cn.ones_f[:], siluc[:, kc:kc + 1], None, ALU.mult)
    for j in sorted(set(col_blocks) | set(row_blocks)):
        P.dma(aw[:], ada_w_d[:, j * 1024:(j + 1) * 1024].rearrange("(kc p) n -> p kc n", p=128))
        if j in col_blocks:
            ab = P.sb([128, 8], F32, f"adab_c{j}")
            P.dma(ab[:], ada_b_d[0, j * 1024:(j + 1) * 1024].rearrange("(k p) -> p k", p=128), allow_slow_non_contiguous=True)
            for k in range(8):
                for kc in range(8):
                    P.matmul(ps_col[:, k:k + 1], aw[:, kc, k * 128:(k + 1) * 128], siluc[:, kc:kc + 1], start=(kc == 0), stop=(kc == 7))
            t = P.sb([128, 8], F32, f"modc{j}")
            P.tt(t[:], ps_col[:, 0:8], ab[:], ALU.add)
            cols[j] = t
        if j in row_blocks:
            t = P.sb([128, 1024], F32, f"modr{j}")
            P.dma(t[:], ada_b_d[0:1, j * 1024:(j + 1) * 1024].bcast([128, 1024]))
            for half in range(2):
                for kc in range(8):
                    P.matmul(ps_row[:], sbc[:, kc, :], aw[:, kc, half * 512:(half + 1) * 512], start=(kc == 0), stop=(kc == 7))
                P.tt(t[:, half * 512:(half + 1) * 512], t[:, half * 512:(half + 1) * 512], ps_row[:], ALU.add)
            rows[j] = t
    return cols, rows


def load_col(P, src_d_row, n, name):
    t = P.sb([128, n], F32, name)
    P.dma(t[:], src_d_row.rearrange("(k p) -> p k", p=128), allow_slow_non_contiguous=True)
    return t


def mk_Acol(P, normw_col, sc_col, name):
    t = P.sb([128, 8], F32, name)
    P.ts(t[:], sc_col[:], 1.0, None, ALU.add)
    P.tt(t[:], t[:], normw_col[:], ALU.mult)
    return t


class HT:
    def __init__(self, P, cn, ps_t):
        self.P, self.cn, self.ps_t = P, cn, ps_t
        self.junk = P.sb([128, 1024], F32, "ht_junk")
        self.ss = P.sb([128, 1], F32, "ht_ss")
        self.xs = P.sb([128, 1024], BF16, "ht_xs")

    def emit(self, xt, A_col, sh_col, dst):
        P = self.P
        ssv = self.ss[:]
        P.act(self.junk[:], xt, AF.Square, accum_out=ssv)
        P.ts(ssv, ssv, 1.0 / 1024, EPS, ALU.mult, ALU.add)
        rsqrt_inplace(P, ssv)
        P.act(self.xs[:], xt, AF.Copy, scale=ssv)
        for half in range(2):
            pt = self.ps_t[half]
            for j in range(4):
                k = half * 4 + j
                P.transpose(pt[:, j * 128:(j + 1) * 128], self.xs[:, k * 128:(k + 1) * 128], self.cn.ident_bf[:])
            for j in range(4):
                k = half * 4 + j
                P.ts(dst(k), pt[:, j * 128:(j + 1) * 128], A_col[:, k:k + 1], sh_col[:, k:k + 1], ALU.mult, ALU.add)


def load_convert(P, dst, src, stage, scale_bc=None, q="sp", eng="pool"):
    P.dma(stage, src, q=q)
    if scale_bc is None:
        P.copy(dst, stage, eng=eng)
    else:
        P.tt(dst, stage, scale_bc, ALU.mult, eng=eng)


class Loader:
    def __init__(self, P, nstage=3):
        self.P = P
        self.stg = [P.sb([128, 1024], F32, f"stg{i}") for i in range(nstage)]
        self.i = 0

    def load(self, dst, src, scale_bc=None, np_=128):
        n = dst.ap.shape[1]
        for c0 in range(0, n, 1024):
            c1 = min(n, c0 + 1024)
            st = self.stg[self.i % len(self.stg)]
            q = "sp" if self.i % 2 == 0 else "act"
            self.i += 1
            self.P.dma(st[0:np_, 0:c1 - c0], src[:, c0:c1], q=q)
            if scale_bc is None:
                self.P.copy(dst[:, c0:c1], st[0:np_, 0:c1 - c0], eng="pool")
            else:
                self.P.tt(dst[:, c0:c1], st[0:np_, 0:c1 - c0], scale_bc[:, c0:c1], ALU.mult, eng="pool")


def build_C(T_c, moe):
    nc = bass.Bass("TRN2", target_bir_lowering=False)
    P = Prog(nc)
    NBLK = T_c // 512
    NT = T_c // 128
    x_d = P.dram("x", [T_c, D], F32, "ExternalInput")
    yT_d = P.dram("yT", [2048, T_c], BF16, "ExternalInput")
    c_d = P.dram("c", [1, D], F32, "ExternalInput")
    ada_w_d = P.dram("ada_w", [D, 6 * D], F32, "ExternalInput")
    ada_b_d = P.dram("ada_b", [1, 6 * D], F32, "ExternalInput")
    nmix_d = P.dram("nmix", [1, D], F32, "ExternalInput")
    nffn_d = P.dram("nffn", [1, D], F32, "ExternalInput")
    wgate_d = P.dram("wgate", [D, 3 * D], F32, "ExternalInput")
    gateb_d = P.dram("gateb", [1, 3 * D], F32, "ExternalInput")
    proj_d = P.dram("proj", [2048, D], F32, "ExternalInput")
    wout_d = P.dram("wout", [D, D], F32, "ExternalInput")
    ssdn_d = P.dram("ssdn", [1, D], F32, "ExternalInput")
    if moe:
        NE, FF, FG = 8, 3584, 4
        routT_d = P.dram("routT", [8, D], F32, "ExternalInput")
    else:
        NE, FF, FG = 1, 2816, 2
    wgu_d = P.dram("wgu", [NE, D, 2 * FF], F32, "ExternalInput")
    wdn_d = P.dram("wdn", [NE, FF, D], F32, "ExternalInput")
    out_d = P.dram("out", [T_c, D], F32, "ExternalOutput")
    xmid_d = P.dram("xmid", [T_c, D], F32, "Internal")
    xmid_r = [Tl(xmid_d.t[i * 128:(i + 1) * 128, :], f"xmid{i}") for i in range(NT)]
    out_r = [Tl(out_d.t[i * 128:(i + 1) * 128, :], f"out{i}") for i in range(NT)]

    cn = mk_consts(P)
    pb = [P.ps([128, 512], F32, f"pb{i}") for i in range(6)]
    pt_bf = [P.ps([128, 1024], BF16, "ptA"), P.ps([128, 1024], BF16, "ptB")]
    ht = HT(P, cn, pt_bf)
    ld = Loader(P)

    gm_bc = P.sb([128, D], F32, "gm_bc")
    gf_bc = P.sb([128, D], F32, "gf_bc")
    if moe:
        Abc = P.sb([128, D], F32, "Abc")
        shbc = P.sb([128, D], F32, "shbc")
    A_m = P.sb([128, 8], F32, "A_m")
    A_f = P.sb([128, 8], F32, "A_f")
    sh_m = P.sb([128, 8], F32, "sh_m")
    sh_f = P.sb([128, 8], F32, "sh_f")
    gateb_c = load_col(P, gateb_d[0, :], 24, "gateb_c")
    ssdn_c = load_col(P, ssdn_d[0, :], 8, "ssdn_c")
    m0 = P.mark()
    cols, rows = emit_mod(P, cn, c_d, ada_w_d, ada_b_d, [0, 1, 3, 4], [2, 5] + ([3, 4] if moe else []), pb[0], pb[1])
    nmix_c = load_col(P, nmix_d[0, :], 8, "nmix_c")
    nffn_c = load_col(P, nffn_d[0, :], 8, "nffn_c")
    P.ts(A_m[:], cols[1][:], 1.0, None, ALU.add)
    P.tt(A_m[:], A_m[:], nmix_c[:], ALU.mult)
    P.ts(A_f[:], cols[4][:], 1.0, None, ALU.add)
    P.tt(A_f[:], A_f[:], nffn_c[:], ALU.mult)
    P.copy(sh_m[:], cols[0][:])
    P.copy(sh_f[:], cols[3][:])
    P.copy(gm_bc[:], rows[2][:])
    P.copy(gf_bc[:], rows[5][:])
    if moe:
        P.dma(Abc[:], nffn_d[0:1, :].bcast([128, D]))
        P.ts(rows[4][:], rows[4][:], 1.0, None, ALU.add)
        P.tt(Abc[:], Abc[:], rows[4][:], ALU.mult)
        P.copy(shbc[:], rows[3][:])
    P.release(m0)

    m1_ = P.mark()
    wg = P.sb([128, 8, 3072], BF16, "wg")
    wp = P.sb([128, 16, 1024], BF16, "wp")
    wo = P.sb([128, 8, 1024], BF16, "wo")
    for k in range(8):
        ld.load(wg[:, k, :], wgate_d[k * 128:(k + 1) * 128, :])
    for k in range(16):
        ld.load(wp[:, k, :], proj_d[k * 128:(k + 1) * 128, :])
    for k in range(8):
        ld.load(wo[:, k, :], wout_d[k * 128:(k + 1) * 128, :], scale_bc=gm_bc)

    xt = [P.sb([128, D], F32, f"xt{i}") for i in range(4)]
    hT = P.sb([128, 8, 512], BF16, "hT")
    ysb = P.sb([128, 16, 512], BF16, "ysb")
    ysq = P.sb([128, 2, 512], BF16, "ysq")
    rs = P.sb([128, 512], F32, "rs")
    sg = [P.sb([128, 512], BF16, f"sg{i}") for i in range(3)]
    tm = [P.sb([128, 512], F32, f"tm{i}") for i in range(2)]
    mT = P.sb([128, 8, 512], BF16, "mT")

    for b in range(NBLK):
        t0 = b * 512
        for i in range(4):
            P.dma(xt[i][:], x_d[t0 + i * 128:t0 + (i + 1) * 128, :], q="sp" if i % 2 == 0 else "act")
        P.dma(ysb[:], yT_d[:, t0:t0 + 512].rearrange("(k p) t -> p k t", p=128), q="pool")
        for i in range(4):
            ht.emit(xt[i][:], A_m, sh_m, lambda k, i=i: hT[:, k, i * 128:(i + 1) * 128])
        for g in range(4):
            for j in range(2):
                ch = 4 + 2 * g + j
                P.tt(ysq[:, j, :], ysb[:, ch, :], ysb[:, ch, :], ALU.mult)
            for j in range(2):
                P.matmul(pb[g % 2][:], cn.ones_bf[:], ysq[:, j, :], start=(j == 0), stop=(j == 1))
            P.ts(rs[:], pb[g % 2][:], 1.0 / 256, EPS, ALU.mult, ALU.add)
            rsqrt_inplace(P, rs[:])
            for j in range(2):
                ch = 4 + 2 * g + j
                P.stt(ysb[:, ch, :], ysb[:, ch, :], ssdn_c[:, 2 * g + j:2 * g + j + 1], rs[:], ALU.mult, ALU.mult)
        for dc in range(8):
            for br in range(3):
                for k in range(8):
                    P.matmul(pb[br][:], wg[:, k, br * 1024 + dc * 128: br * 1024 + (dc + 1) * 128], hT[:, k, :], start=(k == 0), stop=(k == 7))
                P.act(sg[br][:], pb[br][:], AF.Sigmoid, bias=gateb_c[:, br * 8 + dc: br * 8 + dc + 1])
            for br, (k0, k1) in enumerate([(0, 4), (4, 12), (12, 16)]):
                for k in range(k0, k1):
                    P.matmul(pb[3 + br][:], wp[:, k, dc * 128:(dc + 1) * 128], ysb[:, k, :], start=(k == k0), stop=(k == k1 - 1))
            P.tt(tm[0][:], sg[0][:], pb[3][:], ALU.mult)
            P.tt(tm[1][:], sg[1][:], pb[4][:], ALU.mult)
            P.tt(tm[0][:], tm[0][:], tm[1][:], ALU.add)
            P.tt(tm[1][:], sg[2][:], pb[5][:], ALU.mult)
            P.tt(mT[:, dc, :], tm[0][:], tm[1][:], ALU.add)
        for i in range(4):
            for h in range(2):
                pq = pb[(i * 2 + h) % 4]
                for k in range(8):
                    P.matmul(pq[:], mT[:, k, i * 128:(i + 1) * 128], wo[:, k, h * 512:(h + 1) * 512], start=(k == 0), stop=(k == 7))
                P.tt(xt[i][:, h * 512:(h + 1) * 512], xt[i][:, h * 512:(h + 1) * 512], pq[:], ALU.add)
            P.dma(xmid_r[b * 4 + i][:, :], xt[i][:], q="sp" if i % 2 == 0 else "act")
    P.release(m1_)

    h2T = P.sb([128, 8, T_c], BF16, "h2T")
    xall = [P.sb([128, D], F32, f"xall{i}") for i in range(NT)]
    if moe:
        comb = P.sb([128, NT, 8], F32, "comb")
        m2_ = P.mark()
        routbc = [P.sb([128, D], F32, f"routbc{e}") for e in range(8)]
        for e in range(8):
            P.dma(routbc[e][:], routT_d[e:e + 1, :].bcast([128, D]), q="pool")
        h2f = P.sb([128, D], F32, "h2f")
        lg = P.sb([128, 8], F32, "lg")
        l2 = P.sb([128, 8], F32, "l2")
        mx1 = P.sb([128, 1], F32, "mx1")
        mx2 = P.sb([128, 1], F32, "mx2")
        mk1 = P.sb([128, 8], F32, "mk1")
        mk2 = P.sb([128, 8], F32, "mk2")
        w1 = P.sb([128, 1], F32, "w1")
        w2 = P.sb([128, 1], F32, "w2")
    for i in range(NT):
        xv = xall[i][:]
        P.dma(xv, xmid_r[i][:, :], q="sp" if i % 2 == 0 else "act")
        ht.emit(xv, A_f, sh_f, lambda k, i=i: h2T[:, k, i * 128:(i + 1) * 128])
        if moe:
            P.stt(h2f[:], xv, ht.ss[:], Abc[:], ALU.mult, ALU.mult)
            P.tt(h2f[:], h2f[:], shbc[:], ALU.add)
            for e in range(8):
                P.tt(ht.junk[:], h2f[:], routbc[e][:], ALU.mult)
                P.reduce(lg[:, e:e + 1], ht.junk[:], ALU.add)
            P.reduce(mx1[:], lg[:], ALU.max)
            P.ts(mk1[:], lg[:], mx1[:], None, ALU.is_equal)
            P.stt(l2[:], mk1[:], -1e30, lg[:], ALU.mult, ALU.add)
            P.reduce(mx2[:], l2[:], ALU.max)
            P.ts(mk2[:], l2[:], mx2[:], None, ALU.is_equal)
            P.tt(w1[:], mx2[:], mx1[:], ALU.subtract)
            P.act(w1[:], w1[:], AF.Exp)
            P.ts(w1[:], w1[:], 1.0, None, ALU.add)
            P.recip(w1[:], w1[:])
            P.ts(w2[:], w1[:], -1.0, 1.0, ALU.mult, ALU.add)
            P.ts(mk1[:], mk1[:], w1[:], None, ALU.mult)
            P.stt(comb[:, i, :], mk2[:], w2[:], mk1[:], ALU.mult, ALU.add)
    if moe:
        P.release(m2_)
    NCH = FF // 128
    NG = NCH // FG
    wgu_a = P.sb([128, 8, 2 * FG * 128], BF16, "wgu_a")
    wdn_a = P.sb([128, FG, 1024], BF16, "wdn_a")
    actT = P.sb([128, FG, 512], BF16, "actT")
    sl = P.sb([128, 512], F32, "sl")
    for e in range(NE):
        for gi in range(NG):
            f0 = gi * FG * 128
            for k in range(8):
                ld.load(wgu_a[:, k, 0:FG * 128], wgu_d[e, k * 128:(k + 1) * 128, f0:f0 + FG * 128])
                ld.load(wgu_a[:, k, FG * 128:2 * FG * 128], wgu_d[e, k * 128:(k + 1) * 128, FF + f0:FF + f0 + FG * 128])
            for j in range(FG):
                ld.load(wdn_a[:, j, :], wdn_d[e, f0 + j * 128:f0 + (j + 1) * 128, :], scale_bc=gf_bc)
            for b in range(NBLK):
                t0 = b * 512
                for j in range(FG):
                    for k in range(8):
                        P.matmul(pb[0][:], wgu_a[:, k, j * 128:(j + 1) * 128], h2T[:, k, t0:t0 + 512], start=(k == 0), stop=(k == 7))
                    for k in range(8):
                        P.matmul(pb[1][:], wgu_a[:, k, FG * 128 + j * 128:FG * 128 + (j + 1) * 128], h2T[:, k, t0:t0 + 512], start=(k == 0), stop=(k == 7))
                    P.act(sl[:], pb[0][:], AF.Silu)
                    P.tt(actT[:, j, :], sl[:], pb[1][:], ALU.mult)
                for i in range(4):
                    ti = b * 4 + i
                    for h in range(2):
                        pp = pb[2 + (i * 2 + h) % 4]
                        for j in range(FG):
                            P.matmul(pp[:], actT[:, j, i * 128:(i + 1) * 128], wdn_a[:, j, h * 512:(h + 1) * 512], start=(j == 0), stop=(j == FG - 1))
                        xs_ = xall[ti][:, h * 512:(h + 1) * 512]
                        if moe:
                            P.stt(xs_, pp[:], comb[:, ti, e:e + 1], xs_, ALU.mult, ALU.add)
                        else:
                            P.tt(xs_, xs_, pp[:], ALU.add)
    for i in range(NT):
        P.dma(out_r[i][:, :], xall[i][:], q="sp" if i % 2 == 0 else "act")
    print("C peak sbuf", P.peak, "ops", len(P.ops))
    return P.emit()


GN_EPS = 64e-5
C_R, C_K, C_V, C_WLO, C_ALO, C_GLO = 0, 64, 128, 192, 256, 320
C_Z, C_X, C_B, C_C = 480, 608, 736, 864
C_FQ, C_FK, C_FV = 992, 1056, 1120
C_DT, C_F = 1184, 1186
C_VD = 1187
NW0, NW1 = 1187, 1219
PP_MU = 0
PP_W0, PP_A0, PP_KK, PP_KA, PP_RK, PP_V0 = 7, 8, 9, 10, 11, 12
PP_CW = 13
PP_CB = 29
PP_QG, PP_KG = 33, 34
PP_D = 35
PP_SEL = 37
NPP = 40
BC_LNW, BC_LNB, BC_DTB, BC_ALOG, BC_FB = 0, 64, 128, 130, 132
NBC = 133
MK_BIG, MK_SL64, MK_TRI, MK_SLF, MK_CHUNK, MK_NEG, MK_SEL = 0, 1, 2, 3, 4, 5, 6
NMASK = 7


def make_masks():
    p = np.arange(128)[:, None]
    j = np.arange(128)[None, :]
    m = np.zeros((128, NMASK, 128), np.float32)
    pl, jl = p % 64, j % 64
    m[:, MK_BIG, :] = np.where(j < 64, jl > pl, jl >= pl)
    m[:, MK_SL64, :] = (j < p) & (p < 64) & (j < 64)
    m[:, MK_TRI, :] = j >= p
    m[:, MK_SLF, :] = p > j
    m[:, MK_CHUNK, :] = (j >= p) & ((j // 64) == (p // 64))
    m[:, MK_NEG, :] = np.where(p > j, -30000.0, 0.0)
    m[64, MK_SEL, :] = 1.0
    return m


import os
RW_STOP = int(os.environ.get('RW_STOP', '99'))


class RWMixer:
    def __init__(self, st):
        self.st = st
        P = st.P
        self.cur = {}
        for name, rows in [("r", 64), ("k", 64), ("v", 64), ("wlo", 64), ("alo", 64), ("gloa", 128), ("glob", 32)]:
            t = P.sb([rows, 513], F32, f"rw_{name}")
            P.memset(t[:, 0:1], 0.0)
            self.cur[name] = t
        self.S = P.sb([64, 64], F32, "rw_S")
        self.Sb = P.sb([64, 64], BF16, "rw_Sb")
        P.memset(self.S[:], 0.0)
        P.memset(self.Sb[:], 0.0)
        self.negw0 = P.sb([64, 1], F32, "rw_negw0")
        self.omka = P.sb([64, 1], F32, "rw_omka")
        pp = st.pp
        P.ts(self.negw0[:], pp[0:64, PP_W0:PP_W0 + 1], -1.0, None, ALU.mult)
        P.ts(self.omka[:], pp[0:64, PP_KA:PP_KA + 1], -1.0, 1.0, ALU.mult, ALU.add)

    def alloc(self):
        st = self.st
        P = st.P
        f = lambda n, r=64, c=512, dt=F32: P.sb([r, c], dt, "rw_" + n)
        self.tmp = f("tmp")
        self.tmp128 = f("tmp128", 128)
        self.r = f("sr")
        self.k = f("sk")
        self.v = f("sv")
        self.twl = f("twl", dt=BF16)
        self.alo = f("salo", dt=BF16)
        self.sga = f("sga", 128, dt=BF16)
        self.sgb = f("sgb", 32, dt=BF16)
        self.logw = f("logw")
        self.a = f("a")
        self.kk = f("kk")
        self.sq = f("sq", dt=BF16)
        self.k2 = f("k2")
        self.bv = f("bv")
        self.rk = f("rk", dt=BF16)
        self.cl = f("cl")
        self.G = f("G")
        self.E2 = f("E2")
        self.lwT = P.sb([128, 64], F32, "rw_lwT")
        self.AR = P.sb([64, 8, 128], BF16, "rw_AR")
        self.BK = P.sb([64, 8, 128], BF16, "rw_BK")
        self.BKh = P.sb([64, 8, 128], BF16, "rw_BKh")
        self.VV = P.sb([64, 8, 64], BF16, "rw_VV")
        self.MBb = P.sb([64, 8, 64], BF16, "rw_MBb")
        self.MKb = P.sb([64, 8, 128], BF16, "rw_MKb")
        self.Pm = [P.sb([64, 8, 128], F32, f"rw_Pm{i}") for i in range(2)]
        self.TT = P.sb([64, 8, 128], F32, "rw_TT")
        self.Utok = P.sb([64, 8, 64], BF16, "rw_Utok")
        self.Vtok = P.sb([64, 8, 64], BF16, "rw_Vtok")
        self.BKhT = P.sb([64, 8, 128], BF16, "rw_BKhT")
        self.Wf = P.sb([64, 64], F32, "rw_Wf")
        self.yc = P.sb([64, 8, 64], F32, "rw_yc")
        self.yq = P.sb([64, 8, 64], F32, "rw_yq")
        self.st8 = [P.sb([64, 8], F32, f"rw_st{i}") for i in range(3)]
        self.yo = P.sb([64, 8, 64], BF16, "rw_yo")
        self.yTs = P.sb([64, 512], BF16, "rw_yTs")
        if st.layer == 1:
            self.hv = f("hv", 32, dt=BF16)
            self.vf = f("vf")

    def block(self, sb):
        st = self.st
        P, pb, pp, mk, cn = st.P, st.pb, st.pp, st.mk, st.cn
        t0 = sb * 512
        cur = self.cur
        mrk = P.mark()
        self.alloc()
        groups = [("r", C_R, 64), ("k", C_K, 64), ("v", C_V, 64), ("wlo", C_WLO, 64), ("alo", C_ALO, 64),
                  ("gloa", C_GLO, 128), ("glob", C_GLO + 128, 32)]
        for gi, (name, c0, n) in enumerate(groups):
            st.project(c0, n, cur[name][:, 1:513], pb[gi % 2], evac="act" if gi % 2 == 0 else "dve")
        if st.layer == 1:
            st.project(C_VD, 32, self.hv[:], pb[1], evac="act")
            P.dma(self.vf[:], st.vf_d[:, t0:t0 + 512], q="pool")

        def shift(name, mucol, rows, out, func=None):
            c = cur[name]
            tmpv = self.tmp128[0:rows, :]
            P.tt(tmpv, c[:, 0:512], c[:, 1:513], ALU.subtract)
            if func is None:
                P.stt(out, tmpv, pp[0:rows, mucol:mucol + 1], c[:, 1:513], ALU.mult, ALU.add)
            else:
                P.stt(tmpv, tmpv, pp[0:rows, mucol:mucol + 1], c[:, 1:513], ALU.mult, ALU.add)
                P.act(out, tmpv, func)
            P.copy(c[:, 0:1], c[:, 512:513], eng="pool")

        shift("r", PP_MU + 0, 64, self.r[:])
        shift("k", PP_MU + 1, 64, self.k[:])
        shift("v", PP_MU + 2, 64, self.v[:])
        shift("wlo", PP_MU + 3, 64, self.twl[:], AF.Tanh)
        shift("alo", PP_MU + 4, 64, self.alo[:], AF.Copy)
        shift("gloa", PP_MU + 5, 128, self.sga[:], AF.Sigmoid)
        shift("glob", PP_MU + 6, 32, self.sgb[:], AF.Sigmoid)
        if RW_STOP == 1:
            P.release(mrk)
            return
        P.matmul(pb[0][0:64, :], st.wup[:, :], self.twl[:], start=True, stop=True)
        P.act(self.logw[:], pb[0][0:64, :], AF.Exp, scale=-1.0, bias=self.negw0[:])
        P.ts(self.logw[:], self.logw[:], 1.0, None, ALU.add)
        P.recip(self.logw[:], self.logw[:])
        P.ts(self.logw[:], self.logw[:], -float(np.exp(-0.5)), None, ALU.mult)
        P.matmul(pb[1][0:64, :], st.aup[:, :], self.alo[:], start=True, stop=True)
        P.act(self.a[:], pb[1][0:64, :], AF.Sigmoid, bias=pp[0:64, PP_A0:PP_A0 + 1])
        if st.layer == 1:
            P.matmul(pb[0][0:64, :], st.vup[:, :], self.hv[:], start=True, stop=True)
            P.act(self.tmp[:], pb[0][0:64, :], AF.Sigmoid, bias=pp[0:64, PP_V0:PP_V0 + 1])
            P.tt(self.vf[:], self.vf[:], self.v[:], ALU.subtract)
            P.tt(self.vf[:], self.vf[:], self.tmp[:], ALU.mult)
            P.tt(self.v[:], self.v[:], self.vf[:], ALU.add)
        else:
            P.dma(st.vfo_d[:, t0:t0 + 512], self.v[:], q="pool")
        P.ts(self.kk[:], self.k[:], pp[0:64, PP_KK:PP_KK + 1], None, ALU.mult)
        P.tt(self.sq[:], self.kk[:], self.kk[:], ALU.mult)
        P.matmul(pb[1][0:64, :], cn.ones_bf[0:64, 0:64], self.sq[:], start=True, stop=True)
        P.ts(self.tmp[:], pb[1][0:64, :], 1e-24, None, ALU.add)
        rsqrt_inplace(P, self.tmp[:])
        P.tt(self.kk[:], self.kk[:], self.tmp[:], ALU.mult)
        P.ts(self.tmp[:], self.a[:], pp[0:64, PP_KA:PP_KA + 1], self.omka[:], ALU.mult, ALU.add)
        P.tt(self.k2[:], self.k[:], self.tmp[:], ALU.mult)
        P.tt(self.bv[:], self.kk[:], self.a[:], ALU.mult)
        P.stt(self.rk[:], self.r[:], pp[0:64, PP_RK:PP_RK + 1], self.k2[:], ALU.mult, ALU.mult)
        if RW_STOP == 2:
            P.release(mrk)
            return
        for j in range(4):
            P.transpose(pb[0][:, 0:64], self.logw[:, j * 128:(j + 1) * 128], cn.ident_f[0:64, 0:64])
            P.copy(self.lwT[:], pb[0][:, 0:64])
            P.matmul(pb[1][0:64, j * 128:(j + 1) * 128], self.lwT[:], mk[:, MK_CHUNK, :], start=True, stop=True)
        P.copy(self.cl[:], pb[1][0:64, :])
        if RW_STOP == 3:
            P.release(mrk)
            return
        P.act(self.G[:], self.cl[:], AF.Exp)
        P.tt(self.AR[:, :, 64:128], self.r[:].c3(), self.G[:].c3(), ALU.mult)
        P.tt(self.tmp[:], self.cl[:], self.logw[:], ALU.subtract)
        P.act(self.E2[:], self.tmp[:], AF.Exp)
        P.stt(self.AR[:, :, 0:64], self.kk[:].c3(), -1.0, self.E2[:].c3(), ALU.mult, ALU.mult)
        P.act(self.E2[:], self.cl[:], AF.Exp, scale=-1.0)
        P.tt(self.BK[:, :, 0:64], self.bv[:].c3(), self.E2[:].c3(), ALU.mult)
        P.tt(self.BK[:, :, 64:128], self.k2[:].c3(), self.E2[:].c3(), ALU.mult)
        for c in range(8):
            P.act(self.E2[:, c * 64:(c + 1) * 64], self.cl[:, c * 64:(c + 1) * 64], AF.Exp, scale=-1.0, bias=self.cl[:, c * 64 + 63:c * 64 + 64])
        P.tt(self.BKh[:, :, 0:64], self.bv[:].c3(), self.E2[:].c3(), ALU.mult)
        P.tt(self.BKh[:, :, 64:128], self.k2[:].c3(), self.E2[:].c3(), ALU.mult)
        P.copy(self.VV[:, :, 0:64], self.v[:].c3(), eng="pool")
        if RW_STOP == 4:
            P.release(mrk)
            return
        for c in range(8):
            o = (c % 4) * 128
            P.matmul(pb[2 + c // 4][0:64, o:o + 128], self.BK[:, c, 0:64], self.AR[:, c, :], start=True, stop=True)
        for c in range(8):
            o = (c % 4) * 128
            P.matmul(pb[4 + c // 4][0:64, o:o + 128], self.BK[:, c, 64:128], self.AR[:, c, :], start=True, stop=True)
        Pm, TT = self.Pm, self.TT
        mbig = mk[0:64, MK_BIG, :]
        for hb in range(2):
            src = pb[2 + hb][0:64, :].c3(128)
            P.tt(Pm[0][:, hb * 4:(hb + 1) * 4, 0:64], src[:, :, 0:64], mbig[:, 0:64].bmid(4), ALU.mult)
            P.tt(self.MBb[:, hb * 4:(hb + 1) * 4, :], src[:, :, 64:128], mbig[:, 64:128].bmid(4), ALU.mult)
            P.tt(self.MKb[:, hb * 4:(hb + 1) * 4, :], pb[4 + hb][0:64, :].c3(128), mbig.bmid(4), ALU.mult)
        for c in range(8):
            P.matmul(pb[2][0:64, c * 64:(c + 1) * 64], self.AR[:, c, 0:64], self.BK[:, c, 0:64], start=True, stop=True)
        P.tt(Pm[0][:, :, 64:128], pb[2][0:64, :].c3(), mk[0:64, MK_SL64, 0:64].bmid(8), ALU.mult)
        for hf in range(2):
            P.tt(TT[:, :, hf * 64:(hf + 1) * 64], Pm[0][:, :, hf * 64:(hf + 1) * 64], cn.ident_f[0:64, 0:64].bmid(8), ALU.add)
        if RW_STOP == 5:
            P.release(mrk)
            return
        for c in range(8):
            P.transpose(st.pt_bf[0][0:64, c * 64:(c + 1) * 64], self.VV[:, c, 0:64], cn.ident_bf[0:64, 0:64])
        P.copy(self.Vtok[:], st.pt_bf[0][0:64, 0:512].c3())
        for c in range(8):
            for hf in range(2):
                P.transpose(st.pt_bf[1][0:64, c * 128 + hf * 64:c * 128 + (hf + 1) * 64], self.BKh[:, c, hf * 64:(hf + 1) * 64], cn.ident_bf[0:64, 0:64])
        P.copy(self.BKhT[:], st.pt_bf[1][0:64, :].c3(128))
        if RW_STOP == 6:
            P.release(mrk)
            return
        cu = 0
        for rnd in range(5):
            last = rnd == 4
            Pc, Pn = Pm[cu], Pm[1 - cu]
            for c in range(8):
                pbk = pb[2 + c // 4]
                o = (c % 4) * 128
                P.matmul(pbk[0:64, o:o + 64], Pc[:, c, 64:128], Pc[:, c, 0:64], start=True, stop=True)
                if not last:
                    P.matmul(pbk[0:64, o + 64:o + 128], Pc[:, c, 0:64], Pc[:, c, 64:128], start=True, stop=True)
            for hb in range(2):
                if last:
                    P.copy(Pn[:, hb * 4:(hb + 1) * 4, 0:64], pb[2 + hb][0:64, :].c3(128)[:, :, 0:64])
                else:
                    P.copy(Pn[:, hb * 4:(hb + 1) * 4, :], pb[2 + hb][0:64, :].c3(128))
            for c in range(8):
                pbk = pb[4 + c // 4]
                o = (c % 4) * 128
                P.matmul(pbk[0:64, o:o + 64], TT[:, c, 64:128], Pn[:, c, 0:64], start=True, stop=True)
                if not last:
                    P.matmul(pbk[0:64, o + 64:o + 128], TT[:, c, 0:64], Pn[:, c, 64:128], start=True, stop=True)
            for hb in range(2):
                if last:
                    P.tt(TT[:, hb * 4:(hb + 1) * 4, 0:64], TT[:, hb * 4:(hb + 1) * 4, 0:64], pb[4 + hb][0:64, :].c3(128)[:, :, 0:64], ALU.add)
                else:
                    P.tt(TT[:, hb * 4:(hb + 1) * 4, :], TT[:, hb * 4:(hb + 1) * 4, :], pb[4 + hb][0:64, :].c3(128), ALU.add)
            cu = 1 - cu
        if RW_STOP == 7:
            P.release(mrk)
            return
        pw = pb[1]
        for c in range(8):
            P.matmul(pw[0:64, 0:64], self.AR[:, c, 0:64], self.Sb[:, :], start=True, stop=False)
            P.matmul(pw[0:64, 0:64], self.MKb[:, c, 0:64], self.Vtok[:, c, :], start=False, stop=True)
            P.copy(self.Wf[:], pw[0:64, 0:64])
            P.matmul(pw[0:64, 64:128], TT[:, c, 0:64], self.Wf[:], start=True, stop=True)
            P.copy(self.Utok[:, c, :], pw[0:64, 64:128], eng="act")
            yreg = pb[0][0:64, c * 64:(c + 1) * 64]
            P.matmul(yreg, self.AR[:, c, 64:128], self.Sb[:, :], start=True, stop=False)
            P.matmul(yreg, self.MBb[:, c, :], self.Utok[:, c, :], start=False, stop=False)
            P.matmul(yreg, self.MKb[:, c, 64:128], self.Vtok[:, c, :], start=False, stop=True)
            P.matmul(pw[0:64, 128:192], self.BKhT[:, c, 0:64], self.Utok[:, c, :], start=True, stop=False)
            P.matmul(pw[0:64, 128:192], self.BKhT[:, c, 64:128], self.Vtok[:, c, :], start=False, stop=True)
            P.stt(self.S[:], self.S[:], self.G[:, c * 64 + 63:c * 64 + 64], pw[0:64, 128:192], ALU.mult, ALU.add)
            P.copy(self.Sb[:], self.S[:], eng="act")
        if RW_STOP == 8:
            P.release(mrk)
            return
        yall = pb[0][0:64, :].c3()
        s1, s2, bon = self.st8
        P.reduce(s1[:], yall, ALU.add)
        P.ts(s1[:], s1[:], -1.0 / 64, None, ALU.mult)
        P.tt(self.yc[:], yall, s1[:].binner(64), ALU.add)
        P.tt(self.yq[:], self.yc[:], self.yc[:], ALU.mult)
        P.reduce(s2[:], self.yq[:], ALU.add)
        P.ts(s2[:], s2[:], 1.0 / 64, GN_EPS, ALU.mult, ALU.add)
        rsqrt_inplace(P, s2[:])
        P.tt(self.yc[:], self.yc[:], s2[:].binner(64), ALU.mult)
        P.tt(self.yc[:], self.yc[:], st.pbc[0:64, BC_LNW:BC_LNW + 64].bmid(8), ALU.mult)
        P.tt(self.yc[:], self.yc[:], st.pbc[0:64, BC_LNB:BC_LNB + 64].bmid(8), ALU.add)
        for c in range(8):
            P.matmul(pb[1][0:64, 256 + c:257 + c], self.rk[:, c * 64:(c + 1) * 64], cn.ones_bf[0:64, 0:1], start=True, stop=True)
        P.copy(bon[:], pb[1][0:64, 256:264])
        P.tt(self.yq[:], self.Vtok[:], bon[:].binner(64), ALU.mult)
        P.tt(self.yc[:], self.yc[:], self.yq[:], ALU.add)
        for c in range(8):
            P.matmul(pb[4][0:64, c * 64:(c + 1) * 64], self.sga[:, c * 64:(c + 1) * 64], st.gupa[:, :], start=True, stop=False)
            P.matmul(pb[4][0:64, c * 64:(c + 1) * 64], self.sgb[:, c * 64:(c + 1) * 64], st.gupb[:, :], start=False, stop=True)
        P.tt(self.yo[:], self.yc[:], pb[4][0:64, :].c3(), ALU.mult)
        for c in range(8):
            P.transpose(st.pt_bf[0][0:64, c * 64:(c + 1) * 64], self.yo[:, c, :], cn.ident_bf[0:64, 0:64])
        P.copy(self.yTs[:], st.pt_bf[0][0:64, 0:512])
        P.dma(st.yT_d[0:64, t0:t0 + 512], self.yTs[:], q="pool")
        P.release(mrk)


class SSDMixer:
    def __init__(self, st):
        self.st = st
        P = st.P
        self.xh = [P.sb([64, 515], F32, f"ssd_x{e}") for e in range(2)]
        self.Bh = P.sb([128, 515], F32, "ssd_B")
        self.Ch = P.sb([128, 515], F32, "ssd_C")
        for t in self.xh + [self.Bh, self.Ch]:
            P.memset(t[:, 0:3], 0.0)
        self.S = [P.sb([128, 64], F32, f"ssd_S{e}") for e in range(2)]
        self.Sb = [P.sb([128, 64], BF16, f"ssd_Sb{e}") for e in range(2)]
        for t in self.S + self.Sb:
            P.memset(t[:], 0.0)
        self.Arow = P.sb([128, 2], F32, "ssd_Arow")
        P.act(self.Arow[:], st.pbc[:, BC_ALOG:BC_ALOG + 2], AF.Exp)
        P.ts(self.Arow[:], self.Arow[:], -1.0, None, ALU.mult)

    def block(self, sb):
        st = self.st
        P, pb, pp, mk, cn = st.P, st.pb, st.pp, st.mk, st.cn
        t0 = sb * 512
        mrk = P.mark()
        f = lambda n, r=128, c=512, dt=F32: P.sb([r, c], dt, "ssd_" + n)
        z = [f(f"z{e}", 64) for e in range(2)]
        acc = f("acc")
        xs = [f(f"xs{e}", 64) for e in range(2)]
        xsb = [f(f"xsb{e}", 64, dt=BF16) for e in range(2)]
        Bc = f("Bc", dt=BF16)
        Cc = f("Cc")
        dtt = P.sb([128, 4, 4], F32, "ssd_dtt")
        at = P.sb([128, 4, 2], F32, "ssd_at")
        E = f("E", c=512)
        l1 = [f(f"l1{e}", c=128) for e in range(2)]
        l2 = [f(f"l2{e}", c=128) for e in range(2)]
        CBm = f("CBm", c=128)
        Gt = [f(f"Gt{e}", c=128, dt=BF16) for e in range(2)]
        Cst = [f(f"Cst{e}", c=128, dt=BF16) for e in range(2)]
        dq = P.sb([128, 4], F32, "ssd_dq")
        xdt = [P.sb([128, 64], BF16, f"ssd_xdt{e}") for e in range(2)]
        Bw = [f(f"Bw{e}", c=128, dt=BF16) for e in range(2)]
        yo = [f(f"yo{e}", 64, dt=BF16) for e in range(2)]
        yv = f("yv", 64)
        for e in range(2):
            st.project(C_Z + e * 64, 64, z[e][:], pb[e], evac="act")
        for e in range(2):
            st.project(C_X + e * 64, 64, self.xh[e][:, 3:515], pb[e], evac="dve")
        st.project(C_B, 128, self.Bh[:, 3:515], pb[0], evac="act")
        st.project(C_C, 128, self.Ch[:, 3:515], pb[1], evac="dve")
        for j in range(4):
            for k in range(8):
                P.matmul(pb[2][:, j * 4:j * 4 + 3], st.hT[:, k, j * 128:(j + 1) * 128], st.wB[:, k, C_DT:C_DT + 3], start=(k == 0), stop=(k == 7))
        P.copy(dtt[:, :, 0:3], pb[2][:, 0:16].c3(4)[:, :, 0:3])
        st.fraw = dtt
        P.tt(at[:], dtt[:, :, 0:2], st.pbc[:, BC_DTB:BC_DTB + 2].bmid(4), ALU.add)
        P.act(at[:], at[:], AF.Exp)
        P.act(dtt[:, :, 0:2], at[:], AF.Ln, bias=1.0)
        P.tt(at[:], dtt[:, :, 0:2], self.Arow[:].bmid(4), ALU.mult)
        def conv(tile, rows, wc, bc, out, out2=None):
            a_ = acc[0:rows, :]
            P.ts(a_, tile[:, 0:512], pp[0:rows, wc:wc + 1], None, ALU.mult)
            for k in range(1, 4):
                P.stt(a_, tile[:, k:k + 512], pp[0:rows, wc + k:wc + k + 1], a_, ALU.mult, ALU.add)
            P.act(out, a_, AF.Silu, bias=pp[0:rows, bc:bc + 1])
            if out2 is not None:
                P.copy(out2, out, eng="pool")
            P.copy(tile[:, 0:3], tile[:, 512:515], eng="pool")
        for e in range(2):
            conv(self.xh[e], 64, PP_CW + 4 * e, PP_CB + e, xs[e][:], xsb[e][:])
        conv(self.Bh, 128, PP_CW + 8, PP_CB + 2, Bc[:])
        conv(self.Ch, 128, PP_CW + 12, PP_CB + 3, Cc[:])
        Ccb = f("Ccb", dt=BF16)
        P.copy(Ccb[:], Cc[:], eng="pool")
        for j in range(4):
            cs = slice(j * 128, (j + 1) * 128)
            P.matmul(pb[2][:, 0:128], Bc[:, cs], Ccb[:, cs], start=True, stop=True)
            P.tt(CBm[:], pb[2][:, 0:128], mk[:, MK_TRI, :], ALU.mult)
            P.transpose(st.pt_bf[1][:, 0:128], Bc[:, cs], cn.ident_bf[:])
            for e in range(2):
                acol = at[:, j, e:e + 1]
                P.ts(l1[e][:], mk[:, MK_SLF, :], acol, None, ALU.mult)
                P.ts(l2[e][:], cn.ones_f[:], acol, None, ALU.mult)
                P.matmul(pb[3][:, e * 256:e * 256 + 128], l1[e][:], mk[:, MK_TRI, :], start=True, stop=True)
                P.matmul(pb[3][:, e * 256 + 128:e * 256 + 256], l2[e][:], mk[:, MK_TRI, :], start=True, stop=True)
                P.matmul(pb[2][:, 128 + 2 * e:129 + 2 * e], mk[:, MK_SLF, :], acol, start=True, stop=True)
                P.matmul(pb[2][:, 129 + 2 * e:130 + 2 * e], cn.ones_f[:], acol, start=True, stop=True)
            P.act(E[:], pb[3][:, :], AF.Exp)
            P.act(dq[:], pb[2][:, 128:132], AF.Exp)
            for e in range(2):
                P.tt(Gt[e][:], E[:, e * 256:e * 256 + 128], CBm[:], ALU.mult)
                P.tt(Cst[e][:], E[:, e * 256 + 128:e * 256 + 256], Cc[:, cs], ALU.mult)
                P.transpose(st.pt_bf[0][:, e * 64:(e + 1) * 64], xsb[e][:, cs], cn.ident_bf[0:64, 0:64])
                P.ts(xdt[e][:], st.pt_bf[0][:, e * 64:(e + 1) * 64], dtt[:, j, e:e + 1], None, ALU.mult)
                P.ts(Bw[e][:], st.pt_bf[1][:, 0:128], dq[:, 2 * e:2 * e + 1], None, ALU.mult)
                yreg = pb[4 + e][0:64, cs]
                P.matmul(yreg, xdt[e][:], Gt[e][:], start=True, stop=False)
                P.matmul(yreg, self.Sb[e][:], Cst[e][:], start=False, stop=True)
                sreg = pb[2][:, 256 + e * 64:256 + (e + 1) * 64]
                P.matmul(sreg, Bw[e][:], xdt[e][:], start=True, stop=True)
                P.stt(self.S[e][:], self.S[e][:], dq[:, 2 * e + 1:2 * e + 2], sreg, ALU.mult, ALU.add)
                P.copy(self.Sb[e][:], self.S[e][:], eng="act")
        for e in range(2):
            P.act(z[e][:], z[e][:], AF.Silu)
            P.stt(yv[:], xs[e][:], pp[0:64, PP_D + e:PP_D + e + 1], pb[4 + e][0:64, :], ALU.mult, ALU.add)
            P.tt(yo[e][:], yv[:], z[e][:], ALU.mult)
            P.dma(st.yT_d[64 + e * 64:64 + (e + 1) * 64, t0:t0 + 512], yo[e][:], q="pool")
        P.release(mrk)


class FoxMixer:
    def __init__(self, st):
        self.st = st
        P = st.P
        L = st.L
        NB = L // 128
        self.KT = P.sb([67, L], BF16, "fox_KT")
        P.memset(self.KT[64:67, :], 1.0)
        self.Vaug = P.sb([128, NB, 65], BF16, "fox_V")
        P.memset(self.Vaug[:, :, 64:65], 1.0)
        self.negF = P.sb([128, NB], F32, "fox_negF")
        self.totb = P.sb([128, 1], F32, "fox_totb")
        P.memset(self.totb[:], 0.0)
        self.negfb = P.sb([128, 1], F32, "fox_negfb")
        P.ts(self.negfb[:], st.pbc[:, BC_FB:BC_FB + 1], -1.0, None, ALU.mult)
        self.qg = P.sb([64, 1], F32, "fox_qg")
        P.ts(self.qg[:], st.pp[0:64, PP_QG:PP_QG + 1], 0.125, None, ALU.mult)
        self.nmb = P.sb([128, 128], BF16, "fox_nmb")
        P.copy(self.nmb[:], st.mk[:, MK_NEG, :])
        self.nlw = P.sb([128, 4, 67], F32, "fox_nlw")
        P.memset(self.nlw[:], 0.0)
        self.pT = [P.sb([128, 512], BF16, f"fox_pT{i}") for i in range(2)]

    def block(self, sb):
        st = self.st
        P, pb, pp, mk, cn = st.P, st.pb, st.pp, st.mk, st.cn
        t0 = sb * 512
        mrk = P.mark()
        f = lambda n, r=64, c=512, dt=F32: P.sb([r, c], dt, "fox_" + n)
        q, k, v = f("q"), f("k"), f("v")
        sq = f("sq", dt=BF16)
        rs = f("rs")
        vb = f("vb", dt=BF16)
        QT = f("QT", 67, dt=BF16)
        nl = P.sb([128, 4], F32, "fox_nl")
        totc = P.sb([128, 4], F32, "fox_totc")
        tt4 = P.sb([128, 8], F32, "fox_tt4")
        Fv = f("Fv", 67)
        r1 = f("r1", 67)
        hf = f("hf", 67)
        hb = f("hb", 67, dt=BF16)
        ta = f("ta", 67)
        o = f("o", 65)
        rec = f("rec")
        yo = f("yo", dt=BF16)
        st.project(C_FQ, 64, q[:], pb[2], evac="act")
        st.project(C_FK, 64, k[:], pb[3], evac="dve")
        st.project(C_FV, 64, v[:], pb[2], evac="act")
        for j in range(4):
            for kk in range(8):
                P.matmul(pb[3][:, j:j + 1], st.hT[:, kk, j * 128:(j + 1) * 128], st.wB[:, kk, C_F:C_F + 1], start=(kk == 0), stop=(kk == 7))
        P.act(nl[:], pb[3][:, 0:4], AF.Exp, scale=-1.0, bias=self.negfb[:])
        P.act(nl[:], nl[:], AF.Ln, bias=1.0)
        P.matmul(pb[3][:, 8:12], cn.ones_f[:], nl[:], start=True, stop=True)
        P.matmul(pb[3][:, 12:16], mk[:, MK_TRI, :], nl[:], start=True, stop=True)
        P.copy(tt4[:, 0:8], pb[3][:, 8:16])
        P.copy(totc[:, 0:1], self.totb[:])
        for j in range(1, 4):
            P.tt(totc[:, j:j + 1], totc[:, j - 1:j], tt4[:, j - 1:j], ALU.add)
        P.tt(self.totb[:], totc[:, 3:4], tt4[:, 3:4], ALU.add)
        P.tt(self.negF[:, 4 * sb:4 * sb + 4], tt4[:, 4:8], totc[:], ALU.add)
        for j in range(4):
            P.copy(self.nlw[:, j, 64:67], nl[:, j:j + 1].bcast([128, 3]))
        for j in range(4):
            P.matmul(pb[2][0:67, j * 128:(j + 1) * 128], self.nlw[:, j, :], mk[:, MK_TRI, :], start=True, stop=True)
        for j in range(4):
            cs = slice(j * 128, (j + 1) * 128)
            P.ts(Fv[64:67, cs], pb[2][64:67, cs], totc[64:67, j:j + 1], -1.0, ALU.add, ALU.mult)
        R = slice(64, 67)
        P.copy(hb[R, :], Fv[R, :])
        P.copy(hf[R, :], hb[R, :])
        P.ts(ta[R, :], hf[R, :], pp[R, PP_SEL:PP_SEL + 1], None, ALU.mult)
        P.tt(r1[R, :], Fv[R, :], hf[R, :], ALU.subtract)
        P.copy(hb[R, :], r1[R, :])
        P.copy(hf[R, :], hb[R, :])
        P.stt(ta[R, :], hf[R, :], pp[R, PP_SEL + 1:PP_SEL + 2], ta[R, :], ALU.mult, ALU.add)
        P.tt(r1[R, :], r1[R, :], hf[R, :], ALU.subtract)
        P.copy(hb[R, :], r1[R, :])
        P.copy(hf[R, :], hb[R, :])
        P.stt(QT[R, :], hf[R, :], pp[R, PP_SEL + 2:PP_SEL + 3], ta[R, :], ALU.mult, ALU.add)
        for src, gcol, dst in ((q, self.qg[:], QT[0:64, :]), (k, pp[0:64, PP_KG:PP_KG + 1], self.KT[0:64, t0:t0 + 512])):
            P.tt(sq[:], src[:], src[:], ALU.mult)
            P.matmul(pb[3][0:64, :], cn.ones_bf[0:64, 0:64], sq[:], start=True, stop=True)
            P.ts(rs[:], pb[3][0:64, :], 1.0 / 64, EPS, ALU.mult, ALU.add)
            rsqrt_inplace(P, rs[:])
            P.stt(dst, src[:], gcol, rs[:], ALU.mult, ALU.mult)
        P.copy(vb[:], v[:], eng="pool")
        for j in range(4):
            P.transpose(st.pt_bf[0][:, j * 64:(j + 1) * 64], vb[:, j * 128:(j + 1) * 128], cn.ident_bf[0:64, 0:64])
        P.copy(self.Vaug[:, 4 * sb:4 * sb + 4, 0:64], st.pt_bf[0][:, 0:256].c3())
        po = pb[4]
        nkb = 4 * sb + 4
        for kb in range(nkb):
            ps = pb[kb % 2]
            pT = self.pT[kb % 2]
            Kb = self.KT[0:67, kb * 128:(kb + 1) * 128]
            d = kb - 4 * sb
            if d < 0:
                c0 = 0
                P.matmul(ps[:, :], Kb, QT[:, :], start=True, stop=True)
            else:
                c0 = d * 128
                P.matmul(ps[:, c0:c0 + 128], Kb, QT[:, c0:c0 + 128], start=True, stop=False)
                P.matmul(ps[:, c0:c0 + 128], cn.ident_bf[:], self.nmb[:], start=False, stop=True)
                if c0 + 128 < 512:
                    P.matmul(ps[:, c0 + 128:512], Kb, QT[:, c0 + 128:512], start=True, stop=True)
            P.act(pT[:, c0:512], ps[:, c0:512], AF.Exp, bias=self.negF[:, kb:kb + 1])
            Vb = self.Vaug[:, kb, :]
            if c0 > 0:
                P.memset(pT[:, 0:c0], 0.0)
            P.matmul(po[0:65, :], Vb, pT[:, :], start=(kb == 0), stop=(kb == nkb - 1))
        P.copy(o[:], po[0:65, :])
        P.matmul(pb[5][0:64, :], mk[0:65, MK_SEL, 0:64], o[:], start=True, stop=True)
        P.recip(rec[:], pb[5][0:64, :])
        P.tt(yo[:], o[0:64, :], rec[:], ALU.mult)
        P.dma(st.yT_d[192:256, t0:t0 + 512], yo[:], q="pool")
        P.release(mrk)


def build_B(L, layer, parts=("rw", "ssd", "fox")):
    nc = bass.Bass("TRN2", target_bir_lowering=False)
    P = Prog(nc)
    NSB = L // 512
    NWc = NW1 if layer == 1 else NW0
    x_d = P.dram("x", [L, D], F32, "ExternalInput")
    c_d = P.dram("c", [1, D], F32, "ExternalInput")
    ada_w_d = P.dram("ada_w", [D, 6 * D], F32, "ExternalInput")
    ada_b_d = P.dram("ada_b", [1, 6 * D], F32, "ExternalInput")
    nmix_d = P.dram("nmix", [1, D], F32, "ExternalInput")
    wB_d = P.dram("wB", [D, NWc], F32, "ExternalInput")
    pp_d = P.dram("pp", [128, NPP], F32, "ExternalInput")
    pbc_d = P.dram("pbc", [1, NBC], F32, "ExternalInput")
    mask_d = P.dram("masks", [128, NMASK, 128], F32, "ExternalInput")
    wup_d = P.dram("wup", [64, 64], F32, "ExternalInput")
    aup_d = P.dram("aup", [64, 64], F32, "ExternalInput")
    gup_d = P.dram("gup", [160, 64], F32, "ExternalInput")
    if layer == 1:
        vup_d = P.dram("vup", [32, 64], F32, "ExternalInput")
        vf_d = P.dram("vfT", [64, L], F32, "ExternalInput")
    else:
        vfo_d = P.dram("vfT_out", [64, L], F32, "ExternalOutput")
    yT_d = P.dram("yT", [256, L], BF16, "ExternalOutput")

    cn = mk_consts(P)
    pb = [P.ps([128, 512], F32, f"pb{i}") for i in range(6)]
    pt_bf = [P.ps([128, 1024], BF16, "ptA"), P.ps([128, 1024], BF16, "ptB")]
    ht = HT(P, cn, pt_bf)

    pp = P.sb([128, NPP], F32, "pp")
    P.dma(pp[:], pp_d[:, :])
    pbc = P.sb([128, NBC], F32, "pbc")
    P.dma(pbc[:], pbc_d[0:1, :].bcast([128, NBC]))
    mk = P.sb([128, NMASK, 128], F32, "mk")
    P.dma(mk[:], mask_d[:, :, :], q="act")
    A_m = P.sb([128, 8], F32, "A_m")
    sh_m = P.sb([128, 8], F32, "sh_m")
    m0 = P.mark()
    cols, _ = emit_mod(P, cn, c_d, ada_w_d, ada_b_d, [0, 1], [], pb[0], pb[1])
    nmix_c = load_col(P, nmix_d[0, :], 8, "nmix_c")
    P.ts(A_m[:], cols[1][:], 1.0, None, ALU.add)
    P.tt(A_m[:], A_m[:], nmix_c[:], ALU.mult)
    P.copy(sh_m[:], cols[0][:])
    P.release(m0)
    wB = P.sb([128, 8, NWc], BF16, "wB")
    wup = P.sb([64, 64], BF16, "wup")
    aup = P.sb([64, 64], BF16, "aup")
    gupa = P.sb([128, 64], BF16, "gupa")
    gupb = P.sb([32, 64], BF16, "gupb")
    if layer == 1:
        vup = P.sb([32, 64], BF16, "vup")
    m_ld = P.mark()
    ld = Loader(P)
    for k in range(8):
        ld.load(wB[:, k, :], wB_d[k * 128:(k + 1) * 128, :])
    ld.load(wup[:, :], wup_d[:, :], np_=64)
    ld.load(aup[:, :], aup_d[:, :], np_=64)
    ld.load(gupa[:, :], gup_d[0:128, :])
    ld.load(gupb[:, :], gup_d[128:160, :], np_=32)
    if layer == 1:
        ld.load(vup[:, :], vup_d[:, :], np_=32)
    P.release(m_ld)

    xt = [P.sb([128, D], F32, f"xt{i}") for i in range(4)]
    hT = P.sb([128, 8, 512], BF16, "hT")

    def project(c0, ncol, dst, psb, evac="act"):
        for k in range(8):
            P.matmul(psb[0:ncol, :], wB[:, k, c0:c0 + ncol], hT[:, k, :], start=(k == 0), stop=(k == 7))
        if evac == "act":
            P.act(dst, psb[0:ncol, :], AF.Copy)
        else:
            P.copy(dst, psb[0:ncol, :])

    st = Ctx()
    st.P, st.cn, st.pb, st.pp, st.pbc, st.mk, st.hT, st.wB, st.project = P, cn, pb, pp, pbc, mk, hT, wB, project
    st.L, st.layer, st.NSB, st.yT_d, st.pt_bf = L, layer, NSB, yT_d, pt_bf
    st.wup, st.aup, st.gupa, st.gupb = wup, aup, gupa, gupb
    if layer == 1:
        st.vup, st.vf_d = vup, vf_d
    else:
        st.vfo_d = vfo_d
    rw = RWMixer(st) if "rw" in parts else None
    ssd = SSDMixer(st) if "ssd" in parts else None
    fox = FoxMixer(st) if "fox" in parts else None

    for sb in range(NSB):
        t0 = sb * 512
        for i in range(4):
            P.dma(xt[i][:], x_d[t0 + i * 128:t0 + (i + 1) * 128, :], q="sp" if i % 2 == 0 else "act")
        for i in range(4):
            ht.emit(xt[i][:], A_m, sh_m, lambda k, i=i: hT[:, k, i * 128:(i + 1) * 128])
        if rw:
            rw.block(sb)
        if ssd:
            ssd.block(sb)
        if fox:
            fox.block(sb)
    print("B peak sbuf", P.peak, "ops", len(P.ops))
    return P.emit()


RW_W, SSD_W, FOX_W = 512, 1024, 512
RW_COLS_, SSD_COLS_, FOX_COLS_ = 1824, 3088, 1544


def core_inputs_B(inp, l, i, x, vfT=None):
    w_in = inp['w_in'][l]
    o_rw, o_ssd, o_fox = 0, RW_COLS_, RW_COLS_ + SSD_COLS_
    hs = slice(i * 64, (i + 1) * 64)
    W, SW, FW = RW_W, SSD_W, FOX_W
    g = i // 2
    ar = np.arange
    cols = [ar(o_rw + i * 64, o_rw + (i + 1) * 64), ar(o_rw + W + i * 64, o_rw + W + (i + 1) * 64),
            ar(o_rw + 2 * W + i * 64, o_rw + 2 * W + (i + 1) * 64),
            ar(o_rw + 3 * W, o_rw + 3 * W + 64), ar(o_rw + 3 * W + 64, o_rw + 3 * W + 128), ar(o_rw + 3 * W + 128, o_rw + 3 * W + 288),
            ar(o_ssd + i * 128, o_ssd + (i + 1) * 128), ar(o_ssd + SW + i * 128, o_ssd + SW + (i + 1) * 128),
            ar(o_ssd + 2 * SW + g * 128, o_ssd + 2 * SW + (g + 1) * 128), ar(o_ssd + 2 * SW + 512 + g * 128, o_ssd + 2 * SW + 512 + (g + 1) * 128),
            ar(o_fox + i * 64, o_fox + (i + 1) * 64), ar(o_fox + FW + i * 64, o_fox + FW + (i + 1) * 64), ar(o_fox + 2 * FW + i * 64, o_fox + 2 * FW + (i + 1) * 64),
            ar(o_ssd + 2 * SW + 1024 + 2 * i, o_ssd + 2 * SW + 1024 + 2 * i + 2), np.array([o_fox + 3 * FW + i])]
    cols = np.concatenate(cols)
    wB = w_in[:, cols]
    if l == 1:
        wB = np.concatenate([wB, inp['rw_v_down'][0]], axis=1)
    pp = np.zeros((128, NPP), np.float32)
    mu = inp['rw_mu'][l]
    pp[0:64, PP_MU + 0] = mu[0 * W + i * 64:0 * W + (i + 1) * 64]
    pp[0:64, PP_MU + 1] = mu[1 * W + i * 64:1 * W + (i + 1) * 64]
    pp[0:64, PP_MU + 2] = mu[2 * W + i * 64:2 * W + (i + 1) * 64]
    pp[0:64, PP_MU + 3] = mu[3 * W:3 * W + 64]
    pp[0:64, PP_MU + 4] = mu[3 * W + 64:3 * W + 128]
    pp[0:128, PP_MU + 5] = mu[3 * W + 128:3 * W + 256]
    pp[0:32, PP_MU + 6] = mu[3 * W + 256:3 * W + 288]
    pp[0:64, PP_W0] = inp['rw_w0'][l][hs]
    pp[0:64, PP_A0] = inp['rw_a0'][l][hs]
    pp[0:64, PP_KK] = inp['rw_k_k'][l][hs]
    pp[0:64, PP_KA] = inp['rw_k_a'][l][hs]
    pp[0:64, PP_RK] = inp['rw_r_k'][l][i]
    if l == 1:
        pp[0:64, PP_V0] = inp['rw_v0'][0][hs]
    cw, cb = inp['ssd_conv_w'][l], inp['ssd_conv_b'][l]
    xs_ = slice(i * 128, (i + 1) * 128)
    bs_ = slice(SW + g * 128, SW + (g + 1) * 128)
    cs_ = slice(SW + 512 + g * 128, SW + 512 + (g + 1) * 128)
    for t in range(4):
        pp[0:64, PP_CW + t] = cw[t, xs_][0:64]
        pp[0:64, PP_CW + 4 + t] = cw[t, xs_][64:128]
        pp[:, PP_CW + 8 + t] = cw[t, bs_]
        pp[:, PP_CW + 12 + t] = cw[t, cs_]
    pp[0:64, PP_CB] = cb[xs_][0:64]
    pp[0:64, PP_CB + 1] = cb[xs_][64:128]
    pp[:, PP_CB + 2] = cb[bs_]
    pp[:, PP_CB + 3] = cb[cs_]
    pp[0:64, PP_QG] = inp['fox_q_gain'][l]
    pp[0:64, PP_KG] = inp['fox_k_gain'][l]
    pp[:, PP_D] = inp['ssd_d'][l][2 * i]
    pp[:, PP_D + 1] = inp['ssd_d'][l][2 * i + 1]
    pp[64, PP_SEL] = 1.0
    pp[65, PP_SEL + 1] = 1.0
    pp[66, PP_SEL + 2] = 1.0
    pbc = np.zeros((1, NBC), np.float32)
    pbc[0, BC_LNW:BC_LNW + 64] = inp['rw_lnx_w'][l][hs]
    pbc[0, BC_LNB:BC_LNB + 64] = inp['rw_lnx_b'][l][hs]
    pbc[0, BC_DTB:BC_DTB + 2] = inp['ssd_dt_bias'][l][2 * i:2 * i + 2]
    pbc[0, BC_ALOG:BC_ALOG + 2] = inp['ssd_a_log'][l][2 * i:2 * i + 2]
    pbc[0, BC_FB] = inp['fox_f_bias'][l][i]
    m = dict(x=x, c=inp['c'], ada_w=inp['ada_w'][l], ada_b=inp['ada_b'][l][None], nmix=inp['norm_mix'][l][None],
             wB=np.ascontiguousarray(wB), pp=pp, pbc=pbc, masks=make_masks(),
             wup=np.ascontiguousarray(inp['rw_w_up'][l][:, hs]), aup=np.ascontiguousarray(inp['rw_a_up'][l][:, hs]),
             gup=np.ascontiguousarray(inp['rw_g_up'][l][:, hs]))
    if l == 1:
        m['vup'] = np.ascontiguousarray(inp['rw_v_up'][0][:, hs])
        m['vfT'] = vfT
    return m


def common_inputs_C(inp, l):
    off_gate = RW_COLS_ + SSD_COLS_ + FOX_COLS_
    common = dict(
        c=inp['c'], ada_w=inp['ada_w'][l], ada_b=inp['ada_b'][l][None], nmix=inp['norm_mix'][l][None], nffn=inp['norm_ffn'][l][None],
        wgate=np.ascontiguousarray(inp['w_in'][l][:, off_gate:]), gateb=inp['gate_b'][l][None],
        proj=np.concatenate([inp['proj_rw'][l], inp['proj_ssd'][l], inp['proj_fox'][l]], axis=0), wout=inp['w_out'][l], ssdn=inp['ssd_norm'][l][None])
    if l % 2 == 1:
        common.update(routT=np.ascontiguousarray(inp['moe_router'][l // 2].T), wgu=inp['moe_w_gu'][l // 2], wdn=inp['moe_w_down'][l // 2])
    else:
        common.update(wgu=inp['ffn_w_gu'][l // 2][None], wdn=inp['ffn_w_down'][l // 2][None])
    return common


def assemble_yT(resB, L):
    yT = np.empty((2048, L), dtype=resB[0]['yT'].dtype)
    for i, r in enumerate(resB):
        y = r['yT']
        yT[i * 64:(i + 1) * 64] = y[0:64]
        yT[512 + i * 128:512 + (i + 1) * 128] = y[64:192]
        yT[1536 + i * 64:1536 + (i + 1) * 64] = y[192:256]
    return yT


NCORES = 8


def kernel(**inputs):
    inp = {k: np.asarray(v) for k, v in inputs.items()}
    L = inp['x'].shape[1]
    T_c = L // NCORES
    x = np.ascontiguousarray(inp['x'][0])
    cores = list(range(NCORES))
    vfT = [None] * NCORES
    for l in range(2):
        ncB = build_B(L, l)
        mapsB = [core_inputs_B(inp, l, i, x, vfT[i]) for i in cores]
        resB = run_bass_kernel_spmd(ncB, mapsB, core_ids=cores).results
        if l == 0:
            vfT = [np.ascontiguousarray(r['vfT_out']) for r in resB]
        yT = assemble_yT(resB, L)
        ncC = build_C(T_c, moe=(l % 2 == 1))
        common = common_inputs_C(inp, l)
        mapsC = []
        for j in cores:
            m = dict(common)
            m['x'] = np.ascontiguousarray(x[j * T_c:(j + 1) * T_c])
            m['yT'] = np.ascontiguousarray(yT[:, j * T_c:(j + 1) * T_c])
            mapsC.append(m)
        resC = run_bass_kernel_spmd(ncC, mapsC, core_ids=cores).results
        x = np.concatenate([r['out'] for r in resC], axis=0)
    return x[None].astype(np.float32)
```

```python
from contextlib import ExitStack
import numpy as np
import concourse.bass as bass
import concourse.mybir as mybir
from concourse.bass_utils import run_bass_kernel_spmd

F32 = mybir.dt.float32
BF16 = mybir.dt.bfloat16
ALU = mybir.AluOpType
AF = mybir.ActivationFunctionType
AX = mybir.AxisListType


class Tl:
    def __init__(self, t, name):
        self.t = t
        self.name = name
        self.last_w = None
        self.readers = []
        self.alias = None

    def __getitem__(self, key):
        return V(self, self.t[key])


class V:
    def __init__(self, tl, ap):
        self.tl = tl
        self.ap = ap

    def __getitem__(self, key):
        return V(self.tl, self.ap[key])

    def bitcast(self, dt):
        return V(self.tl, self.ap.bitcast(dt))

    def rearrange(self, s, **kw):
        return V(self.tl, self.ap.rearrange(s, **kw))

    def bcast(self, shape):
        return V(self.tl, self.ap.to_broadcast(shape))

    def bmid(self, n):
        p, m = self.ap.shape
        return V(self.tl, self.ap.unsqueeze(1).to_broadcast([p, n, m]))

    def binner(self, m):
        p, n = self.ap.shape
        return V(self.tl, self.ap.unsqueeze(2).to_broadcast([p, n, m]))

    def c3(self, t=64):
        return V(self.tl, self.ap.rearrange("p (c t) -> p c t", t=t))


class Op:
    __slots__ = ("eng", "fn", "reads", "writes", "dma", "deps", "need_inc", "cnt", "slot", "slot_prev")

    def __init__(self, eng, fn, reads, writes, dma):
        self.eng = eng
        self.fn = fn
        self.reads = reads
        self.writes = writes
        self.dma = dma
        self.deps = []
        self.need_inc = False
        self.cnt = 0
        self.slot = None
        self.slot_prev = 0


NDMA_SLOTS = 8
ARENA_BYTES = 212736
ENGS = ["pe", "act", "dve", "pool", "sp"]


class Prog:
    def __init__(self, nc):
        self.nc = nc
        self.ops = []
        self.stack = ExitStack()
        self.n_t = 0
        self.arena_t = self.stack.enter_context(nc.sbuf_tensor("arena", [128, ARENA_BYTES // 2], BF16))
        self.bump = 0
        self.live = []
        self.freed = []
        self.peak = 0

    def sb(self, shape, dt, name=None):
        self.n_t += 1
        name = name or f"t{self.n_t}"
        esz = 4 if dt == F32 else 2
        n = int(np.prod(shape[1:]))
        nbytes = n * esz
        nal = (nbytes + 63) // 64 * 64
        off = self.bump
        assert off + nal <= ARENA_BYTES, f"SBUF arena overflow allocating {name}: {off}+{nal}"
        self.bump += nal
        self.peak = max(self.peak, self.bump)
        ap = self.arena_t[0:shape[0], off // 2:(off + nbytes) // 2]
        if dt == F32:
            ap = ap.bitcast(F32)
        if len(shape) == 3:
            ap = ap.rearrange("p (a b) -> p a b", b=shape[2])
        tl = Tl(ap, name)
        al = [f for (o, e, f) in self.freed if o < off + nal and e > off]
        tl.alias = al if al else None
        self.live.append((off, off + nal, tl))
        return tl

    def mark(self):
        return (self.bump, len(self.live))

    def release(self, m):
        b, n = m
        self.freed.extend(self.live[n:])
        del self.live[n:]
        self.bump = b

    def ps(self, shape, dt, name=None):
        self.n_t += 1
        name = name or f"p{self.n_t}"
        t = self.stack.enter_context(self.nc.psum_tensor(name, list(shape), dt))
        return Tl(t, name)

    def dram(self, name, shape, dt, kind):
        t = self.nc.dram_tensor(name, list(shape), dt, kind=kind).ap()
        return Tl(t, name)

    def _add(self, eng, fn, reads, writes, dma=False):
        r = []
        for v in reads:
            if isinstance(v, V) and v.tl not in r:
                r.append(v.tl)
        w = []
        for v in writes:
            if isinstance(v, V) and v.tl not in w:
                w.append(v.tl)
        self.ops.append(Op(eng, fn, r, w, 16 if dma is True else (dma or 0)))

    @staticmethod
    def _a(v):
        return v.ap if isinstance(v, V) else v

    def dma(self, out, in_, q="sp", **kw):
        a = self._a
        self._add(q, lambda e: e.dma_start(out=a(out), in_=a(in_), **kw), [in_], [out], dma=True)

    def matmul(self, out, lhsT, rhs, start=True, stop=True, **kw):
        a = self._a
        self._add("pe", lambda e: e.matmul(a(out), a(lhsT), a(rhs), start=start, stop=stop, **kw), [lhsT, rhs], [out])

    def transpose(self, out, in_, ident):
        a = self._a
        self._add("pe", lambda e: e.transpose(a(out), a(in_), a(ident)), [in_, ident], [out])

    def act(self, out, in_, func, bias=None, scale=None, accum_out=None, eng="act"):
        a = self._a
        kw = {}
        if bias is not None:
            kw["bias"] = a(bias)
        if scale is not None:
            kw["scale"] = a(scale)
        if accum_out is not None:
            kw["accum_out"] = a(accum_out)
        self._add(eng, lambda e: e.activation(a(out), a(in_), func, **kw), [in_, bias, scale], [out, accum_out])

    def ts(self, out, in0, s1, s2, op0, op1=None, accum_out=None, eng="dve"):
        a = self._a
        kw = {}
        if accum_out is not None:
            kw["accum_out"] = a(accum_out)
        if op1 is None:
            self._add(eng, lambda e: e.tensor_single_scalar(a(out), a(in0), a(s1), op0), [in0, s1], [out])
        else:
            self._add(eng, lambda e: e.tensor_scalar(a(out), a(in0), a(s1), a(s2), op0, op1, **kw), [in0, s1, s2], [out, accum_out])

    def tt(self, out, in0, in1, op, eng="dve"):
        a = self._a
        self._add(eng, lambda e: e.tensor_tensor(a(out), a(in0), a(in1), op), [in0, in1], [out])

    def stt(self, out, in0, scalar, in1, op0, op1, accum_out=None, eng="dve"):
        a = self._a
        kw = {}
        if accum_out is not None:
            kw["accum_out"] = a(accum_out)
        self._add(eng, lambda e: e.scalar_tensor_tensor(a(out), a(in0), a(scalar), a(in1), op0, op1, **kw), [in0, scalar, in1], [out, accum_out])

    def copy(self, out, in_, eng="dve"):
        a = self._a
        if eng == "act":
            self._add(eng, lambda e: e.activation(a(out), a(in_), AF.Copy), [in_], [out])
        else:
            self._add(eng, lambda e: e.tensor_copy(a(out), a(in_)), [in_], [out])

    def memset(self, out, val, eng="pool"):
        a = self._a
        self._add(eng, lambda e: e.memset(a(out), val), [], [out])

    def recip(self, out, in_):
        a = self._a
        self._add("dve", lambda e: e.reciprocal(a(out), a(in_)), [in_], [out])

    def reduce(self, out, in_, op, axis=AX.X, eng="dve"):
        a = self._a
        self._add(eng, lambda e: e.tensor_reduce(a(out), a(in_), axis, op), [in_], [out])

    def affine_select(self, out, in_, pattern, compare_op, fill, base, channel_multiplier):
        a = self._a
        self._add("pool", lambda e: e.affine_select(out=a(out), in_=a(in_), pattern=pattern, compare_op=compare_op, fill=fill, base=base, channel_multiplier=channel_multiplier), [in_], [out])

    def generic(self, eng, fn, reads, writes, dma=False):
        self._add(eng, fn, reads, writes, dma)

    def emit(self):
        nc = self.nc
        ops = self.ops
        for i, op in enumerate(ops):
            deps = {}
            for tl in op.reads + op.writes:
                if tl.alias:
                    for par in tl.alias:
                        tl.readers.extend(par.readers)
                        if par.last_w is not None:
                            tl.readers.append(par.last_w)
                    tl.alias = None
            for tl in op.reads:
                if tl.last_w is not None:
                    deps[tl.last_w] = "raw"
            for tl in op.writes:
                if tl.last_w is not None and tl.last_w not in deps:
                    deps[tl.last_w] = "waw"
                for r in tl.readers:
                    if r not in deps:
                        deps[r] = "war"
            for p, kind in deps.items():
                if p == i:
                    continue
                po = ops[p]
                if (not po.dma) and (not op.dma) and po.eng == op.eng and op.eng == "pe":
                    continue
                op.deps.append(p)
                po.need_inc = True
            for tl in op.reads:
                tl.readers.append(i)
            for tl in op.writes:
                tl.last_w = i
                tl.readers = []
        cnt = {e: 0 for e in ENGS}
        slot_n = {e: 0 for e in ENGS}
        slot_cnt = {}
        for op in ops:
            if op.dma:
                sk = op.eng if op.dma == 16 else op.eng + "_cc"
                slot_n.setdefault(sk, 0)
                k = slot_n[sk] % NDMA_SLOTS
                slot_n[sk] += 1
                key = (sk, k)
                op.slot = key
                op.slot_prev = slot_cnt.get(key, 0)
                slot_cnt[key] = op.slot_prev + 1
                op.cnt = op.slot_prev + 1
            elif op.need_inc:
                cnt[op.eng] += 1
                op.cnt = cnt[op.eng]
        st = self.stack
        sem_e = {e: st.enter_context(nc.semaphore(f"s_{e}")) for e in ENGS}
        sem_d = {}
        for e in list(slot_n.keys()):
            if slot_n[e] > 0:
                for k in range(min(NDMA_SLOTS, slot_n[e])):
                    sem_d[(e, k)] = st.enter_context(nc.semaphore(f"d_{e}{k}"))
        block = st.enter_context(nc.Block())

        def run_engine(ename):
            def body(eng):
                waited = {}

                def wait(sem, key, val):
                    if waited.get(key, 0) >= val:
                        return
                    waited[key] = val
                    eng.wait_ge(sem, val)

                for op in ops:
                    if op.eng != ename:
                        continue
                    need = {}
                    for p in op.deps:
                        po = ops[p]
                        if po.dma:
                            key = ("d",) + po.slot
                            val = po.dma * po.cnt
                            sem = sem_d[po.slot]
                        else:
                            key = ("e", po.eng)
                            val = po.cnt
                            sem = sem_e[po.eng]
                        if need.get(key, (None, 0))[1] < val:
                            need[key] = (sem, val)
                    if op.dma and op.slot_prev > 0:
                        key = ("d",) + op.slot
                        val = op.dma * op.slot_prev
                        if need.get(key, (None, 0))[1] < val:
                            need[key] = (sem_d[op.slot], val)
                    for key, (sem, val) in need.items():
                        wait(sem, key, val)
                    ins = op.fn(eng)
                    if op.dma == 16:
                        ins.then_inc(sem_d[op.slot], 16)
                    elif op.dma:
                        ins.then_inc(sem_d[op.slot])
                    elif op.need_inc:
                        ins.then_inc(sem_e[ename], 1)
                for (e, k), sem in sem_d.items():
                    if e == ename or e == ename + "_cc":
                        wait(sem, ("d", e, k), (16 if e == ename else 1) * slot_cnt[(e, k)])
            return body

        used = set(op.eng for op in ops)
        if "pe" in used:
            block.tensor(run_engine("pe"))
        if "act" in used:
            block.scalar(run_engine("act"))
        if "dve" in used:
            block.vector(run_engine("dve"))
        if "pool" in used:
            block.gpsimd(run_engine("pool"))
        if "sp" in used:
            block.sync(run_engine("sp"))
        st.close()
        return nc


D = 1024
EPS = 1e-6


class Ctx:
    pass


def mk_consts(P):
    c = Ctx()
    c.ones_bf = P.sb([128, 128], BF16, "ones_bf")
    c.ones_f = P.sb([128, 128], F32, "ones_f")
    c.ident_bf = P.sb([128, 128], BF16, "ident_bf")
    c.ident_f = P.sb([128, 128], F32, "ident_f")
    P.memset(c.ones_bf[:], 1.0)
    P.memset(c.ones_f[:], 1.0)
    P.affine_select(c.ident_bf[:], c.ones_bf[:], [[-1, 128]], ALU.is_equal, 0.0, 0, 1)
    P.affine_select(c.ident_f[:], c.ones_f[:], [[-1, 128]], ALU.is_equal, 0.0, 0, 1)
    return c


def rsqrt_inplace(P, v, eps=None):
    if eps is not None:
        P.ts(v, v, eps, None, ALU.add)
    P.act(v, v, AF.Ln)
    P.act(v, v, AF.Exp, scale=-0.5)


def emit_mod(P, cn, c_d, ada_w_d, ada_b_d, col_blocks, row_blocks, ps_col, ps_row):
    siluc = P.sb([128, 8], F32, "siluc")
    P.dma(siluc[:], c_d[0, :].rearrange("(k p) -> p k", p=128), allow_slow_non_contiguous=True)
    P.act(siluc[:], siluc[:], AF.Silu)
    aw = P.sb([128, 8, 1024], F32, "aw_stage")
    cols, rows = {}, {}
    sbc = None
    if row_blocks:
        sbc = P.sb([128, 8, 128], F32, "siluc_bc")
        for kc in range(8):
            P.ts(sbc[:, kc, :], cn.ones_f[:], siluc[:, kc:kc + 1], None, ALU.mult)
    for j in sorted(set(col_blocks) | set(row_blocks)):
        P.dma(aw[:], ada_w_d[:, j * 1024:(j + 1) * 1024].rearrange("(kc p) n -> p kc n", p=128))
        if j in col_blocks:
            ab = P.sb([128, 8], F32, f"adab_c{j}")
            P.dma(ab[:], ada_b_d[0, j * 1024:(j + 1) * 1024].rearrange("(k p) -> p k", p=128), allow_slow_non_contiguous=True)
            for k in range(8):
                for kc in range(8):
                    P.matmul(ps_col[:, k:k + 1], aw[:, kc, k * 128:(k + 1) * 128], siluc[:, kc:kc + 1], start=(kc == 0), stop=(kc == 7))
            t = P.sb([128, 8], F32, f"modc{j}")
            P.tt(t[:], ps_col[:, 0:8], ab[:], ALU.add)
            cols[j] = t
        if j in row_blocks:
            t = P.sb([128, 1024], F32, f"modr{j}")
            P.dma(t[:], ada_b_d[0:1, j * 1024:(j + 1) * 1024].bcast([128, 1024]))
            for half in range(2):
                for kc in range(8):
                    P.matmul(ps_row[:], sbc[:, kc, :], aw[:, kc, half * 512:(half + 1) * 512], start=(kc == 0), stop=(kc == 7))
                P.tt(t[:, half * 512:(half + 1) * 512], t[:, half * 512:(half + 1) * 512], ps_row[:], ALU.add)
            rows[j] = t
    return cols, rows


def load_col(P, src_d_row, n, name):
    t = P.sb([128, n], F32, name)
    P.dma(t[:], src_d_row.rearrange("(k p) -> p k", p=128), allow_slow_non_contiguous=True)
    return t


def mk_Acol(P, normw_col, sc_col, name):
    t = P.sb([128, 8], F32, name)
    P.ts(t[:], sc_col[:], 1.0, None, ALU.add)
    P.tt(t[:], t[:], normw_col[:], ALU.mult)
    return t


class HT:
    def __init__(self, P, cn, ps_t):
        self.P, self.cn, self.ps_t = P, cn, ps_t
        self.junk = P.sb([128, 1024], F32, "ht_junk")
        self.ss = P.sb([128, 1], F32, "ht_ss")
        self.xs = P.sb([128, 1024], BF16, "ht_xs")

    def emit(self, xt, A_col, sh_col, dst):
        P = self.P
        ssv = self.ss[:]
        P.act(self.junk[:], xt, AF.Square, accum_out=ssv)
        P.ts(ssv, ssv, 1.0 / 1024, EPS, ALU.mult, ALU.add)
        rsqrt_inplace(P, ssv)
        P.act(self.xs[:], xt, AF.Copy, scale=ssv)
        for half in range(2):
            pt = self.ps_t[half]
            for j in range(4):
                k = half * 4 + j
                P.transpose(pt[:, j * 128:(j + 1) * 128], self.xs[:, k * 128:(k + 1) * 128], self.cn.ident_bf[:])
            for j in range(4):
                k = half * 4 + j
                P.ts(dst(k), pt[:, j * 128:(j + 1) * 128], A_col[:, k:k + 1], sh_col[:, k:k + 1], ALU.mult, ALU.add)


def load_convert(P, dst, src, stage, scale_bc=None, q="sp", eng="pool"):
    P.dma(stage, src, q=q)
    if scale_bc is None:
        P.copy(dst, stage, eng=eng)
    else:
        P.tt(dst, stage, scale_bc, ALU.mult, eng=eng)


class Loader:
    def __init__(self, P, nstage=3):
        self.P = P
        self.stg = [P.sb([128, 1024], F32, f"stg{i}") for i in range(nstage)]
        self.i = 0

    def load(self, dst, src, scale_bc=None, np_=128):
        n = dst.ap.shape[1]
        for c0 in range(0, n, 1024):
            c1 = min(n, c0 + 1024)
            st = self.stg[self.i % len(self.stg)]
            q = "sp" if self.i % 2 == 0 else "act"
            self.i += 1
            self.P.dma(st[0:np_, 0:c1 - c0], src[:, c0:c1], q=q)
            if scale_bc is None:
                self.P.copy(dst[:, c0:c1], st[0:np_, 0:c1 - c0], eng="pool")
            else:
                self.P.tt(dst[:, c0:c1], st[0:np_, 0:c1 - c0], scale_bc[:, c0:c1], ALU.mult, eng="pool")


def build_C(T_c, moe):
    nc = bass.Bass("TRN2", target_bir_lowering=False)
    P = Prog(nc)
    NBLK = T_c // 512
    NT = T_c // 128
    x_d = P.dram("x", [T_c, D], F32, "ExternalInput")
    yT_d = P.dram("yT", [2048, T_c], BF16, "ExternalInput")
    c_d = P.dram("c", [1, D], F32, "ExternalInput")
    ada_w_d = P.dram("ada_w", [D, 6 * D], F32, "ExternalInput")
    ada_b_d = P.dram("ada_b", [1, 6 * D], F32, "ExternalInput")
    nmix_d = P.dram("nmix", [1, D], F32, "ExternalInput")
    nffn_d = P.dram("nffn", [1, D], F32, "ExternalInput")
    wgate_d = P.dram("wgate", [D, 3 * D], F32, "ExternalInput")
    gateb_d = P.dram("gateb", [1, 3 * D], F32, "ExternalInput")
    proj_d = P.dram("proj", [2048, D], F32, "ExternalInput")
    wout_d = P.dram("wout", [D, D], F32, "ExternalInput")
    ssdn_d = P.dram("ssdn", [1, D], F32, "ExternalInput")
    if moe:
        NE, FF, FG = 8, 3584, 4
        routT_d = P.dram("routT", [8, D], F32, "ExternalInput")
    else:
        NE, FF, FG = 1, 2816, 2
    wgu_d = P.dram("wgu", [NE, D, 2 * FF], F32, "ExternalInput")
    wdn_d = P.dram("wdn", [NE, FF, D], F32, "ExternalInput")
    out_d = P.dram("out", [T_c, D], F32, "ExternalOutput")
    xmid_d = P.dram("xmid", [T_c, D], F32, "Internal")
    xmid_r = [Tl(xmid_d.t[i * 128:(i + 1) * 128, :], f"xmid{i}") for i in range(NT)]
    out_r = [Tl(out_d.t[i * 128:(i + 1) * 128, :], f"out{i}") for i in range(NT)]

    cn = mk_consts(P)
    pb = [P.ps([128, 512], F32, f"pb{i}") for i in range(6)]
    pt_bf = [P.ps([128, 1024], BF16, "ptA"), P.ps([128, 1024], BF16, "ptB")]
    ht = HT(P, cn, pt_bf)
    ld = Loader(P)

    gm_bc = P.sb([128, D], F32, "gm_bc")
    gf_bc = P.sb([128, D], F32, "gf_bc")
    if moe:
        Abc = P.sb([128, D], F32, "Abc")
        shbc = P.sb([128, D], F32, "shbc")
    A_m = P.sb([128, 8], F32, "A_m")
    A_f = P.sb([128, 8], F32, "A_f")
    sh_m = P.sb([128, 8], F32, "sh_m")
    sh_f = P.sb([128, 8], F32, "sh_f")
    gateb_c = load_col(P, gateb_d[0, :], 24, "gateb_c")
    ssdn_c = load_col(P, ssdn_d[0, :], 8, "ssdn_c")
    m0 = P.mark()
    cols, rows = emit_mod(P, cn, c_d, ada_w_d, ada_b_d, [0, 1, 3, 4], [2, 5] + ([3, 4] if moe else []), pb[0], pb[1])
    nmix_c = load_col(P, nmix_d[0, :], 8, "nmix_c")
    nffn_c = load_col(P, nffn_d[0, :], 8, "nffn_c")
    P.ts(A_m[:], cols[1][:], 1.0, None, ALU.add)
    P.tt(A_m[:], A_m[:], nmix_c[:], ALU.mult)
    P.ts(A_f[:], cols[4][:], 1.0, None, ALU.add)
    P.tt(A_f[:], A_f[:], nffn_c[:], ALU.mult)
    P.copy(sh_m[:], cols[0][:])
    P.copy(sh_f[:], cols[3][:])
    P.copy(gm_bc[:], rows[2][:])
    P.copy(gf_bc[:], rows[5][:])
    if moe:
        P.dma(Abc[:], nffn_d[0:1, :].bcast([128, D]))
        P.ts(rows[4][:], rows[4][:], 1.0, None, ALU.add)
        P.tt(Abc[:], Abc[:], rows[4][:], ALU.mult)
        P.copy(shbc[:], rows[3][:])
    P.release(m0)

    m1_ = P.mark()
    wg = P.sb([128, 8, 3072], BF16, "wg")
    wp = P.sb([128, 16, 1024], BF16, "wp")
    wo = P.sb([128, 8, 1024], BF16, "wo")
    for k in range(8):
        ld.load(wg[:, k, :], wgate_d[k * 128:(k + 1) * 128, :])
    for k in range(16):
        ld.load(wp[:, k, :], proj_d[k * 128:(k + 1) * 128, :])
    for k in range(8):
        ld.load(wo[:, k, :], wout_d[k * 128:(k + 1) * 128, :], scale_bc=gm_bc)

    xt = [P.sb([128, D], F32, f"xt{i}") for i in range(4)]
    hT = P.sb([128, 8, 512], BF16, "hT")
    ysb = P.sb([128, 16, 512], BF16, "ysb")
    ysq = P.sb([128, 2, 512], BF16, "ysq")
    rs = P.sb([128, 512], F32, "rs")
    sg = [P.sb([128, 512], BF16, f"sg{i}") for i in range(3)]
    tm = [P.sb([128, 512], F32, f"tm{i}") for i in range(2)]
    mT = P.sb([128, 8, 512], BF16, "mT")

    for b in range(NBLK):
        t0 = b * 512
        for i in range(4):
            P.dma(xt[i][:], x_d[t0 + i * 128:t0 + (i + 1) * 128, :], q="sp" if i % 2 == 0 else "act")
        P.dma(ysb[:], yT_d[:, t0:t0 + 512].rearrange("(k p) t -> p k t", p=128), q="pool")
        for i in range(4):
            ht.emit(xt[i][:], A_m, sh_m, lambda k, i=i: hT[:, k, i * 128:(i + 1) * 128])
        for g in range(4):
            for j in range(2):
                ch = 4 + 2 * g + j
                P.tt(ysq[:, j, :], ysb[:, ch, :], ysb[:, ch, :], ALU.mult)
            for j in range(2):
                P.matmul(pb[g % 2][:], cn.ones_bf[:], ysq[:, j, :], start=(j == 0), stop=(j == 1))
            P.ts(rs[:], pb[g % 2][:], 1.0 / 256, EPS, ALU.mult, ALU.add)
            rsqrt_inplace(P, rs[:])
            for j in range(2):
                ch = 4 + 2 * g + j
                P.stt(ysb[:, ch, :], ysb[:, ch, :], ssdn_c[:, 2 * g + j:2 * g + j + 1], rs[:], ALU.mult, ALU.mult)
        for dc in range(8):
            for br in range(3):
                for k in range(8):
                    P.matmul(pb[br][:], wg[:, k, br * 1024 + dc * 128: br * 1024 + (dc + 1) * 128], hT[:, k, :], start=(k == 0), stop=(k == 7))
                P.act(sg[br][:], pb[br][:], AF.Sigmoid, bias=gateb_c[:, br * 8 + dc: br * 8 + dc + 1])
            for br, (k0, k1) in enumerate([(0, 4), (4, 12), (12, 16)]):
                for k in range(k0, k1):
                    P.matmul(pb[3 + br][:], wp[:, k, dc * 128:(dc + 1) * 128], ysb[:, k, :], start=(k == k0), stop=(k == k1 - 1))
            P.tt(tm[0][:], sg[0][:], pb[3][:], ALU.mult)
            P.tt(tm[1][:], sg[1][:], pb[4][:], ALU.mult)
            P.tt(tm[0][:], tm[0][:], tm[1][:], ALU.add)
            P.tt(tm[1][:], sg[2][:], pb[5][:], ALU.mult)
            P.tt(mT[:, dc, :], tm[0][:], tm[1][:], ALU.add)
        for i in range(4):
            for h in range(2):
                pq = pb[(i * 2 + h) % 4]
                for k in range(8):
                    P.matmul(pq[:], mT[:, k, i * 128:(i + 1) * 128], wo[:, k, h * 512:(h + 1) * 512], start=(k == 0), stop=(k == 7))
                P.tt(xt[i][:, h * 512:(h + 1) * 512], xt[i][:, h * 512:(h + 1) * 512], pq[:], ALU.add)
            P.dma(xmid_r[b * 4 + i][:, :], xt[i][:], q="sp" if i % 2 == 0 else "act")
    P.release(m1_)

    h2T = P.sb([128, 8, T_c], BF16, "h2T")
    xall = [P.sb([128, D], F32, f"xall{i}") for i in range(NT)]
    if moe:
        comb = P.sb([128, NT, 8], F32, "comb")
        m2_ = P.mark()
        routbc = [P.sb([128, D], F32, f"routbc{e}") for e in range(8)]
        for e in range(8):
            P.dma(routbc[e][:], routT_d[e:e + 1, :].bcast([128, D]), q="pool")
        h2f = P.sb([128, D], F32, "h2f")
        lg = P.sb([128, 8], F32, "lg")
        l2 = P.sb([128, 8], F32, "l2")
        mx1 = P.sb([128, 1], F32, "mx1")
        mx2 = P.sb([128, 1], F32, "mx2")
        mk1 = P.sb([128, 8], F32, "mk1")
        mk2 = P.sb([128, 8], F32, "mk2")
        w1 = P.sb([128, 1], F32, "w1")
        w2 = P.sb([128, 1], F32, "w2")
    for i in range(NT):
        xv = xall[i][:]
        P.dma(xv, xmid_r[i][:, :], q="sp" if i % 2 == 0 else "act")
        ht.emit(xv, A_f, sh_f, lambda k, i=i: h2T[:, k, i * 128:(i + 1) * 128])
        if moe:
            P.stt(h2f[:], xv, ht.ss[:], Abc[:], ALU.mult, ALU.mult)
            P.tt(h2f[:], h2f[:], shbc[:], ALU.add)
            for e in range(8):
                P.tt(ht.junk[:], h2f[:], routbc[e][:], ALU.mult)
                P.reduce(lg[:, e:e + 1], ht.junk[:], ALU.add)
            P.reduce(mx1[:], lg[:], ALU.max)
            P.ts(mk1[:], lg[:], mx1[:], None, ALU.is_equal)
            P.stt(l2[:], mk1[:], -1e30, lg[:], ALU.mult, ALU.add)
            P.reduce(mx2[:], l2[:], ALU.max)
            P.ts(mk2[:], l2[:], mx2[:], None, ALU.is_equal)
            P.tt(w1[:], mx2[:], mx1[:], ALU.subtract)
            P.act(w1[:], w1[:], AF.Exp)
            P.ts(w1[:], w1[:], 1.0, None, ALU.add)
            P.recip(w1[:], w1[:])
            P.ts(w2[:], w1[:], -1.0, 1.0, ALU.mult, ALU.add)
            P.ts(mk1[:], mk1[:], w1[:], None, ALU.mult)
            P.stt(comb[:, i, :], mk2[:], w2[:], mk1[:], ALU.mult, ALU.add)
    if moe:
        P.release(m2_)
    NCH = FF // 128
    NG = NCH // FG
    wgu_a = P.sb([128, 8, 2 * FG * 128], BF16, "wgu_a")
    wdn_a = P.sb([128, FG, 1024], BF16, "wdn_a")
    actT = P.sb([128, FG, 512], BF16, "actT")
    sl = P.sb([128, 512], F32, "sl")
    for e in range(NE):
        for gi in range(NG):
            f0 = gi * FG * 128
            for k in range(8):
                ld.load(wgu_a[:, k, 0:FG * 128], wgu_d[e, k * 128:(k + 1) * 128, f0:f0 + FG * 128])
                ld.load(wgu_a[:, k, FG * 128:2 * FG * 128], wgu_d[e, k * 128:(k + 1) * 128, FF + f0:FF + f0 + FG * 128])
            for j in range(FG):
                ld.load(wdn_a[:, j, :], wdn_d[e, f0 + j * 128:f0 + (j + 1) * 128, :], scale_bc=gf_bc)
            for b in range(NBLK):
                t0 = b * 512
                for j in range(FG):
                    for k in range(8):
                        P.matmul(pb[0][:], wgu_a[:, k, j * 128:(j + 1) * 128], h2T[:, k, t0:t0 + 512], start=(k == 0), stop=(k == 7))
                    for k in range(8):
                        P.matmul(pb[1][:], wgu_a[:, k, FG * 128 + j * 128:FG * 128 + (j + 1) * 128], h2T[:, k, t0:t0 + 512], start=(k == 0), stop=(k == 7))
                    P.act(sl[:], pb[0][:], AF.Silu)
                    P.tt(actT[:, j, :], sl[:], pb[1][:], ALU.mult)
                for i in range(4):
                    ti = b * 4 + i
                    for h in range(2):
                        pp = pb[2 + (i * 2 + h) % 4]
                        for j in range(FG):
                            P.matmul(pp[:], actT[:, j, i * 128:(i + 1) * 128], wdn_a[:, j, h * 512:(h + 1) * 512], start=(j == 0), stop=(j == FG - 1))
                        xs_ = xall[ti][:, h * 512:(h + 1) * 512]
                        if moe:
                            P.stt(xs_, pp[:], comb[:, ti, e:e + 1], xs_, ALU.mult, ALU.add)
                        else:
                            P.tt(xs_, xs_, pp[:], ALU.add)
    for i in range(NT):
        P.dma(out_r[i][:, :], xall[i][:], q="sp" if i % 2 == 0 else "act")
    print("C peak sbuf", P.peak, "ops", len(P.ops))
    return P.emit()


GN_EPS = 64e-5
C_R, C_K, C_V, C_WLO, C_ALO, C_GLO = 0, 64, 128, 192, 256, 320
C_Z, C_X, C_B, C_C = 480, 608, 736, 864
C_FQ, C_FK, C_FV = 992, 1056, 1120
C_DT, C_F = 1184, 1186
C_VD = 1187
NW0, NW1 = 1187, 1219
PP_MU = 0
PP_W0, PP_A0, PP_KK, PP_KA, PP_RK, PP_V0 = 7, 8, 9, 10, 11, 12
PP_CW = 13
PP_CB = 29
PP_QG, PP_KG = 33, 34
PP_D = 35
PP_SEL = 37
NPP = 40
BC_LNW, BC_LNB, BC_DTB, BC_ALOG, BC_FB = 0, 64, 128, 130, 132
NBC = 133
MK_BIG, MK_SL64, MK_TRI, MK_SLF, MK_CHUNK, MK_NEG, MK_SEL = 0, 1, 2, 3, 4, 5, 6
NMASK = 7


def make_masks():
    p = np.arange(128)[:, None]
    j = np.arange(128)[None, :]
    m = np.zeros((128, NMASK, 128), np.float32)
    pl, jl = p % 64, j % 64
    m[:, MK_BIG, :] = np.where(j < 64, jl > pl, jl >= pl)
    m[:, MK_SL64, :] = (j < p) & (p < 64) & (j < 64)
    m[:, MK_TRI, :] = j >= p
    m[:, MK_SLF, :] = p > j
    m[:, MK_CHUNK, :] = (j >= p) & ((j // 64) == (p // 64))
    m[:, MK_NEG, :] = np.where(p > j, -30000.0, 0.0)
    m[64, MK_SEL, :] = 1.0
    return m


import os
RW_STOP = int(os.environ.get('RW_STOP', '99'))


class RWMixer:
    def __init__(self, st):
        self.st = st
        P = st.P
        self.cur = {}
        for name, rows in [("r", 64), ("k", 64), ("v", 64), ("wlo", 64), ("alo", 64), ("gloa", 128), ("glob", 32)]:
            t = P.sb([rows, 513], F32, f"rw_{name}")
            P.memset(t[:, 0:1], 0.0)
            self.cur[name] = t
        self.S = P.sb([64, 64], F32, "rw_S")
        self.Sb = P.sb([64, 64], BF16, "rw_Sb")
        P.memset(self.S[:], 0.0)
        P.memset(self.Sb[:], 0.0)
        self.negw0 = P.sb([64, 1], F32, "rw_negw0")
        self.omka = P.sb([64, 1], F32, "rw_omka")
        pp = st.pp
        P.ts(self.negw0[:], pp[0:64, PP_W0:PP_W0 + 1], -1.0, None, ALU.mult)
        P.ts(self.omka[:], pp[0:64, PP_KA:PP_KA + 1], -1.0, 1.0, ALU.mult, ALU.add)

    def alloc(self):
        st = self.st
        P = st.P
        f = lambda n, r=64, c=512, dt=F32: P.sb([r, c], dt, "rw_" + n)
        self.tmp = f("tmp")
        self.tmp128 = f("tmp128", 128)
        self.r = f("sr")
        self.k = f("sk")
        self.v = f("sv")
        self.twl = f("twl", dt=BF16)
        self.alo = f("salo", dt=BF16)
        self.sga = f("sga", 128, dt=BF16)
        self.sgb = f("sgb", 32, dt=BF16)
        self.logw = f("logw")
        self.a = f("a")
        self.kk = f("kk")
        self.sq = f("sq", dt=BF16)
        self.k2 = f("k2")
        self.bv = f("bv")
        self.rk = f("rk", dt=BF16)
        self.cl = f("cl")
        self.G = f("G")
        self.E2 = f("E2")
        self.lwT = P.sb([128, 64], F32, "rw_lwT")
        self.AR = P.sb([64, 8, 128], BF16, "rw_AR")
        self.BK = P.sb([64, 8, 128], BF16, "rw_BK")
        self.BKh = P.sb([64, 8, 128], BF16, "rw_BKh")
        self.VV = P.sb([64, 8, 64], BF16, "rw_VV")
        self.MBb = P.sb([64, 8, 64], BF16, "rw_MBb")
        self.MKb = P.sb([64, 8, 128], BF16, "rw_MKb")
        self.Pm = [P.sb([64, 8, 128], F32, f"rw_Pm{i}") for i in range(2)]
        self.TT = P.sb([64, 8, 128], F32, "rw_TT")
        self.Utok = P.sb([64, 8, 64], BF16, "rw_Utok")
        self.Vtok = P.sb([64, 8, 64], BF16, "rw_Vtok")
        self.BKhT = P.sb([64, 8, 128], BF16, "rw_BKhT")
        self.Wf = P.sb([64, 64], F32, "rw_Wf")
        self.yc = P.sb([64, 8, 64], F32, "rw_yc")
        self.yq = P.sb([64, 8, 64], F32, "rw_yq")
        self.st8 = [P.sb([64, 8], F32, f"rw_st{i}") for i in range(3)]
        self.yo = P.sb([64, 8, 64], BF16, "rw_yo")
        self.yTs = P.sb([64, 512], BF16, "rw_yTs")
        if st.layer == 1:
            self.hv = f("hv", 32, dt=BF16)
            self.vf = f("vf")

    def block(self, sb):
        st = self.st
        P, pb, pp, mk, cn = st.P, st.pb, st.pp, st.mk, st.cn
        t0 = sb * 512
        cur = self.cur
        mrk = P.mark()
        self.alloc()
        groups = [("r", C_R, 64), ("k", C_K, 64), ("v", C_V, 64), ("wlo", C_WLO, 64), ("alo", C_ALO, 64),
                  ("gloa", C_GLO, 128), ("glob", C_GLO + 128, 32)]
        for gi, (name, c0, n) in enumerate(groups):
            st.project(c0, n, cur[name][:, 1:513], pb[gi % 2], evac="act" if gi % 2 == 0 else "dve")
        if st.layer == 1:
            st.project(C_VD, 32, self.hv[:], pb[1], evac="act")
            P.dma(self.vf[:], st.vf_d[:, t0:t0 + 512], q="pool")

        def shift(name, mucol, rows, out, func=None):
            c = cur[name]
            tmpv = self.tmp128[0:rows, :]
            P.tt(tmpv, c[:, 0:512], c[:, 1:513], ALU.subtract)
            if func is None:
                P.stt(out, tmpv, pp[0:rows, mucol:mucol + 1], c[:, 1:513], ALU.mult, ALU.add)
            else:
                P.stt(tmpv, tmpv, pp[0:rows, mucol:mucol + 1], c[:, 1:513], ALU.mult, ALU.add)
                P.act(out, tmpv, func)
            P.copy(c[:, 0:1], c[:, 512:513], eng="pool")

        shift("r", PP_MU + 0, 64, self.r[:])
        shift("k", PP_MU + 1, 64, self.k[:])
        shift("v", PP_MU + 2, 64, self.v[:])
        shift("wlo", PP_MU + 3, 64, self.twl[:], AF.Tanh)
        shift("alo", PP_MU + 4, 64, self.alo[:], AF.Copy)
        shift("gloa", PP_MU + 5, 128, self.sga[:], AF.Sigmoid)
        shift("glob", PP_MU + 6, 32, self.sgb[:], AF.Sigmoid)
        if RW_STOP == 1:
            P.release(mrk)
            return
        P.matmul(pb[0][0:64, :], st.wup[:, :], self.twl[:], start=True, stop=True)
        P.act(self.logw[:], pb[0][0:64, :], AF.Exp, scale=-1.0, bias=self.negw0[:])
        P.ts(self.logw[:], self.logw[:], 1.0, None, ALU.add)
        P.recip(self.logw[:], self.logw[:])
        P.ts(self.logw[:], self.logw[:], -float(np.exp(-0.5)), None, ALU.mult)
        P.matmul(pb[1][0:64, :], st.aup[:, :], self.alo[:], start=True, stop=True)
        P.act(self.a[:], pb[1][0:64, :], AF.Sigmoid, bias=pp[0:64, PP_A0:PP_A0 + 1])
        if st.layer == 1:
            P.matmul(pb[0][0:64, :], st.vup[:, :], self.hv[:], start=True, stop=True)
            P.act(self.tmp[:], pb[0][0:64, :], AF.Sigmoid, bias=pp[0:64, PP_V0:PP_V0 + 1])
            P.tt(self.vf[:], self.vf[:], self.v[:], ALU.subtract)
            P.tt(self.vf[:], self.vf[:], self.tmp[:], ALU.mult)
            P.tt(self.v[:], self.v[:], self.vf[:], ALU.add)
        else:
            P.dma(st.vfo_d[:, t0:t0 + 512], self.v[:], q="pool")
        P.ts(self.kk[:], self.k[:], pp[0:64, PP_KK:PP_KK + 1], None, ALU.mult)
        P.tt(self.sq[:], self.kk[:], self.kk[:], ALU.mult)
        P.matmul(pb[1][0:64, :], cn.ones_bf[0:64, 0:64], self.sq[:], start=True, stop=True)
        P.ts(self.tmp[:], pb[1][0:64, :], 1e-24, None, ALU.add)
        rsqrt_inplace(P, self.tmp[:])
        P.tt(self.kk[:], self.kk[:], self.tmp[:], ALU.mult)
        P.ts(self.tmp[:], self.a[:], pp[0:64, PP_KA:PP_KA + 1], self.omka[:], ALU.mult, ALU.add)
        P.tt(self.k2[:], self.k[:], self.tmp[:], ALU.mult)
        P.tt(self.bv[:], self.kk[:], self.a[:], ALU.mult)
        P.stt(self.rk[:], self.r[:], pp[0:64, PP_RK:PP_RK + 1], self.k2[:], ALU.mult, ALU.mult)
        if RW_STOP == 2:
            P.release(mrk)
            return
        for j in range(4):
            P.transpose(pb[0][:, 0:64], self.logw[:, j * 128:(j + 1) * 128], cn.ident_f[0:64, 0:64])
            P.copy(self.lwT[:], pb[0][:, 0:64])
            P.matmul(pb[1][0:64, j * 128:(j + 1) * 128], self.lwT[:], mk[:, MK_CHUNK, :], start=True, stop=True)
        P.copy(self.cl[:], pb[1][0:64, :])
        if RW_STOP == 3:
            P.release(mrk)
            return
        P.act(self.G[:], self.cl[:], AF.Exp)
        P.tt(self.AR[:, :, 64:128], self.r[:].c3(), self.G[:].c3(), ALU.mult)
        P.tt(self.tmp[:], self.cl[:], self.logw[:], ALU.subtract)
        P.act(self.E2[:], self.tmp[:], AF.Exp)
        P.stt(self.AR[:, :, 0:64], self.kk[:].c3(), -1.0, self.E2[:].c3(), ALU.mult, ALU.mult)
        P.act(self.E2[:], self.cl[:], AF.Exp, scale=-1.0)
        P.tt(self.BK[:, :, 0:64], self.bv[:].c3(), self.E2[:].c3(), ALU.mult)
        P.tt(self.BK[:, :, 64:128], self.k2[:].c3(), self.E2[:].c3(), ALU.mult)
        for c in range(8):
            P.act(self.E2[:, c * 64:(c + 1) * 64], self.cl[:, c * 64:(c + 1) * 64], AF.Exp, scale=-1.0, bias=self.cl[:, c * 64 + 63:c * 64 + 64])
        P.tt(self.BKh[:, :, 0:64], self.bv[:].c3(), self.E2[:].c3(), ALU.mult)
        P.tt(self.BKh[:, :, 64:128], self.k2[:].c3(), self.E2[:].c3(), ALU.mult)
        P.copy(self.VV[:, :, 0:64], self.v[:].c3(), eng="pool")
        if RW_STOP == 4:
            P.release(mrk)
            return
        for c in range(8):
            o = (c % 4) * 128
            P.matmul(pb[2 + c // 4][0:64, o:o + 128], self.BK[:, c, 0:64], self.AR[:, c, :], start=True, stop=True)
        for c in range(8):
            o = (c % 4) * 128
            P.matmul(pb[4 + c // 4][0:64, o:o + 128], self.BK[:, c, 64:128], self.AR[:, c, :], start=True, stop=True)
        Pm, TT = self.Pm, self.TT
        mbig = mk[0:64, MK_BIG, :]
        for hb in range(2):
            src = pb[2 + hb][0:64, :].c3(128)
            P.tt(Pm[0][:, hb * 4:(hb + 1) * 4, 0:64], src[:, :, 0:64], mbig[:, 0:64].bmid(4), ALU.mult)
            P.tt(self.MBb[:, hb * 4:(hb + 1) * 4, :], src[:, :, 64:128], mbig[:, 64:128].bmid(4), ALU.mult)
            P.tt(self.MKb[:, hb * 4:(hb + 1) * 4, :], pb[4 + hb][0:64, :].c3(128), mbig.bmid(4), ALU.mult)
        for c in range(8):
            P.matmul(pb[2][0:64, c * 64:(c + 1) * 64], self.AR[:, c, 0:64], self.BK[:, c, 0:64], start=True, stop=True)
        P.tt(Pm[0][:, :, 64:128], pb[2][0:64, :].c3(), mk[0:64, MK_SL64, 0:64].bmid(8), ALU.mult)
        for hf in range(2):
            P.tt(TT[:, :, hf * 64:(hf + 1) * 64], Pm[0][:, :, hf * 64:(hf + 1) * 64], cn.ident_f[0:64, 0:64].bmid(8), ALU.add)
        if RW_STOP == 5:
            P.release(mrk)
            return
        for c in range(8):
            P.transpose(st.pt_bf[0][0:64, c * 64:(c + 1) * 64], self.VV[:, c, 0:64], cn.ident_bf[0:64, 0:64])
        P.copy(self.Vtok[:], st.pt_bf[0][0:64, 0:512].c3())
        for c in range(8):
            for hf in range(2):
                P.transpose(st.pt_bf[1][0:64, c * 128 + hf * 64:c * 128 + (hf + 1) * 64], self.BKh[:, c, hf * 64:(hf + 1) * 64], cn.ident_bf[0:64, 0:64])
        P.copy(self.BKhT[:], st.pt_bf[1][0:64, :].c3(128))
        if RW_STOP == 6:
            P.release(mrk)
            return
        cu = 0
        for rnd in range(5):
            last = rnd == 4
            Pc, Pn = Pm[cu], Pm[1 - cu]
            for c in range(8):
                pbk = pb[2 + c // 4]
                o = (c % 4) * 128
                P.matmul(pbk[0:64, o:o + 64], Pc[:, c, 64:128], Pc[:, c, 0:64], start=True, stop=True)
                if not last:
                    P.matmul(pbk[0:64, o + 64:o + 128], Pc[:, c, 0:64], Pc[:, c, 64:128], start=True, stop=True)
            for hb in range(2):
                if last:
                    P.copy(Pn[:, hb * 4:(hb + 1) * 4, 0:64], pb[2 + hb][0:64, :].c3(128)[:, :, 0:64])
                else:
                    P.copy(Pn[:, hb * 4:(hb + 1) * 4, :], pb[2 + hb][0:64, :].c3(128))
            for c in range(8):
                pbk = pb[4 + c // 4]
                o = (c % 4) * 128
                P.matmul(pbk[0:64, o:o + 64], TT[:, c, 64:128], Pn[:, c, 0:64], start=True, stop=True)
                if not last:
                    P.matmul(pbk[0:64, o + 64:o + 128], TT[:, c, 0:64], Pn[:, c, 64:128], start=True, stop=True)
            for hb in range(2):
                if last:
                    P.tt(TT[:, hb * 4:(hb + 1) * 4, 0:64], TT[:, hb * 4:(hb + 1) * 4, 0:64], pb[4 + hb][0:64, :].c3(128)[:, :, 0:64], ALU.add)
                else:
                    P.tt(TT[:, hb * 4:(hb + 1) * 4, :], TT[:, hb * 4:(hb + 1) * 4, :], pb[4 + hb][0:64, :].c3(128), ALU.add)
            cu = 1 - cu
        if RW_STOP == 7:
            P.release(mrk)
            return
        pw = pb[1]
        for c in range(8):
            P.matmul(pw[0:64, 0:64], self.AR[:, c, 0:64], self.Sb[:, :], start=True, stop=False)
            P.matmul(pw[0:64, 0:64], self.MKb[:, c, 0:64], self.Vtok[:, c, :], start=False, stop=True)
            P.copy(self.Wf[:], pw[0:64, 0:64])
            P.matmul(pw[0:64, 64:128], TT[:, c, 0:64], self.Wf[:], start=True, stop=True)
            P.copy(self.Utok[:, c, :], pw[0:64, 64:128], eng="act")
            yreg = pb[0][0:64, c * 64:(c + 1) * 64]
            P.matmul(yreg, self.AR[:, c, 64:128], self.Sb[:, :], start=True, stop=False)
            P.matmul(yreg, self.MBb[:, c, :], self.Utok[:, c, :], start=False, stop=False)
            P.matmul(yreg, self.MKb[:, c, 64:128], self.Vtok[:, c, :], start=False, stop=True)
            P.matmul(pw[0:64, 128:192], self.BKhT[:, c, 0:64], self.Utok[:, c, :], start=True, stop=False)
            P.matmul(pw[0:64, 128:192], self.BKhT[:, c, 64:128], self.Vtok[:, c, :], start=False, stop=True)
            P.stt(self.S[:], self.S[:], self.G[:, c * 64 + 63:c * 64 + 64], pw[0:64, 128:192], ALU.mult, ALU.add)
            P.copy(self.Sb[:], self.S[:], eng="act")
        if RW_STOP == 8:
            P.release(mrk)
            return
        yall = pb[0][0:64, :].c3()
        s1, s2, bon = self.st8
        P.reduce(s1[:], yall, ALU.add)
        P.ts(s1[:], s1[:], -1.0 / 64, None, ALU.mult)
        P.tt(self.yc[:], yall, s1[:].binner(64), ALU.add)
        P.tt(self.yq[:], self.yc[:], self.yc[:], ALU.mult)
        P.reduce(s2[:], self.yq[:], ALU.add)
        P.ts(s2[:], s2[:], 1.0 / 64, GN_EPS, ALU.mult, ALU.add)
        rsqrt_inplace(P, s2[:])
        P.tt(self.yc[:], self.yc[:], s2[:].binner(64), ALU.mult)
        P.tt(self.yc[:], self.yc[:], st.pbc[0:64, BC_LNW:BC_LNW + 64].bmid(8), ALU.mult)
        P.tt(self.yc[:], self.yc[:], st.pbc[0:64, BC_LNB:BC_LNB + 64].bmid(8), ALU.add)
        for c in range(8):
            P.matmul(pb[1][0:64, 256 + c:257 + c], self.rk[:, c * 64:(c + 1) * 64], cn.ones_bf[0:64, 0:1], start=True, stop=True)
        P.copy(bon[:], pb[1][0:64, 256:264])
        P.tt(self.yq[:], self.Vtok[:], bon[:].binner(64), ALU.mult)
        P.tt(self.yc[:], self.yc[:], self.yq[:], ALU.add)
        for c in range(8):
            P.matmul(pb[4][0:64, c * 64:(c + 1) * 64], self.sga[:, c * 64:(c + 1) * 64], st.gupa[:, :], start=True, stop=False)
            P.matmul(pb[4][0:64, c * 64:(c + 1) * 64], self.sgb[:, c * 64:(c + 1) * 64], st.gupb[:, :], start=False, stop=True)
        P.tt(self.yo[:], self.yc[:], pb[4][0:64, :].c3(), ALU.mult)
        for c in range(8):
            P.transpose(st.pt_bf[0][0:64, c * 64:(c + 1) * 64], self.yo[:, c, :], cn.ident_bf[0:64, 0:64])
        P.copy(self.yTs[:], st.pt_bf[0][0:64, 0:512])
        P.dma(st.yT_d[0:64, t0:t0 + 512], self.yTs[:], q="pool")
        P.release(mrk)


class SSDMixer:
    def __init__(self, st):
        self.st = st
        P = st.P
        self.xh = [P.sb([64, 515], F32, f"ssd_x{e}") for e in range(2)]
        self.Bh = P.sb([128, 515], F32, "ssd_B")
        self.Ch = P.sb([128, 515], F32, "ssd_C")
        for t in self.xh + [self.Bh, self.Ch]:
            P.memset(t[:, 0:3], 0.0)
        self.S = [P.sb([128, 64], F32, f"ssd_S{e}") for e in range(2)]
        self.Sb = [P.sb([128, 64], BF16, f"ssd_Sb{e}") for e in range(2)]
        for t in self.S + self.Sb:
            P.memset(t[:], 0.0)
        self.Arow = P.sb([128, 2], F32, "ssd_Arow")
        P.act(self.Arow[:], st.pbc[:, BC_ALOG:BC_ALOG + 2], AF.Exp)
        P.ts(self.Arow[:], self.Arow[:], -1.0, None, ALU.mult)

    def block(self, sb):
        st = self.st
        P, pb, pp, mk, cn = st.P, st.pb, st.pp, st.mk, st.cn
        t0 = sb * 512
        mrk = P.mark()
        f = lambda n, r=128, c=512, dt=F32: P.sb([r, c], dt, "ssd_" + n)
        z = [f(f"z{e}", 64) for e in range(2)]
        acc = f("acc")
        xs = [f(f"xs{e}", 64) for e in range(2)]
        xsb = [f(f"xsb{e}", 64, dt=BF16) for e in range(2)]
        Bc = f("Bc", dt=BF16)
        Cc = f("Cc")
        dtt = P.sb([128, 4, 4], F32, "ssd_dtt")
        at = P.sb([128, 4, 2], F32, "ssd_at")
        E = f("E", c=512)
        l1 = [f(f"l1{e}", c=128) for e in range(2)]
        l2 = [f(f"l2{e}", c=128) for e in range(2)]
        CBm = f("CBm", c=128)
        Gt = [f(f"Gt{e}", c=128, dt=BF16) for e in range(2)]
        Cst = [f(f"Cst{e}", c=128, dt=BF16) for e in range(2)]
        dq = P.sb([128, 4], F32, "ssd_dq")
        xdt = [P.sb([128, 64], BF16, f"ssd_xdt{e}") for e in range(2)]
        Bw = [f(f"Bw{e}", c=128, dt=BF16) for e in range(2)]
        yo = [f(f"yo{e}", 64, dt=BF16) for e in range(2)]
        yv = f("yv", 64)
        for e in range(2):
            st.project(C_Z + e * 64, 64, z[e][:], pb[e], evac="act")
        for e in range(2):
            st.project(C_X + e * 64, 64, self.xh[e][:, 3:515], pb[e], evac="dve")
        st.project(C_B, 128, self.Bh[:, 3:515], pb[0], evac="act")
        st.project(C_C, 128, self.Ch[:, 3:515], pb[1], evac="dve")
        for j in range(4):
            for k in range(8):
                P.matmul(pb[2][:, j * 4:j * 4 + 3], st.hT[:, k, j * 128:(j + 1) * 128], st.wB[:, k, C_DT:C_DT + 3], start=(k == 0), stop=(k == 7))
        P.copy(dtt[:, :, 0:3], pb[2][:, 0:16].c3(4)[:, :, 0:3])
        st.fraw = dtt
        P.tt(at[:], dtt[:, :, 0:2], st.pbc[:, BC_DTB:BC_DTB + 2].bmid(4), ALU.add)
        P.act(at[:], at[:], AF.Exp)
        P.act(dtt[:, :, 0:2], at[:], AF.Ln, bias=1.0)
        P.tt(at[:], dtt[:, :, 0:2], self.Arow[:].bmid(4), ALU.mult)
        def conv(tile, rows, wc, bc, out, out2=None):
            a_ = acc[0:rows, :]
            P.ts(a_, tile[:, 0:512], pp[0:rows, wc:wc + 1], None, ALU.mult)
            for k in range(1, 4):
                P.stt(a_, tile[:, k:k + 512], pp[0:rows, wc + k:wc + k + 1], a_, ALU.mult, ALU.add)
            P.act(out, a_, AF.Silu, bias=pp[0:rows, bc:bc + 1])
            if out2 is not None:
                P.copy(out2, out, eng="pool")
            P.copy(tile[:, 0:3], tile[:, 512:515], eng="pool")
        for e in range(2):
            conv(self.xh[e], 64, PP_CW + 4 * e, PP_CB + e, xs[e][:], xsb[e][:])
        conv(self.Bh, 128, PP_CW + 8, PP_CB + 2, Bc[:])
        conv(self.Ch, 128, PP_CW + 12, PP_CB + 3, Cc[:])
        Ccb = f("Ccb", dt=BF16)
        P.copy(Ccb[:], Cc[:], eng="pool")
        for j in range(4):
            cs = slice(j * 128, (j + 1) * 128)
            P.matmul(pb[2][:, 0:128], Bc[:, cs], Ccb[:, cs], start=True, stop=True)
            P.tt(CBm[:], pb[2][:, 0:128], mk[:, MK_TRI, :], ALU.mult)
            P.transpose(st.pt_bf[1][:, 0:128], Bc[:, cs], cn.ident_bf[:])
            for e in range(2):
                acol = at[:, j, e:e + 1]
                P.ts(l1[e][:], mk[:, MK_SLF, :], acol, None, ALU.mult)
                P.ts(l2[e][:], cn.ones_f[:], acol, None, ALU.mult)
                P.matmul(pb[3][:, e * 256:e * 256 + 128], l1[e][:], mk[:, MK_TRI, :], start=True, stop=True)
                P.matmul(pb[3][:, e * 256 + 128:e * 256 + 256], l2[e][:], mk[:, MK_TRI, :], start=True, stop=True)
                P.matmul(pb[2][:, 128 + 2 * e:129 + 2 * e], mk[:, MK_SLF, :], acol, start=True, stop=True)
                P.matmul(pb[2][:, 129 + 2 * e:130 + 2 * e], cn.ones_f[:], acol, start=True, stop=True)
            P.act(E[:], pb[3][:, :], AF.Exp)
            P.act(dq[:], pb[2][:, 128:132], AF.Exp)
            for e in range(2):
                P.tt(Gt[e][:], E[:, e * 256:e * 256 + 128], CBm[:], ALU.mult)
                P.tt(Cst[e][:], E[:, e * 256 + 128:e * 256 + 256], Cc[:, cs], ALU.mult)
                P.transpose(st.pt_bf[0][:, e * 64:(e + 1) * 64], xsb[e][:, cs], cn.ident_bf[0:64, 0:64])
                P.ts(xdt[e][:], st.pt_bf[0][:, e * 64:(e + 1) * 64], dtt[:, j, e:e + 1], None, ALU.mult)
                P.ts(Bw[e][:], st.pt_bf[1][:, 0:128], dq[:, 2 * e:2 * e + 1], None, ALU.mult)
                yreg = pb[4 + e][0:64, cs]
                P.matmul(yreg, xdt[e][:], Gt[e][:], start=True, stop=False)
                P.matmul(yreg, self.Sb[e][:], Cst[e][:], start=False, stop=True)
                sreg = pb[2][:, 256 + e * 64:256 + (e + 1) * 64]
                P.matmul(sreg, Bw[e][:], xdt[e][:], start=True, stop=True)
                P.stt(self.S[e][:], self.S[e][:], dq[:, 2 * e + 1:2 * e + 2], sreg, ALU.mult, ALU.add)
                P.copy(self.Sb[e][:], self.S[e][:], eng="act")
        for e in range(2):
            P.act(z[e][:], z[e][:], AF.Silu)
            P.stt(yv[:], xs[e][:], pp[0:64, PP_D + e:PP_D + e + 1], pb[4 + e][0:64, :], ALU.mult, ALU.add)
            P.tt(yo[e][:], yv[:], z[e][:], ALU.mult)
            P.dma(st.yT_d[64 + e * 64:64 + (e + 1) * 64, t0:t0 + 512], yo[e][:], q="pool")
        P.release(mrk)


class FoxMixer:
    def __init__(self, st):
        self.st = st
        P = st.P
        L = st.L
        NB = L // 128
        self.KT = P.sb([67, L], BF16, "fox_KT")
        P.memset(self.KT[64:67, :], 1.0)
        self.Vaug = P.sb([128, NB, 65], BF16, "fox_V")
        P.memset(self.Vaug[:, :, 64:65], 1.0)
        self.negF = P.sb([128, NB], F32, "fox_negF")
        self.totb = P.sb([128, 1], F32, "fox_totb")
        P.memset(self.totb[:], 0.0)
        self.negfb = P.sb([128, 1], F32, "fox_negfb")
        P.ts(self.negfb[:], st.pbc[:, BC_FB:BC_FB + 1], -1.0, None, ALU.mult)
        self.qg = P.sb([64, 1], F32, "fox_qg")
        P.ts(self.qg[:], st.pp[0:64, PP_QG:PP_QG + 1], 0.125, None, ALU.mult)
        self.nmb = P.sb([128, 128], BF16, "fox_nmb")
        P.copy(self.nmb[:], st.mk[:, MK_NEG, :])
        self.nlw = P.sb([128, 4, 67], F32, "fox_nlw")
        P.memset(self.nlw[:], 0.0)
        self.pT = [P.sb([128, 512], BF16, f"fox_pT{i}") for i in range(4)]

    def block(self, sb):
        st = self.st
        P, pb, pp, mk, cn = st.P, st.pb, st.pp, st.mk, st.cn
        t0 = sb * 512
        mrk = P.mark()
        f = lambda n, r=64, c=512, dt=F32: P.sb([r, c], dt, "fox_" + n)
        q, k, v = f("q"), f("k"), f("v")
        sq = f("sq", dt=BF16)
        rs = f("rs")
        vb = f("vb", dt=BF16)
        QT = f("QT", 67, dt=BF16)
        nl = P.sb([128, 4], F32, "fox_nl")
        totc = P.sb([128, 4], F32, "fox_totc")
        tt4 = P.sb([128, 8], F32, "fox_tt4")
        Fv = f("Fv", 67)
        r1 = f("r1", 67)
        hf = f("hf", 67)
        hb = f("hb", 67, dt=BF16)
        ta = f("ta", 67)
        o = f("o", 65)
        rec = f("rec")
        yo = f("yo", dt=BF16)
        st.project(C_FQ, 64, q[:], pb[2], evac="act")
        st.project(C_FK, 64, k[:], pb[3], evac="dve")
        st.project(C_FV, 64, v[:], pb[2], evac="act")
        for j in range(4):
            for kk in range(8):
                P.matmul(pb[3][:, j:j + 1], st.hT[:, kk, j * 128:(j + 1) * 128], st.wB[:, kk, C_F:C_F + 1], start=(kk == 0), stop=(kk == 7))
        P.act(nl[:], pb[3][:, 0:4], AF.Exp, scale=-1.0, bias=self.negfb[:])
        P.act(nl[:], nl[:], AF.Ln, bias=1.0)
        P.matmul(pb[3][:, 8:12], cn.ones_f[:], nl[:], start=True, stop=True)
        P.matmul(pb[3][:, 12:16], mk[:, MK_TRI, :], nl[:], start=True, stop=True)
        P.copy(tt4[:, 0:8], pb[3][:, 8:16])
        P.copy(totc[:, 0:1], self.totb[:])
        for j in range(1, 4):
            P.tt(totc[:, j:j + 1], totc[:, j - 1:j], tt4[:, j - 1:j], ALU.add)
        P.tt(self.totb[:], totc[:, 3:4], tt4[:, 3:4], ALU.add)
        P.tt(self.negF[:, 4 * sb:4 * sb + 4], tt4[:, 4:8], totc[:], ALU.add)
        for j in range(4):
            P.copy(self.nlw[:, j, 64:67], nl[:, j:j + 1].bcast([128, 3]))
        for j in range(4):
            P.matmul(pb[2][0:67, j * 128:(j + 1) * 128], self.nlw[:, j, :], mk[:, MK_TRI, :], start=True, stop=True)
        for j in range(4):
            cs = slice(j * 128, (j + 1) * 128)
            P.ts(Fv[64:67, cs], pb[2][64:67, cs], totc[64:67, j:j + 1], -1.0, ALU.add, ALU.mult)
        R = slice(64, 67)
        P.copy(hb[R, :], Fv[R, :])
        P.copy(hf[R, :], hb[R, :])
        P.ts(ta[R, :], hf[R, :], pp[R, PP_SEL:PP_SEL + 1], None, ALU.mult)
        P.tt(r1[R, :], Fv[R, :], hf[R, :], ALU.subtract)
        P.copy(hb[R, :], r1[R, :])
        P.copy(hf[R, :], hb[R, :])
        P.stt(ta[R, :], hf[R, :], pp[R, PP_SEL + 1:PP_SEL + 2], ta[R, :], ALU.mult, ALU.add)
        P.tt(r1[R, :], r1[R, :], hf[R, :], ALU.subtract)
        P.copy(hb[R, :], r1[R, :])
        P.copy(hf[R, :], hb[R, :])
        P.stt(QT[R, :], hf[R, :], pp[R, PP_SEL + 2:PP_SEL + 3], ta[R, :], ALU.mult, ALU.add)
        for src, gcol, dst in ((q, self.qg[:], QT[0:64, :]), (k, pp[0:64, PP_KG:PP_KG + 1], self.KT[0:64, t0:t0 + 512])):
            P.tt(sq[:], src[:], src[:], ALU.mult)
            P.matmul(pb[3][0:64, :], cn.ones_bf[0:64, 0:64], sq[:], start=True, stop=True)
            P.ts(rs[:], pb[3][0:64, :], 1.0 / 64, EPS, ALU.mult, ALU.add)
            rsqrt_inplace(P, rs[:])
            P.stt(dst, src[:], gcol, rs[:], ALU.mult, ALU.mult)
        P.copy(vb[:], v[:], eng="pool")
        for j in range(4):
            P.transpose(st.pt_bf[0][:, j * 64:(j + 1) * 64], vb[:, j * 128:(j + 1) * 128], cn.ident_bf[0:64, 0:64])
        P.copy(self.Vaug[:, 4 * sb:4 * sb + 4, 0:64], st.pt_bf[0][:, 0:256].c3())
        po = pb[4]
        nkb = 4 * sb + 4
        NSC = 4
        AHEAD = 3

        def score(kb):
            ps = pb[kb % NSC]
            Kb = self.KT[0:67, kb * 128:(kb + 1) * 128]
            d = kb - 4 * sb
            if d < 0:
                P.matmul(ps[:, :], Kb, QT[:, :], start=True, stop=True)
            else:
                c0 = d * 128
                P.matmul(ps[:, c0:c0 + 128], Kb, QT[:, c0:c0 + 128], start=True, stop=False)
                P.matmul(ps[:, c0:c0 + 128], cn.ident_bf[:], self.nmb[:], start=False, stop=True)
                if c0 + 128 < 512:
                    P.matmul(ps[:, c0 + 128:512], Kb, QT[:, c0 + 128:512], start=True, stop=True)

        def expo(kb):
            c0 = max(kb - 4 * sb, 0) * 128
            pT = self.pT[kb % NSC]
            P.act(pT[:, c0:512], pb[kb % NSC][:, c0:512], AF.Exp, bias=self.negF[:, kb:kb + 1])
            if c0 > 0:
                P.memset(pT[:, 0:c0], 0.0)

        def pv(kb):
            P.matmul(po[0:65, :], self.Vaug[:, kb, :], self.pT[kb % NSC][:, :], start=(kb == 0), stop=(kb == nkb - 1))

        for kb in range(min(AHEAD, nkb)):
            score(kb)
        for kb in range(nkb):
            expo(kb)
            if kb + AHEAD < nkb:
                score(kb + AHEAD)
            pv(kb)
        P.copy(o[:], po[0:65, :])
        P.matmul(pb[5][0:64, :], mk[0:65, MK_SEL, 0:64], o[:], start=True, stop=True)
        P.recip(rec[:], pb[5][0:64, :])
        P.tt(yo[:], o[0:64, :], rec[:], ALU.mult)
        P.dma(st.yT_d[192:256, t0:t0 + 512], yo[:], q="pool")
        P.release(mrk)


def build_B(L, layer, parts=("rw", "ssd", "fox")):
    nc = bass.Bass("TRN2", target_bir_lowering=False)
    P = Prog(nc)
    NSB = L // 512
    NWc = NW1 if layer == 1 else NW0
    x_d = P.dram("x", [L, D], F32, "ExternalInput")
    c_d = P.dram("c", [1, D], F32, "ExternalInput")
    ada_w_d = P.dram("ada_w", [D, 6 * D], F32, "ExternalInput")
    ada_b_d = P.dram("ada_b", [1, 6 * D], F32, "ExternalInput")
    nmix_d = P.dram("nmix", [1, D], F32, "ExternalInput")
    wB_d = P.dram("wB", [D, NWc], F32, "ExternalInput")
    pp_d = P.dram("pp", [128, NPP], F32, "ExternalInput")
    pbc_d = P.dram("pbc", [1, NBC], F32, "ExternalInput")
    mask_d = P.dram("masks", [128, NMASK, 128], F32, "ExternalInput")
    wup_d = P.dram("wup", [64, 64], F32, "ExternalInput")
    aup_d = P.dram("aup", [64, 64], F32, "ExternalInput")
    gup_d = P.dram("gup", [160, 64], F32, "ExternalInput")
    if layer == 1:
        vup_d = P.dram("vup", [32, 64], F32, "ExternalInput")
        vf_d = P.dram("vfT", [64, L], F32, "ExternalInput")
    else:
        vfo_d = P.dram("vfT_out", [64, L], F32, "ExternalOutput")
    yT_d = P.dram("yT", [256, L], BF16, "ExternalOutput")

    cn = mk_consts(P)
    pb = [P.ps([128, 512], F32, f"pb{i}") for i in range(6)]
    pt_bf = [P.ps([128, 1024], BF16, "ptA"), P.ps([128, 1024], BF16, "ptB")]
    ht = HT(P, cn, pt_bf)

    pp = P.sb([128, NPP], F32, "pp")
    P.dma(pp[:], pp_d[:, :])
    pbc = P.sb([128, NBC], F32, "pbc")
    P.dma(pbc[:], pbc_d[0:1, :].bcast([128, NBC]))
    mk = P.sb([128, NMASK, 128], F32, "mk")
    P.dma(mk[:], mask_d[:, :, :], q="act")
    A_m = P.sb([128, 8], F32, "A_m")
    sh_m = P.sb([128, 8], F32, "sh_m")
    m0 = P.mark()
    cols, _ = emit_mod(P, cn, c_d, ada_w_d, ada_b_d, [0, 1], [], pb[0], pb[1])
    nmix_c = load_col(P, nmix_d[0, :], 8, "nmix_c")
    P.ts(A_m[:], cols[1][:], 1.0, None, ALU.add)
    P.tt(A_m[:], A_m[:], nmix_c[:], ALU.mult)
    P.copy(sh_m[:], cols[0][:])
    P.release(m0)
    wB = P.sb([128, 8, NWc], BF16, "wB")
    wup = P.sb([64, 64], BF16, "wup")
    aup = P.sb([64, 64], BF16, "aup")
    gupa = P.sb([128, 64], BF16, "gupa")
    gupb = P.sb([32, 64], BF16, "gupb")
    if layer == 1:
        vup = P.sb([32, 64], BF16, "vup")
    m_ld = P.mark()
    ld = Loader(P)
    for k in range(8):
        ld.load(wB[:, k, :], wB_d[k * 128:(k + 1) * 128, :])
    ld.load(wup[:, :], wup_d[:, :], np_=64)
    ld.load(aup[:, :], aup_d[:, :], np_=64)
    ld.load(gupa[:, :], gup_d[0:128, :])
    ld.load(gupb[:, :], gup_d[128:160, :], np_=32)
    if layer == 1:
        ld.load(vup[:, :], vup_d[:, :], np_=32)
    P.release(m_ld)

    xt = [P.sb([128, D], F32, f"xt{i}") for i in range(4)]
    hT = P.sb([128, 8, 512], BF16, "hT")

    def project(c0, ncol, dst, psb, evac="act"):
        for k in range(8):
            P.matmul(psb[0:ncol, :], wB[:, k, c0:c0 + ncol], hT[:, k, :], start=(k == 0), stop=(k == 7))
        if evac == "act":
            P.act(dst, psb[0:ncol, :], AF.Copy)
        else:
            P.copy(dst, psb[0:ncol, :])

    st = Ctx()
    st.P, st.cn, st.pb, st.pp, st.pbc, st.mk, st.hT, st.wB, st.project = P, cn, pb, pp, pbc, mk, hT, wB, project
    st.L, st.layer, st.NSB, st.yT_d, st.pt_bf = L, layer, NSB, yT_d, pt_bf
    st.wup, st.aup, st.gupa, st.gupb = wup, aup, gupa, gupb
    if layer == 1:
        st.vup, st.vf_d = vup, vf_d
    else:
        st.vfo_d = vfo_d
    rw = RWMixer(st) if "rw" in parts else None
    ssd = SSDMixer(st) if "ssd" in parts else None
    fox = FoxMixer(st) if "fox" in parts else None

    for sb in range(NSB):
        t0 = sb * 512
        for i in range(4):
            P.dma(xt[i][:], x_d[t0 + i * 128:t0 + (i + 1) * 128, :], q="sp" if i % 2 == 0 else "act")
        for i in range(4):
            ht.emit(xt[i][:], A_m, sh_m, lambda k, i=i: hT[:, k, i * 128:(i + 1) * 128])
        if rw:
            rw.block(sb)
        if ssd:
            ssd.block(sb)
        if fox:
            fox.block(sb)
    print("B peak sbuf", P.peak, "ops", len(P.ops))
    return P.emit()


RW_W, SSD_W, FOX_W = 512, 1024, 512
RW_COLS_, SSD_COLS_, FOX_COLS_ = 1824, 3088, 1544


def core_inputs_B(inp, l, i, x, vfT=None):
    w_in = inp['w_in'][l]
    o_rw, o_ssd, o_fox = 0, RW_COLS_, RW_COLS_ + SSD_COLS_
    hs = slice(i * 64, (i + 1) * 64)
    W, SW, FW = RW_W, SSD_W, FOX_W
    g = i // 2
    ar = np.arange
    cols = [ar(o_rw + i * 64, o_rw + (i + 1) * 64), ar(o_rw + W + i * 64, o_rw + W + (i + 1) * 64),
            ar(o_rw + 2 * W + i * 64, o_rw + 2 * W + (i + 1) * 64),
            ar(o_rw + 3 * W, o_rw + 3 * W + 64), ar(o_rw + 3 * W + 64, o_rw + 3 * W + 128), ar(o_rw + 3 * W + 128, o_rw + 3 * W + 288),
            ar(o_ssd + i * 128, o_ssd + (i + 1) * 128), ar(o_ssd + SW + i * 128, o_ssd + SW + (i + 1) * 128),
            ar(o_ssd + 2 * SW + g * 128, o_ssd + 2 * SW + (g + 1) * 128), ar(o_ssd + 2 * SW + 512 + g * 128, o_ssd + 2 * SW + 512 + (g + 1) * 128),
            ar(o_fox + i * 64, o_fox + (i + 1) * 64), ar(o_fox + FW + i * 64, o_fox + FW + (i + 1) * 64), ar(o_fox + 2 * FW + i * 64, o_fox + 2 * FW + (i + 1) * 64),
            ar(o_ssd + 2 * SW + 1024 + 2 * i, o_ssd + 2 * SW + 1024 + 2 * i + 2), np.array([o_fox + 3 * FW + i])]
    cols = np.concatenate(cols)
    wB = w_in[:, cols]
    if l == 1:
        wB = np.concatenate([wB, inp['rw_v_down'][0]], axis=1)
    pp = np.zeros((128, NPP), np.float32)
    mu = inp['rw_mu'][l]
    pp[0:64, PP_MU + 0] = mu[0 * W + i * 64:0 * W + (i + 1) * 64]
    pp[0:64, PP_MU + 1] = mu[1 * W + i * 64:1 * W + (i + 1) * 64]
    pp[0:64, PP_MU + 2] = mu[2 * W + i * 64:2 * W + (i + 1) * 64]
    pp[0:64, PP_MU + 3] = mu[3 * W:3 * W + 64]
    pp[0:64, PP_MU + 4] = mu[3 * W + 64:3 * W + 128]
    pp[0:128, PP_MU + 5] = mu[3 * W + 128:3 * W + 256]
    pp[0:32, PP_MU + 6] = mu[3 * W + 256:3 * W + 288]
    pp[0:64, PP_W0] = inp['rw_w0'][l][hs]
    pp[0:64, PP_A0] = inp['rw_a0'][l][hs]
    pp[0:64, PP_KK] = inp['rw_k_k'][l][hs]
    pp[0:64, PP_KA] = inp['rw_k_a'][l][hs]
    pp[0:64, PP_RK] = inp['rw_r_k'][l][i]
    if l == 1:
        pp[0:64, PP_V0] = inp['rw_v0'][0][hs]
    cw, cb = inp['ssd_conv_w'][l], inp['ssd_conv_b'][l]
    xs_ = slice(i * 128, (i + 1) * 128)
    bs_ = slice(SW + g * 128, SW + (g + 1) * 128)
    cs_ = slice(SW + 512 + g * 128, SW + 512 + (g + 1) * 128)
    for t in range(4):
        pp[0:64, PP_CW + t] = cw[t, xs_][0:64]
        pp[0:64, PP_CW + 4 + t] = cw[t, xs_][64:128]
        pp[:, PP_CW + 8 + t] = cw[t, bs_]
        pp[:, PP_CW + 12 + t] = cw[t, cs_]
    pp[0:64, PP_CB] = cb[xs_][0:64]
    pp[0:64, PP_CB + 1] = cb[xs_][64:128]
    pp[:, PP_CB + 2] = cb[bs_]
    pp[:, PP_CB + 3] = cb[cs_]
    pp[0:64, PP_QG] = inp['fox_q_gain'][l]
    pp[0:64, PP_KG] = inp['fox_k_gain'][l]
    pp[:, PP_D] = inp['ssd_d'][l][2 * i]
    pp[:, PP_D + 1] = inp['ssd_d'][l][2 * i + 1]
    pp[64, PP_SEL] = 1.0
    pp[65, PP_SEL + 1] = 1.0
    pp[66, PP_SEL + 2] = 1.0
    pbc = np.zeros((1, NBC), np.float32)
    pbc[0, BC_LNW:BC_LNW + 64] = inp['rw_lnx_w'][l][hs]
    pbc[0, BC_LNB:BC_LNB + 64] = inp['rw_lnx_b'][l][hs]
    pbc[0, BC_DTB:BC_DTB + 2] = inp['ssd_dt_bias'][l][2 * i:2 * i + 2]
    pbc[0, BC_ALOG:BC_ALOG + 2] = inp['ssd_a_log'][l][2 * i:2 * i + 2]
    pbc[0, BC_FB] = inp['fox_f_bias'][l][i]
    m = dict(x=x, c=inp['c'], ada_w=inp['ada_w'][l], ada_b=inp['ada_b'][l][None], nmix=inp['norm_mix'][l][None],
             wB=np.ascontiguousarray(wB), pp=pp, pbc=pbc, masks=make_masks(),
             wup=np.ascontiguousarray(inp['rw_w_up'][l][:, hs]), aup=np.ascontiguousarray(inp['rw_a_up'][l][:, hs]),
             gup=np.ascontiguousarray(inp['rw_g_up'][l][:, hs]))
    if l == 1:
        m['vup'] = np.ascontiguousarray(inp['rw_v_up'][0][:, hs])
        m['vfT'] = vfT
    return m


def common_inputs_C(inp, l):
    off_gate = RW_COLS_ + SSD_COLS_ + FOX_COLS_
    common = dict(
        c=inp['c'], ada_w=inp['ada_w'][l], ada_b=inp['ada_b'][l][None], nmix=inp['norm_mix'][l][None], nffn=inp['norm_ffn'][l][None],
        wgate=np.ascontiguousarray(inp['w_in'][l][:, off_gate:]), gateb=inp['gate_b'][l][None],
        proj=np.concatenate([inp['proj_rw'][l], inp['proj_ssd'][l], inp['proj_fox'][l]], axis=0), wout=inp['w_out'][l], ssdn=inp['ssd_norm'][l][None])
    if l % 2 == 1:
        common.update(routT=np.ascontiguousarray(inp['moe_router'][l // 2].T), wgu=inp['moe_w_gu'][l // 2], wdn=inp['moe_w_down'][l // 2])
    else:
        common.update(wgu=inp['ffn_w_gu'][l // 2][None], wdn=inp['ffn_w_down'][l // 2][None])
    return common


def assemble_yT(resB, L):
    yT = np.empty((2048, L), dtype=resB[0]['yT'].dtype)
    for i, r in enumerate(resB):
        y = r['yT']
        yT[i * 64:(i + 1) * 64] = y[0:64]
        yT[512 + i * 128:512 + (i + 1) * 128] = y[64:192]
        yT[1536 + i * 64:1536 + (i + 1) * 64] = y[192:256]
    return yT


NCORES = 8


def kernel(**inputs):
    inp = {k: np.asarray(v) for k, v in inputs.items()}
    L = inp['x'].shape[1]
    T_c = L // NCORES
    x = np.ascontiguousarray(inp['x'][0])
    cores = list(range(NCORES))
    vfT = [None] * NCORES
    for l in range(2):
        ncB = build_B(L, l)
        mapsB = [core_inputs_B(inp, l, i, x, vfT[i]) for i in cores]
        resB = run_bass_kernel_spmd(ncB, mapsB, core_ids=cores).results
        if l == 0:
            vfT = [np.ascontiguousarray(r['vfT_out']) for r in resB]
        yT = assemble_yT(resB, L)
        ncC = build_C(T_c, moe=(l % 2 == 1))
        common = common_inputs_C(inp, l)
        mapsC = []
        for j in cores:
            m = dict(common)
            m['x'] = np.ascontiguousarray(x[j * T_c:(j + 1) * T_c])
            m['yT'] = np.ascontiguousarray(yT[:, j * T_c:(j + 1) * T_c])
            mapsC.append(m)
        resC = run_bass_kernel_spmd(ncC, mapsC, core_ids=cores).results
        x = np.concatenate([r['out'] for r in resC], axis=0)
    return x[None].astype(np.float32)
```

```python
from contextlib import ExitStack
import numpy as np
import concourse.bass as bass
import concourse.mybir as mybir
from concourse.bass_utils import run_bass_kernel_spmd

F32 = mybir.dt.float32
BF16 = mybir.dt.bfloat16
ALU = mybir.AluOpType
AF = mybir.ActivationFunctionType
AX = mybir.AxisListType


class Tl:
    def __init__(self, t, name):
        self.t = t
        self.name = name
        self.last_w = None
        self.readers = []
        self.alias = None

    def __getitem__(self, key):
        return V(self, self.t[key])


class V:
    def __init__(self, tl, ap):
        self.tl = tl
        self.ap = ap

    def __getitem__(self, key):
        return V(self.tl, self.ap[key])

    def bitcast(self, dt):
        return V(self.tl, self.ap.bitcast(dt))

    def rearrange(self, s, **kw):
        return V(self.tl, self.ap.rearrange(s, **kw))

    def bcast(self, shape):
        return V(self.tl, self.ap.to_broadcast(shape))

    def bmid(self, n):
        p, m = self.ap.shape
        return V(self.tl, self.ap.unsqueeze(1).to_broadcast([p, n, m]))

    def binner(self, m):
        p, n = self.ap.shape
        return V(self.tl, self.ap.unsqueeze(2).to_broadcast([p, n, m]))

    def c3(self, t=64):
        return V(self.tl, self.ap.rearrange("p (c t) -> p c t", t=t))


class Op:
    __slots__ = ("eng", "fn", "reads", "writes", "dma", "deps", "need_inc", "cnt", "slot", "slot_prev")

    def __init__(self, eng, fn, reads, writes, dma):
        self.eng = eng
        self.fn = fn
        self.reads = reads
        self.writes = writes
        self.dma = dma
        self.deps = []
        self.need_inc = False
        self.cnt = 0
        self.slot = None
        self.slot_prev = 0


NDMA_SLOTS = 8
ARENA_BYTES = 212736
ENGS = ["pe", "act", "dve", "pool", "sp"]


class Prog:
    def __init__(self, nc):
        self.nc = nc
        self.ops = []
        self.stack = ExitStack()
        self.n_t = 0
        self.arena_t = self.stack.enter_context(nc.sbuf_tensor("arena", [128, ARENA_BYTES // 2], BF16))
        self.bump = 0
        self.live = []
        self.freed = []
        self.peak = 0

    def sb(self, shape, dt, name=None):
        self.n_t += 1
        name = name or f"t{self.n_t}"
        esz = 4 if dt == F32 else 2
        n = int(np.prod(shape[1:]))
        nbytes = n * esz
        nal = (nbytes + 63) // 64 * 64
        off = self.bump
        assert off + nal <= ARENA_BYTES, f"SBUF arena overflow allocating {name}: {off}+{nal}"
        self.bump += nal
        self.peak = max(self.peak, self.bump)
        ap = self.arena_t[0:shape[0], off // 2:(off + nbytes) // 2]
        if dt == F32:
            ap = ap.bitcast(F32)
        if len(shape) == 3:
            ap = ap.rearrange("p (a b) -> p a b", b=shape[2])
        tl = Tl(ap, name)
        al = [f for (o, e, f) in self.freed if o < off + nal and e > off]
        tl.alias = al if al else None
        self.live.append((off, off + nal, tl))
        return tl

    def mark(self):
        return (self.bump, len(self.live))

    def release(self, m):
        b, n = m
        self.freed.extend(self.live[n:])
        del self.live[n:]
        self.bump = b

    def ps(self, shape, dt, name=None):
        self.n_t += 1
        name = name or f"p{self.n_t}"
        t = self.stack.enter_context(self.nc.psum_tensor(name, list(shape), dt))
        return Tl(t, name)

    def dram(self, name, shape, dt, kind):
        t = self.nc.dram_tensor(name, list(shape), dt, kind=kind).ap()
        return Tl(t, name)

    def _add(self, eng, fn, reads, writes, dma=False):
        r = []
        for v in reads:
            if isinstance(v, V) and v.tl not in r:
                r.append(v.tl)
        w = []
        for v in writes:
            if isinstance(v, V) and v.tl not in w:
                w.append(v.tl)
        self.ops.append(Op(eng, fn, r, w, 16 if dma is True else (dma or 0)))

    @staticmethod
    def _a(v):
        return v.ap if isinstance(v, V) else v

    def dma(self, out, in_, q="sp", **kw):
        a = self._a
        self._add(q, lambda e: e.dma_start(out=a(out), in_=a(in_), **kw), [in_], [out], dma=True)

    def matmul(self, out, lhsT, rhs, start=True, stop=True, **kw):
        a = self._a
        self._add("pe", lambda e: e.matmul(a(out), a(lhsT), a(rhs), start=start, stop=stop, **kw), [lhsT, rhs], [out])

    def transpose(self, out, in_, ident):
        a = self._a
        self._add("pe", lambda e: e.transpose(a(out), a(in_), a(ident)), [in_, ident], [out])

    def act(self, out, in_, func, bias=None, scale=None, accum_out=None, eng="act"):
        a = self._a
        kw = {}
        if bias is not None:
            kw["bias"] = a(bias)
        if scale is not None:
            kw["scale"] = a(scale)
        if accum_out is not None:
            kw["accum_out"] = a(accum_out)
        self._add(eng, lambda e: e.activation(a(out), a(in_), func, **kw), [in_, bias, scale], [out, accum_out])

    def ts(self, out, in0, s1, s2, op0, op1=None, accum_out=None, eng="dve"):
        a = self._a
        kw = {}
        if accum_out is not None:
            kw["accum_out"] = a(accum_out)
        if op1 is None:
            self._add(eng, lambda e: e.tensor_single_scalar(a(out), a(in0), a(s1), op0), [in0, s1], [out])
        else:
            self._add(eng, lambda e: e.tensor_scalar(a(out), a(in0), a(s1), a(s2), op0, op1, **kw), [in0, s1, s2], [out, accum_out])

    def tt(self, out, in0, in1, op, eng="dve"):
        a = self._a
        self._add(eng, lambda e: e.tensor_tensor(a(out), a(in0), a(in1), op), [in0, in1], [out])

    def stt(self, out, in0, scalar, in1, op0, op1, accum_out=None, eng="dve"):
        a = self._a
        kw = {}
        if accum_out is not None:
            kw["accum_out"] = a(accum_out)
        self._add(eng, lambda e: e.scalar_tensor_tensor(a(out), a(in0), a(scalar), a(in1), op0, op1, **kw), [in0, scalar, in1], [out, accum_out])

    def copy(self, out, in_, eng="dve"):
        a = self._a
        if eng == "act":
            self._add(eng, lambda e: e.activation(a(out), a(in_), AF.Copy), [in_], [out])
        else:
            self._add(eng, lambda e: e.tensor_copy(a(out), a(in_)), [in_], [out])

    def memset(self, out, val, eng="pool"):
        a = self._a
        self._add(eng, lambda e: e.memset(a(out), val), [], [out])

    def recip(self, out, in_):
        a = self._a
        self._add("dve", lambda e: e.reciprocal(a(out), a(in_)), [in_], [out])

    def reduce(self, out, in_, op, axis=AX.X, eng="dve"):
        a = self._a
        self._add(eng, lambda e: e.tensor_reduce(a(out), a(in_), axis, op), [in_], [out])

    def affine_select(self, out, in_, pattern, compare_op, fill, base, channel_multiplier):
        a = self._a
        self._add("pool", lambda e: e.affine_select(out=a(out), in_=a(in_), pattern=pattern, compare_op=compare_op, fill=fill, base=base, channel_multiplier=channel_multiplier), [in_], [out])

    def generic(self, eng, fn, reads, writes, dma=False):
        self._add(eng, fn, reads, writes, dma)

    def emit(self):
        nc = self.nc
        ops = self.ops
        for i, op in enumerate(ops):
            deps = {}
            for tl in op.reads + op.writes:
                if tl.alias:
                    for par in tl.alias:
                        tl.readers.extend(par.readers)
                        if par.last_w is not None:
                            tl.readers.append(par.last_w)
                    tl.alias = None
            for tl in op.reads:
                if tl.last_w is not None:
                    deps[tl.last_w] = "raw"
            for tl in op.writes:
                if tl.last_w is not None and tl.last_w not in deps:
                    deps[tl.last_w] = "waw"
                for r in tl.readers:
                    if r not in deps:
                        deps[r] = "war"
            for p, kind in deps.items():
                if p == i:
                    continue
                po = ops[p]
                if (not po.dma) and (not op.dma) and po.eng == op.eng and op.eng == "pe":
                    continue
                op.deps.append(p)
                po.need_inc = True
            for tl in op.reads:
                tl.readers.append(i)
            for tl in op.writes:
                tl.last_w = i
                tl.readers = []
        cnt = {e: 0 for e in ENGS}
        slot_n = {e: 0 for e in ENGS}
        slot_cnt = {}
        for op in ops:
            if op.dma:
                sk = op.eng if op.dma == 16 else op.eng + "_cc"
                slot_n.setdefault(sk, 0)
                k = slot_n[sk] % NDMA_SLOTS
                slot_n[sk] += 1
                key = (sk, k)
                op.slot = key
                op.slot_prev = slot_cnt.get(key, 0)
                slot_cnt[key] = op.slot_prev + 1
                op.cnt = op.slot_prev + 1
            elif op.need_inc:
                cnt[op.eng] += 1
                op.cnt = cnt[op.eng]
        st = self.stack
        sem_e = {e: st.enter_context(nc.semaphore(f"s_{e}")) for e in ENGS}
        sem_d = {}
        for e in list(slot_n.keys()):
            if slot_n[e] > 0:
                for k in range(min(NDMA_SLOTS, slot_n[e])):
                    sem_d[(e, k)] = st.enter_context(nc.semaphore(f"d_{e}{k}"))
        block = st.enter_context(nc.Block())

        def run_engine(ename):
            def body(eng):
                waited = {}

                def wait(sem, key, val):
                    if waited.get(key, 0) >= val:
                        return
                    waited[key] = val
                    eng.wait_ge(sem, val)

                for op in ops:
                    if op.eng != ename:
                        continue
                    need = {}
                    for p in op.deps:
                        po = ops[p]
                        if po.dma:
                            key = ("d",) + po.slot
                            val = po.dma * po.cnt
                            sem = sem_d[po.slot]
                        else:
                            key = ("e", po.eng)
                            val = po.cnt
                            sem = sem_e[po.eng]
                        if need.get(key, (None, 0))[1] < val:
                            need[key] = (sem, val)
                    if op.dma and op.slot_prev > 0:
                        key = ("d",) + op.slot
                        val = op.dma * op.slot_prev
                        if need.get(key, (None, 0))[1] < val:
                            need[key] = (sem_d[op.slot], val)
                    for key, (sem, val) in need.items():
                        wait(sem, key, val)
                    ins = op.fn(eng)
                    if op.dma == 16:
                        ins.then_inc(sem_d[op.slot], 16)
                    elif op.dma:
                        ins.then_inc(sem_d[op.slot])
                    elif op.need_inc:
                        ins.then_inc(sem_e[ename], 1)
                for (e, k), sem in sem_d.items():
                    if e == ename or e == ename + "_cc":
                        wait(sem, ("d", e, k), (16 if e == ename else 1) * slot_cnt[(e, k)])
            return body

        used = set(op.eng for op in ops)
        if "pe" in used:
            block.tensor(run_engine("pe"))
        if "act" in used:
            block.scalar(run_engine("act"))
        if "dve" in used:
            block.vector(run_engine("dve"))
        if "pool" in used:
            block.gpsimd(run_engine("pool"))
        if "sp" in used:
            block.sync(run_engine("sp"))
        st.close()
        return nc


D = 1024
EPS = 1e-6


class Ctx:
    pass


def mk_consts(P):
    c = Ctx()
    c.ones_bf = P.sb([128, 128], BF16, "ones_bf")
    c.ones_f = P.sb([128, 128], F32, "ones_f")
    c.ident_bf = P.sb([128, 128], BF16, "ident_bf")
    c.ident_f = P.sb([128, 128], F32, "ident_f")
    P.memset(c.ones_bf[:], 1.0)
    P.memset(c.ones_f[:], 1.0)
    P.affine_select(c.ident_bf[:], c.ones_bf[:], [[-1, 128]], ALU.is_equal, 0.0, 0, 1)
    P.affine_select(c.ident_f[:], c.ones_f[:], [[-1, 128]], ALU.is_equal, 0.0, 0, 1)
    return c


def rsqrt_inplace(P, v, eps=None):
    if eps is not None:
        P.ts(v, v, eps, None, ALU.add)
    P.act(v, v, AF.Ln)
    P.act(v, v, AF.Exp, scale=-0.5)


def emit_mod(P, cn, c_d, ada_w_d, ada_b_d, col_blocks, row_blocks, ps_col, ps_row):
    siluc = P.sb([128, 8], F32, "siluc")
    P.dma(siluc[:], c_d[0, :].rearrange("(k p) -> p k", p=128), allow_slow_non_contiguous=True)
    P.act(siluc[:], siluc[:], AF.Silu)
    aw = P.sb([128, 8, 1024], F32, "aw_stage")
    cols, rows = {}, {}
    sbc = None
    if row_blocks:
        sbc = P.sb([128, 8, 128], F32, "siluc_bc")
        for kc in range(8):
            P.ts(sbc[:, kc, :], cn.ones_f[:], siluc[:, kc:kc + 1], None, ALU.mult)
    for j in sorted(set(col_blocks) | set(row_blocks)):
        P.dma(aw[:], ada_w_d[:, j * 1024:(j + 1) * 1024].rearrange("(kc p) n -> p kc n", p=128))
        if j in col_blocks:
            ab = P.sb([128, 8], F32, f"adab_c{j}")
            P.dma(ab[:], ada_b_d[0, j * 1024:(j + 1) * 1024].rearrange("(k p) -> p k", p=128), allow_slow_non_contiguous=True)
            for k in range(8):
                for kc in range(8):
                    P.matmul(ps_col[:, k:k + 1], aw[:, kc, k * 128:(k + 1) * 128], siluc[:, kc:kc + 1], start=(kc == 0), stop=(kc == 7))
            t = P.sb([128, 8], F32, f"modc{j}")
            P.tt(t[:], ps_col[:, 0:8], ab[:], ALU.add)
            cols[j] = t
        if j in row_blocks:
            t = P.sb([128, 1024], F32, f"modr{j}")
            P.dma(t[:], ada_b_d[0:1, j * 1024:(j + 1) * 1024].bcast([128, 1024]))
            for half in range(2):
                for kc in range(8):
                    P.matmul(ps_row[:], sbc[:, kc, :], aw[:, kc, half * 512:(half + 1) * 512], start=(kc == 0), stop=(kc == 7))
                P.tt(t[:, half * 512:(half + 1) * 512], t[:, half * 512:(half + 1) * 512], ps_row[:], ALU.add)
            rows[j] = t
    return cols, rows


def load_col(P, src_d_row, n, name):
    t = P.sb([128, n], F32, name)
    P.dma(t[:], src_d_row.rearrange("(k p) -> p k", p=128), allow_slow_non_contiguous=True)
    return t


def mk_Acol(P, normw_col, sc_col, name):
    t = P.sb([128, 8], F32, name)
    P.ts(t[:], sc_col[:], 1.0, None, ALU.add)
    P.tt(t[:], t[:], normw_col[:], ALU.mult)
    return t


class HT:
    def __init__(self, P, cn, ps_t):
        self.P, self.cn, self.ps_t = P, cn, ps_t
        self.ss = P.sb([128, 1], F32, "ht_ss")
        self.xs = P.sb([128, 1024], BF16, "ht_xs")
        self.junk = self.xs

    def emit(self, xt, A_col, sh_col, dst, banks=None):
        P = self.P
        ssv = self.ss[:]
        P.act(self.xs[:], xt, AF.Square, accum_out=ssv)
        P.ts(ssv, ssv, 1.0 / 1024, EPS, ALU.mult, ALU.add)
        rsqrt_inplace(P, ssv)
        P.act(self.xs[:], xt, AF.Copy, scale=ssv)
        for half in range(2):
            pt = (banks or self.ps_t)[half]
            for j in range(4):
                k = half * 4 + j
                P.transpose(pt[:, j * 128:(j + 1) * 128], self.xs[:, k * 128:(k + 1) * 128], self.cn.ident_bf[:])
            for j in range(4):
                k = half * 4 + j
                P.ts(dst(k), pt[:, j * 128:(j + 1) * 128], A_col[:, k:k + 1], sh_col[:, k:k + 1], ALU.mult, ALU.add)


def load_convert(P, dst, src, stage, scale_bc=None, q="sp", eng="pool"):
    P.dma(stage, src, q=q)
    if scale_bc is None:
        P.copy(dst, stage, eng=eng)
    else:
        P.tt(dst, stage, scale_bc, ALU.mult, eng=eng)


class Loader:
    def __init__(self, P, nstage=3):
        self.P = P
        self.stg = [P.sb([128, 1024], F32, f"stg{i}") for i in range(nstage)]
        self.i = 0

    def load(self, dst, src, scale_bc=None, np_=128):
        n = dst.ap.shape[1]
        for c0 in range(0, n, 1024):
            c1 = min(n, c0 + 1024)
            st = self.stg[self.i % len(self.stg)]
            q = "sp" if self.i % 2 == 0 else "act"
            self.i += 1
            self.P.dma(st[0:np_, 0:c1 - c0], src[:, c0:c1], q=q)
            if scale_bc is None:
                self.P.copy(dst[:, c0:c1], st[0:np_, 0:c1 - c0], eng="pool")
            else:
                self.P.tt(dst[:, c0:c1], st[0:np_, 0:c1 - c0], scale_bc[:, c0:c1], ALU.mult, eng="pool")


def build_C(T_c, moe):
    nc = bass.Bass("TRN2", target_bir_lowering=False)
    P = Prog(nc)
    NBLK = T_c // 512
    NT = T_c // 128
    x_d = P.dram("x", [T_c, D], F32, "ExternalInput")
    yT_d = P.dram("yT", [2048, T_c], BF16, "ExternalInput")
    c_d = P.dram("c", [1, D], F32, "ExternalInput")
    ada_w_d = P.dram("ada_w", [D, 6 * D], F32, "ExternalInput")
    ada_b_d = P.dram("ada_b", [1, 6 * D], F32, "ExternalInput")
    nmix_d = P.dram("nmix", [1, D], F32, "ExternalInput")
    nffn_d = P.dram("nffn", [1, D], F32, "ExternalInput")
    wgate_d = P.dram("wgate", [D, 3 * D], F32, "ExternalInput")
    gateb_d = P.dram("gateb", [1, 3 * D], F32, "ExternalInput")
    proj_d = P.dram("proj", [2048, D], F32, "ExternalInput")
    wout_d = P.dram("wout", [D, D], F32, "ExternalInput")
    ssdn_d = P.dram("ssdn", [1, D], F32, "ExternalInput")
    if moe:
        NE, FF, FG = 8, 3584, 4
        routT_d = P.dram("routT", [8, D], F32, "ExternalInput")
    else:
        NE, FF, FG = 1, 2816, 2
    wgu_d = P.dram("wgu", [NE, D, 2 * FF], F32, "ExternalInput")
    wdn_d = P.dram("wdn", [NE, FF, D], F32, "ExternalInput")
    out_d = P.dram("out", [T_c, D], F32, "ExternalOutput")
    xmid_d = P.dram("xmid", [T_c, D], F32, "Internal")
    xmid_r = [Tl(xmid_d.t[i * 128:(i + 1) * 128, :], f"xmid{i}") for i in range(NT)]
    out_r = [Tl(out_d.t[i * 128:(i + 1) * 128, :], f"out{i}") for i in range(NT)]

    cn = mk_consts(P)
    pb = [P.ps([128, 512], F32, f"pb{i}") for i in range(6)]
    pt_bf = [P.ps([128, 1024], BF16, "ptA"), P.ps([128, 1024], BF16, "ptB")]
    ht = HT(P, cn, pt_bf)
    ld = Loader(P)

    gm_bc = P.sb([128, D], F32, "gm_bc")
    gf_bc = P.sb([128, D], F32, "gf_bc")
    if moe:
        Abc = P.sb([128, D], F32, "Abc")
        shbc = P.sb([128, D], F32, "shbc")
    A_m = P.sb([128, 8], F32, "A_m")
    A_f = P.sb([128, 8], F32, "A_f")
    sh_m = P.sb([128, 8], F32, "sh_m")
    sh_f = P.sb([128, 8], F32, "sh_f")
    gateb_c = load_col(P, gateb_d[0, :], 24, "gateb_c")
    ssdn_c = load_col(P, ssdn_d[0, :], 8, "ssdn_c")
    m0 = P.mark()
    cols, rows = emit_mod(P, cn, c_d, ada_w_d, ada_b_d, [0, 1, 3, 4], [2, 5] + ([3, 4] if moe else []), pb[0], pb[1])
    nmix_c = load_col(P, nmix_d[0, :], 8, "nmix_c")
    nffn_c = load_col(P, nffn_d[0, :], 8, "nffn_c")
    P.ts(A_m[:], cols[1][:], 1.0, None, ALU.add)
    P.tt(A_m[:], A_m[:], nmix_c[:], ALU.mult)
    P.ts(A_f[:], cols[4][:], 1.0, None, ALU.add)
    P.tt(A_f[:], A_f[:], nffn_c[:], ALU.mult)
    P.copy(sh_m[:], cols[0][:])
    P.copy(sh_f[:], cols[3][:])
    P.copy(gm_bc[:], rows[2][:])
    P.copy(gf_bc[:], rows[5][:])
    if moe:
        P.dma(Abc[:], nffn_d[0:1, :].bcast([128, D]))
        P.ts(rows[4][:], rows[4][:], 1.0, None, ALU.add)
        P.tt(Abc[:], Abc[:], rows[4][:], ALU.mult)
        P.copy(shbc[:], rows[3][:])
    P.release(m0)

    m1_ = P.mark()
    wg = P.sb([128, 8, 3072], BF16, "wg")
    wp = P.sb([128, 16, 1024], BF16, "wp")
    wo = P.sb([128, 8, 1024], BF16, "wo")
    for k in range(8):
        ld.load(wg[:, k, :], wgate_d[k * 128:(k + 1) * 128, :])
    for k in range(16):
        ld.load(wp[:, k, :], proj_d[k * 128:(k + 1) * 128, :])
    for k in range(8):
        ld.load(wo[:, k, :], wout_d[k * 128:(k + 1) * 128, :], scale_bc=gm_bc)

    xt = [P.sb([128, D], F32, f"xt{i}") for i in range(4)]
    hT = P.sb([128, 8, 512], BF16, "hT")
    ysb = P.sb([128, 16, 512], BF16, "ysb")
    ysq = P.sb([128, 2, 512], BF16, "ysq")
    rs = P.sb([128, 512], F32, "rs")
    sg = [P.sb([128, 512], BF16, f"sg{i}") for i in range(3)]
    tm = [P.sb([128, 512], F32, f"tm{i}") for i in range(2)]
    mT = P.sb([128, 8, 512], BF16, "mT")

    for b in range(NBLK):
        t0 = b * 512
        for i in range(4):
            P.dma(xt[i][:], x_d[t0 + i * 128:t0 + (i + 1) * 128, :], q="sp" if i % 2 == 0 else "act")
        P.dma(ysb[:], yT_d[:, t0:t0 + 512].rearrange("(k p) t -> p k t", p=128), q="pool")
        for i in range(4):
            ht.emit(xt[i][:], A_m, sh_m, lambda k, i=i: hT[:, k, i * 128:(i + 1) * 128])
        for g in range(4):
            for j in range(2):
                ch = 4 + 2 * g + j
                P.tt(ysq[:, j, :], ysb[:, ch, :], ysb[:, ch, :], ALU.mult)
            for j in range(2):
                P.matmul(pb[g % 2][:], cn.ones_bf[:], ysq[:, j, :], start=(j == 0), stop=(j == 1))
            P.ts(rs[:], pb[g % 2][:], 1.0 / 256, EPS, ALU.mult, ALU.add)
            rsqrt_inplace(P, rs[:])
            for j in range(2):
                ch = 4 + 2 * g + j
                P.stt(ysb[:, ch, :], ysb[:, ch, :], ssdn_c[:, 2 * g + j:2 * g + j + 1], rs[:], ALU.mult, ALU.mult)
        for dc in range(8):
            for br in range(3):
                for k in range(8):
                    P.matmul(pb[br][:], wg[:, k, br * 1024 + dc * 128: br * 1024 + (dc + 1) * 128], hT[:, k, :], start=(k == 0), stop=(k == 7))
                P.act(sg[br][:], pb[br][:], AF.Sigmoid, bias=gateb_c[:, br * 8 + dc: br * 8 + dc + 1])
            for br, (k0, k1) in enumerate([(0, 4), (4, 12), (12, 16)]):
                for k in range(k0, k1):
                    P.matmul(pb[3 + br][:], wp[:, k, dc * 128:(dc + 1) * 128], ysb[:, k, :], start=(k == k0), stop=(k == k1 - 1))
            P.tt(tm[0][:], sg[0][:], pb[3][:], ALU.mult)
            P.tt(tm[1][:], sg[1][:], pb[4][:], ALU.mult)
            P.tt(tm[0][:], tm[0][:], tm[1][:], ALU.add)
            P.tt(tm[1][:], sg[2][:], pb[5][:], ALU.mult)
            P.tt(mT[:, dc, :], tm[0][:], tm[1][:], ALU.add)
        for i in range(4):
            for h in range(2):
                pq = pb[(i * 2 + h) % 4]
                for k in range(8):
                    P.matmul(pq[:], mT[:, k, i * 128:(i + 1) * 128], wo[:, k, h * 512:(h + 1) * 512], start=(k == 0), stop=(k == 7))
                P.tt(xt[i][:, h * 512:(h + 1) * 512], xt[i][:, h * 512:(h + 1) * 512], pq[:], ALU.add)
            P.dma(xmid_r[b * 4 + i][:, :], xt[i][:], q="sp" if i % 2 == 0 else "act")
    P.release(m1_)

    h2T = P.sb([128, 8, T_c], BF16, "h2T")
    xall = [P.sb([128, D], F32, f"xall{i}") for i in range(NT)]
    if moe:
        comb = P.sb([128, NT, 8], F32, "comb")
        m2_ = P.mark()
        routbc = [P.sb([128, D], F32, f"routbc{e}") for e in range(8)]
        for e in range(8):
            P.dma(routbc[e][:], routT_d[e:e + 1, :].bcast([128, D]), q="pool")
        h2f = P.sb([128, D], F32, "h2f")
        rjunk = P.sb([128, D], F32, "rjunk")
        lg = P.sb([128, 8], F32, "lg")
        l2 = P.sb([128, 8], F32, "l2")
        mx1 = P.sb([128, 1], F32, "mx1")
        mx2 = P.sb([128, 1], F32, "mx2")
        mk1 = P.sb([128, 8], F32, "mk1")
        mk2 = P.sb([128, 8], F32, "mk2")
        w1 = P.sb([128, 1], F32, "w1")
        w2 = P.sb([128, 1], F32, "w2")
    for i in range(NT):
        xv = xall[i][:]
        P.dma(xv, xmid_r[i][:, :], q="sp" if i % 2 == 0 else "act")
        ht.emit(xv, A_f, sh_f, lambda k, i=i: h2T[:, k, i * 128:(i + 1) * 128])
        if moe:
            P.stt(h2f[:], xv, ht.ss[:], Abc[:], ALU.mult, ALU.mult)
            P.tt(h2f[:], h2f[:], shbc[:], ALU.add)
            for e in range(8):
                P.tt(rjunk[:], h2f[:], routbc[e][:], ALU.mult)
                P.reduce(lg[:, e:e + 1], rjunk[:], ALU.add)
            P.reduce(mx1[:], lg[:], ALU.max)
            P.ts(mk1[:], lg[:], mx1[:], None, ALU.is_equal)
            P.stt(l2[:], mk1[:], -1e30, lg[:], ALU.mult, ALU.add)
            P.reduce(mx2[:], l2[:], ALU.max)
            P.ts(mk2[:], l2[:], mx2[:], None, ALU.is_equal)
            P.tt(w1[:], mx2[:], mx1[:], ALU.subtract)
            P.act(w1[:], w1[:], AF.Exp)
            P.ts(w1[:], w1[:], 1.0, None, ALU.add)
            P.recip(w1[:], w1[:])
            P.ts(w2[:], w1[:], -1.0, 1.0, ALU.mult, ALU.add)
            P.ts(mk1[:], mk1[:], w1[:], None, ALU.mult)
            P.stt(comb[:, i, :], mk2[:], w2[:], mk1[:], ALU.mult, ALU.add)
    if moe:
        P.release(m2_)
    NCH = FF // 128
    NG = NCH // FG
    wgu_a = P.sb([128, 8, 2 * FG * 128], BF16, "wgu_a")
    wdn_a = P.sb([128, FG, 1024], BF16, "wdn_a")
    actT = P.sb([128, FG, 512], BF16, "actT")
    sl = P.sb([128, 512], F32, "sl")
    for e in range(NE):
        for gi in range(NG):
            f0 = gi * FG * 128
            for k in range(8):
                ld.load(wgu_a[:, k, 0:FG * 128], wgu_d[e, k * 128:(k + 1) * 128, f0:f0 + FG * 128])
                ld.load(wgu_a[:, k, FG * 128:2 * FG * 128], wgu_d[e, k * 128:(k + 1) * 128, FF + f0:FF + f0 + FG * 128])
            for j in range(FG):
                ld.load(wdn_a[:, j, :], wdn_d[e, f0 + j * 128:f0 + (j + 1) * 128, :], scale_bc=gf_bc)
            for b in range(NBLK):
                t0 = b * 512
                for j in range(FG):
                    for k in range(8):
                        P.matmul(pb[0][:], wgu_a[:, k, j * 128:(j + 1) * 128], h2T[:, k, t0:t0 + 512], start=(k == 0), stop=(k == 7))
                    for k in range(8):
                        P.matmul(pb[1][:], wgu_a[:, k, FG * 128 + j * 128:FG * 128 + (j + 1) * 128], h2T[:, k, t0:t0 + 512], start=(k == 0), stop=(k == 7))
                    P.act(sl[:], pb[0][:], AF.Silu)
                    P.tt(actT[:, j, :], sl[:], pb[1][:], ALU.mult)
                for i in range(4):
                    ti = b * 4 + i
                    for h in range(2):
                        pp = pb[2 + (i * 2 + h) % 4]
                        for j in range(FG):
                            P.matmul(pp[:], actT[:, j, i * 128:(i + 1) * 128], wdn_a[:, j, h * 512:(h + 1) * 512], start=(j == 0), stop=(j == FG - 1))
                        xs_ = xall[ti][:, h * 512:(h + 1) * 512]
                        if moe:
                            P.stt(xs_, pp[:], comb[:, ti, e:e + 1], xs_, ALU.mult, ALU.add)
                        else:
                            P.tt(xs_, xs_, pp[:], ALU.add)
    for i in range(NT):
        P.dma(out_r[i][:, :], xall[i][:], q="sp" if i % 2 == 0 else "act")
    print("C peak sbuf", P.peak, "ops", len(P.ops))
    return P.emit()


GN_EPS = 64e-5
C_R, C_K, C_V, C_WLO, C_ALO, C_GLO = 0, 64, 128, 192, 256, 320
C_Z, C_X, C_B, C_C = 480, 608, 736, 864
C_FQ, C_FK, C_FV = 992, 1056, 1120
C_DT, C_F = 1184, 1186
C_VD = 1187
NW0, NW1 = 1187, 1219
PP_MU = 0
PP_W0, PP_A0, PP_KK, PP_KA, PP_RK, PP_V0 = 7, 8, 9, 10, 11, 12
PP_CW = 13
PP_CB = 29
PP_QG, PP_KG = 33, 34
PP_D = 35
PP_SEL = 37
NPP = 40
BC_LNW, BC_LNB, BC_DTB, BC_ALOG, BC_FB = 0, 64, 128, 130, 132
NBC = 133
MK_BIG, MK_SL64, MK_TRI, MK_SLF, MK_CHUNK, MK_NEG, MK_SEL = 0, 1, 2, 3, 4, 5, 6
NMASK = 7


def make_masks():
    p = np.arange(128)[:, None]
    j = np.arange(128)[None, :]
    m = np.zeros((128, NMASK, 128), np.float32)
    pl, jl = p % 64, j % 64
    m[:, MK_BIG, :] = np.where(j < 64, jl > pl, jl >= pl)
    m[:, MK_SL64, :] = (j < p) & (p < 64) & (j < 64)
    m[:, MK_TRI, :] = j >= p
    m[:, MK_SLF, :] = p > j
    m[:, MK_CHUNK, :] = (j >= p) & ((j // 64) == (p // 64))
    m[:, MK_NEG, :] = np.where(p > j, -30000.0, 0.0)
    m[64, MK_SEL, :] = 1.0
    return m


import os
RW_STOP = int(os.environ.get('RW_STOP', '99'))


class RWMixer:
    def __init__(self, st):
        self.st = st
        P = st.P
        self.cur = {}
        for name, rows in [("r", 64), ("k", 64), ("v", 64), ("wlo", 64), ("alo", 64), ("gloa", 128), ("glob", 32)]:
            t = P.sb([rows, 513], F32, f"rw_{name}")
            P.memset(t[:, 0:1], 0.0)
            self.cur[name] = t
        self.S = P.sb([64, 64], F32, "rw_S")
        self.Sb = P.sb([64, 64], BF16, "rw_Sb")
        P.memset(self.S[:], 0.0)
        P.memset(self.Sb[:], 0.0)
        self.negw0 = P.sb([64, 1], F32, "rw_negw0")
        self.omka = P.sb([64, 1], F32, "rw_omka")
        pp = st.pp
        P.ts(self.negw0[:], pp[0:64, PP_W0:PP_W0 + 1], -1.0, None, ALU.mult)
        P.ts(self.omka[:], pp[0:64, PP_KA:PP_KA + 1], -1.0, 1.0, ALU.mult, ALU.add)

    def alloc(self):
        st = self.st
        P = st.P
        f = lambda n, r=64, c=512, dt=F32: P.sb([r, c], dt, "rw_" + n)
        self.tmp128 = f("tmp128", 128)
        self.tmp = self.tmp128[0:64, :]
        self.r = f("sr")
        self.k = f("sk")
        self.v = f("sv")
        self.twl = f("twl", dt=BF16)
        self.alo = f("salo", dt=BF16)
        self.sga = f("sga", 128, dt=BF16)
        self.sgb = f("sgb", 32, dt=BF16)
        self.logw = f("logw")
        self.a = f("a")
        self.kk = f("kk")
        self.sq = f("sq", dt=BF16)
        self.k2 = f("k2")
        self.bv = f("bv")
        self.rk = f("rk", dt=BF16)
        self.cl = f("cl")
        self.G = f("G")
        self.E2 = f("E2")
        self.lwT = P.sb([128, 64], F32, "rw_lwT")
        self.AR = P.sb([64, 8, 128], BF16, "rw_AR")
        self.BK = P.sb([64, 8, 128], BF16, "rw_BK")
        self.BKh = P.sb([64, 8, 128], BF16, "rw_BKh")
        self.VV = P.sb([64, 8, 64], BF16, "rw_VV")
        self.MBb = P.sb([64, 8, 64], BF16, "rw_MBb")
        self.MKb = P.sb([64, 8, 128], BF16, "rw_MKb")
        self.Pm = [P.sb([64, 8, 128], F32, f"rw_Pm{i}") for i in range(2)]
        self.TT = P.sb([64, 8, 128], F32, "rw_TT")
        self.Utok = P.sb([64, 8, 64], BF16, "rw_Utok")
        self.Vtok = P.sb([64, 8, 64], BF16, "rw_Vtok")
        self.BKhT = P.sb([64, 8, 128], BF16, "rw_BKhT")
        self.Wf = P.sb([64, 64], F32, "rw_Wf")
        self.yc = P.sb([64, 8, 64], F32, "rw_yc")
        self.yq = P.sb([64, 8, 64], F32, "rw_yq")
        self.st8 = [P.sb([64, 8], F32, f"rw_st{i}") for i in range(3)]
        self.yo = P.sb([64, 8, 64], BF16, "rw_yo")
        self.yTs = P.sb([64, 512], BF16, "rw_yTs")
        if st.layer == 1:
            self.hv = self.sq[0:32, :]
            self.vf = self.E2[:, :]

    def block(self, sb, filler=None):
        st = self.st
        P, pb, pp, mk, cn = st.P, st.pb, st.pp, st.mk, st.cn
        t0 = sb * 512
        cur = self.cur
        mrk = P.mark()
        self.alloc()
        groups = [("r", C_R, 64), ("k", C_K, 64), ("v", C_V, 64), ("wlo", C_WLO, 64), ("alo", C_ALO, 64),
                  ("gloa", C_GLO, 128), ("glob", C_GLO + 128, 32)]
        for gi, (name, c0, n) in enumerate(groups):
            st.project(c0, n, cur[name][:, 1:513], pb[gi % 2], evac="act" if gi % 2 == 0 else "dve")
        if st.layer == 1:
            st.project(C_VD, 32, self.hv[:], pb[1], evac="act")
            P.dma(self.vf[:], st.vf_d[:, t0:t0 + 512], q="pool")

        def shift(name, mucol, rows, out, func=None):
            c = cur[name]
            tmpv = self.tmp128[0:rows, :]
            P.tt(tmpv, c[:, 0:512], c[:, 1:513], ALU.subtract)
            if func is None:
                P.stt(out, tmpv, pp[0:rows, mucol:mucol + 1], c[:, 1:513], ALU.mult, ALU.add)
            else:
                P.stt(tmpv, tmpv, pp[0:rows, mucol:mucol + 1], c[:, 1:513], ALU.mult, ALU.add)
                P.act(out, tmpv, func)
            P.copy(c[:, 0:1], c[:, 512:513], eng="pool")

        shift("r", PP_MU + 0, 64, self.r[:])
        shift("k", PP_MU + 1, 64, self.k[:])
        shift("v", PP_MU + 2, 64, self.v[:])
        shift("wlo", PP_MU + 3, 64, self.twl[:], AF.Tanh)
        shift("alo", PP_MU + 4, 64, self.alo[:], AF.Copy)
        shift("gloa", PP_MU + 5, 128, self.sga[:], AF.Sigmoid)
        shift("glob", PP_MU + 6, 32, self.sgb[:], AF.Sigmoid)
        if RW_STOP == 1:
            P.release(mrk)
            return
        P.matmul(pb[0][0:64, :], st.wup[:, :], self.twl[:], start=True, stop=True)
        P.act(self.logw[:], pb[0][0:64, :], AF.Exp, scale=-1.0, bias=self.negw0[:])
        P.ts(self.logw[:], self.logw[:], 1.0, None, ALU.add)
        P.recip(self.logw[:], self.logw[:])
        P.ts(self.logw[:], self.logw[:], -float(np.exp(-0.5)), None, ALU.mult)
        P.matmul(pb[1][0:64, :], st.aup[:, :], self.alo[:], start=True, stop=True)
        P.act(self.a[:], pb[1][0:64, :], AF.Sigmoid, bias=pp[0:64, PP_A0:PP_A0 + 1])
        if st.layer == 1:
            P.matmul(pb[0][0:64, :], st.vup[:, :], self.hv[:], start=True, stop=True)
            P.act(self.tmp[:], pb[0][0:64, :], AF.Sigmoid, bias=pp[0:64, PP_V0:PP_V0 + 1])
            P.tt(self.vf[:], self.vf[:], self.v[:], ALU.subtract)
            P.tt(self.vf[:], self.vf[:], self.tmp[:], ALU.mult)
            P.tt(self.v[:], self.v[:], self.vf[:], ALU.add)
        else:
            P.dma(st.vfo_d[:, t0:t0 + 512], self.v[:], q="pool")
        P.ts(self.kk[:], self.k[:], pp[0:64, PP_KK:PP_KK + 1], None, ALU.mult)
        P.tt(self.sq[:], self.kk[:], self.kk[:], ALU.mult)
        P.matmul(pb[1][0:64, :], cn.ones_bf[0:64, 0:64], self.sq[:], start=True, stop=True)
        P.ts(self.tmp[:], pb[1][0:64, :], 1e-24, None, ALU.add)
        rsqrt_inplace(P, self.tmp[:])
        P.tt(self.kk[:], self.kk[:], self.tmp[:], ALU.mult)
        P.ts(self.tmp[:], self.a[:], pp[0:64, PP_KA:PP_KA + 1], self.omka[:], ALU.mult, ALU.add)
        P.tt(self.k2[:], self.k[:], self.tmp[:], ALU.mult)
        P.tt(self.bv[:], self.kk[:], self.a[:], ALU.mult)
        P.stt(self.rk[:], self.r[:], pp[0:64, PP_RK:PP_RK + 1], self.k2[:], ALU.mult, ALU.mult)
        if RW_STOP == 2:
            P.release(mrk)
            return
        for j in range(4):
            P.transpose(pb[0][:, 0:64], self.logw[:, j * 128:(j + 1) * 128], cn.ident_f[0:64, 0:64])
            P.copy(self.lwT[:], pb[0][:, 0:64])
            P.matmul(pb[1][0:64, j * 128:(j + 1) * 128], self.lwT[:], mk[:, MK_CHUNK, :], start=True, stop=True)
        P.copy(self.cl[:], pb[1][0:64, :])
        if RW_STOP == 3:
            P.release(mrk)
            return
        P.act(self.G[:], self.cl[:], AF.Exp)
        P.tt(self.AR[:, :, 64:128], self.r[:].c3(), self.G[:].c3(), ALU.mult)
        P.tt(self.tmp[:], self.cl[:], self.logw[:], ALU.subtract)
        P.act(self.E2[:], self.tmp[:], AF.Exp)
        P.stt(self.AR[:, :, 0:64], self.kk[:].c3(), -1.0, self.E2[:].c3(), ALU.mult, ALU.mult)
        P.act(self.E2[:], self.cl[:], AF.Exp, scale=-1.0)
        P.tt(self.BK[:, :, 0:64], self.bv[:].c3(), self.E2[:].c3(), ALU.mult)
        P.tt(self.BK[:, :, 64:128], self.k2[:].c3(), self.E2[:].c3(), ALU.mult)
        for c in range(8):
            P.act(self.E2[:, c * 64:(c + 1) * 64], self.cl[:, c * 64:(c + 1) * 64], AF.Exp, scale=-1.0, bias=self.cl[:, c * 64 + 63:c * 64 + 64])
        P.tt(self.BKh[:, :, 0:64], self.bv[:].c3(), self.E2[:].c3(), ALU.mult)
        P.tt(self.BKh[:, :, 64:128], self.k2[:].c3(), self.E2[:].c3(), ALU.mult)
        P.copy(self.VV[:, :, 0:64], self.v[:].c3(), eng="pool")
        if RW_STOP == 4:
            P.release(mrk)
            return
        for c in range(8):
            o = (c % 4) * 128
            P.matmul(pb[2 + c // 4][0:64, o:o + 128], self.BK[:, c, 0:64], self.AR[:, c, :], start=True, stop=True)
        for c in range(8):
            o = (c % 4) * 128
            P.matmul(pb[4 + c // 4][0:64, o:o + 128], self.BK[:, c, 64:128], self.AR[:, c, :], start=True, stop=True)
        Pm, TT = self.Pm, self.TT
        mbig = mk[0:64, MK_BIG, :]
        for hb in range(2):
            src = pb[2 + hb][0:64, :].c3(128)
            P.tt(Pm[0][:, hb * 4:(hb + 1) * 4, 0:64], src[:, :, 0:64], mbig[:, 0:64].bmid(4), ALU.mult)
            P.tt(self.MBb[:, hb * 4:(hb + 1) * 4, :], src[:, :, 64:128], mbig[:, 64:128].bmid(4), ALU.mult)
            P.tt(self.MKb[:, hb * 4:(hb + 1) * 4, :], pb[4 + hb][0:64, :].c3(128), mbig.bmid(4), ALU.mult)
        for c in range(8):
            P.matmul(pb[2][0:64, c * 64:(c + 1) * 64], self.AR[:, c, 0:64], self.BK[:, c, 0:64], start=True, stop=True)
        P.tt(Pm[0][:, :, 64:128], pb[2][0:64, :].c3(), mk[0:64, MK_SL64, 0:64].bmid(8), ALU.mult)
        for hf in range(2):
            P.tt(TT[:, :, hf * 64:(hf + 1) * 64], Pm[0][:, :, hf * 64:(hf + 1) * 64], cn.ident_f[0:64, 0:64].bmid(8), ALU.add)
        if RW_STOP == 5:
            P.release(mrk)
            return
        for c in range(8):
            P.transpose(st.pt_bf[0][0:64, c * 64:(c + 1) * 64], self.VV[:, c, 0:64], cn.ident_bf[0:64, 0:64])
        P.copy(self.Vtok[:], st.pt_bf[0][0:64, 0:512].c3())
        for c in range(8):
            for hf in range(2):
                P.transpose(st.pt_bf[1][0:64, c * 128 + hf * 64:c * 128 + (hf + 1) * 64], self.BKh[:, c, hf * 64:(hf + 1) * 64], cn.ident_bf[0:64, 0:64])
        P.copy(self.BKhT[:], st.pt_bf[1][0:64, :].c3(128))
        if RW_STOP == 6:
            P.release(mrk)
            return
        cu = 0
        for rnd in range(5):
            last = rnd == 4
            Pc, Pn = Pm[cu], Pm[1 - cu]
            for c in range(8):
                pbk = pb[2 + c // 4]
                o = (c % 4) * 128
                P.matmul(pbk[0:64, o:o + 64], Pc[:, c, 64:128], Pc[:, c, 0:64], start=True, stop=True)
                if not last:
                    P.matmul(pbk[0:64, o + 64:o + 128], Pc[:, c, 0:64], Pc[:, c, 64:128], start=True, stop=True)
            for hb in range(2):
                if last:
                    P.copy(Pn[:, hb * 4:(hb + 1) * 4, 0:64], pb[2 + hb][0:64, :].c3(128)[:, :, 0:64])
                else:
                    P.copy(Pn[:, hb * 4:(hb + 1) * 4, :], pb[2 + hb][0:64, :].c3(128))
            for c in range(8):
                pbk = pb[4 + c // 4]
                o = (c % 4) * 128
                P.matmul(pbk[0:64, o:o + 64], TT[:, c, 64:128], Pn[:, c, 0:64], start=True, stop=True)
                if not last:
                    P.matmul(pbk[0:64, o + 64:o + 128], TT[:, c, 0:64], Pn[:, c, 64:128], start=True, stop=True)
            for hb in range(2):
                if last:
                    P.tt(TT[:, hb * 4:(hb + 1) * 4, 0:64], TT[:, hb * 4:(hb + 1) * 4, 0:64], pb[4 + hb][0:64, :].c3(128)[:, :, 0:64], ALU.add)
                else:
                    P.tt(TT[:, hb * 4:(hb + 1) * 4, :], TT[:, hb * 4:(hb + 1) * 4, :], pb[4 + hb][0:64, :].c3(128), ALU.add)
            cu = 1 - cu
        if RW_STOP == 7:
            P.release(mrk)
            return
        pw = st.pt_bf[1][:, :].bitcast(F32)
        py = st.pt_bf[0][:, :].bitcast(F32)
        fill = (lambda: filler(0.0)) if filler is not None else (lambda: None)
        for c in range(8):
            P.matmul(pw[0:64, 0:64], self.AR[:, c, 0:64], self.Sb[:, :], start=True, stop=False)
            P.matmul(pw[0:64, 0:64], self.MKb[:, c, 0:64], self.Vtok[:, c, :], start=False, stop=True)
            fill()
            P.copy(self.Wf[:], pw[0:64, 0:64])
            P.matmul(pw[0:64, 64:128], TT[:, c, 0:64], self.Wf[:], start=True, stop=True)
            fill()
            P.copy(self.Utok[:, c, :], pw[0:64, 64:128])
            yreg = py[0:64, c * 64:(c + 1) * 64]
            P.matmul(yreg, self.AR[:, c, 64:128], self.Sb[:, :], start=True, stop=False)
            P.matmul(yreg, self.MBb[:, c, :], self.Utok[:, c, :], start=False, stop=False)
            P.matmul(yreg, self.MKb[:, c, 64:128], self.Vtok[:, c, :], start=False, stop=True)
            P.matmul(pw[0:64, 128:192], self.BKhT[:, c, 0:64], self.Utok[:, c, :], start=True, stop=False)
            P.matmul(pw[0:64, 128:192], self.BKhT[:, c, 64:128], self.Vtok[:, c, :], start=False, stop=True)
            fill()
            P.stt(self.S[:], self.S[:], self.G[:, c * 64 + 63:c * 64 + 64], pw[0:64, 128:192], ALU.mult, ALU.add)
            P.copy(self.Sb[:], self.S[:], eng="pool")
        if RW_STOP == 8:
            P.release(mrk)
            return
        yall = py[0:64, :].c3()
        s1, s2, bon = self.st8
        P.reduce(s1[:], yall, ALU.add)
        P.ts(s1[:], s1[:], -1.0 / 64, None, ALU.mult)
        P.tt(self.yc[:], yall, s1[:].binner(64), ALU.add)
        P.tt(self.yq[:], self.yc[:], self.yc[:], ALU.mult)
        P.reduce(s2[:], self.yq[:], ALU.add)
        P.ts(s2[:], s2[:], 1.0 / 64, GN_EPS, ALU.mult, ALU.add)
        rsqrt_inplace(P, s2[:])
        P.tt(self.yc[:], self.yc[:], s2[:].binner(64), ALU.mult)
        P.tt(self.yc[:], self.yc[:], st.pbc[0:64, BC_LNW:BC_LNW + 64].bmid(8), ALU.mult)
        P.tt(self.yc[:], self.yc[:], st.pbc[0:64, BC_LNB:BC_LNB + 64].bmid(8), ALU.add)
        for c in range(8):
            P.matmul(pw[0:64, 256 + c:257 + c], self.rk[:, c * 64:(c + 1) * 64], cn.ones_bf[0:64, 0:1], start=True, stop=True)
        P.copy(bon[:], pw[0:64, 256:264])
        P.tt(self.yq[:], self.Vtok[:], bon[:].binner(64), ALU.mult)
        P.tt(self.yc[:], self.yc[:], self.yq[:], ALU.add)
        for c in range(8):
            P.matmul(pw[0:64, c * 64:(c + 1) * 64], self.sga[:, c * 64:(c + 1) * 64], st.gupa[:, :], start=True, stop=False)
            P.matmul(pw[0:64, c * 64:(c + 1) * 64], self.sgb[:, c * 64:(c + 1) * 64], st.gupb[:, :], start=False, stop=True)
        fill()
        P.tt(self.yo[:], self.yc[:], pw[0:64, :].c3(), ALU.mult)
        for c in range(8):
            P.transpose(st.pt_bf[0][0:64, c * 64:(c + 1) * 64], self.yo[:, c, :], cn.ident_bf[0:64, 0:64])
        P.copy(self.yTs[:], st.pt_bf[0][0:64, 0:512])
        P.dma(st.yT_d[0:64, t0:t0 + 512], self.yTs[:], q="pool")
        P.release(mrk)


class SSDMixer:
    def __init__(self, st):
        self.st = st
        P = st.P
        self.xh = [P.sb([64, 515], F32, f"ssd_x{e}") for e in range(2)]
        self.Bh = P.sb([128, 515], F32, "ssd_B")
        self.Ch = P.sb([128, 515], F32, "ssd_C")
        for t in self.xh + [self.Bh, self.Ch]:
            P.memset(t[:, 0:3], 0.0)
        self.S = [P.sb([128, 64], F32, f"ssd_S{e}") for e in range(2)]
        self.Sb = [P.sb([128, 64], BF16, f"ssd_Sb{e}") for e in range(2)]
        for t in self.S + self.Sb:
            P.memset(t[:], 0.0)
        self.Arow = P.sb([128, 2], F32, "ssd_Arow")
        P.act(self.Arow[:], st.pbc[:, BC_ALOG:BC_ALOG + 2], AF.Exp)
        P.ts(self.Arow[:], self.Arow[:], -1.0, None, ALU.mult)

    def block(self, sb):
        st = self.st
        P, pb, pp, mk, cn = st.P, st.pb, st.pp, st.mk, st.cn
        t0 = sb * 512
        mrk = P.mark()
        f = lambda n, r=128, c=512, dt=F32: P.sb([r, c], dt, "ssd_" + n)
        z = [f(f"z{e}", 64) for e in range(2)]
        acc = f("acc")
        xs = [f(f"xs{e}", 64) for e in range(2)]
        xsb = [f(f"xsb{e}", 64, dt=BF16) for e in range(2)]
        Bc = f("Bc", dt=BF16)
        Cc = f("Cc")
        dtt = P.sb([128, 4, 4], F32, "ssd_dtt")
        at = P.sb([128, 4, 2], F32, "ssd_at")
        E = f("E", c=512)
        l1 = [f(f"l1{e}", c=128) for e in range(2)]
        l2 = [f(f"l2{e}", c=128) for e in range(2)]
        CBm = f("CBm", c=128)
        Gt = [f(f"Gt{e}", c=128, dt=BF16) for e in range(2)]
        Cst = [f(f"Cst{e}", c=128, dt=BF16) for e in range(2)]
        dq = P.sb([128, 4], F32, "ssd_dq")
        xdt = [P.sb([128, 64], BF16, f"ssd_xdt{e}") for e in range(2)]
        Bw = [f(f"Bw{e}", c=128, dt=BF16) for e in range(2)]
        yo = [f(f"yo{e}", 64, dt=BF16) for e in range(2)]
        yv = f("yv", 64)
        for e in range(2):
            st.project(C_Z + e * 64, 64, z[e][:], pb[e], evac="act")
        for e in range(2):
            st.project(C_X + e * 64, 64, self.xh[e][:, 3:515], pb[e], evac="dve")
        st.project(C_B, 128, self.Bh[:, 3:515], pb[0], evac="act")
        st.project(C_C, 128, self.Ch[:, 3:515], pb[1], evac="dve")
        for j in range(4):
            for k in range(8):
                P.matmul(pb[2][:, j * 4:j * 4 + 3], st.hT[:, k, j * 128:(j + 1) * 128], st.wB[:, k, C_DT:C_DT + 3], start=(k == 0), stop=(k == 7))
        P.copy(dtt[:, :, 0:3], pb[2][:, 0:16].c3(4)[:, :, 0:3])
        st.fraw = dtt
        P.tt(at[:], dtt[:, :, 0:2], st.pbc[:, BC_DTB:BC_DTB + 2].bmid(4), ALU.add)
        P.act(at[:], at[:], AF.Exp)
        P.act(dtt[:, :, 0:2], at[:], AF.Ln, bias=1.0)
        P.tt(at[:], dtt[:, :, 0:2], self.Arow[:].bmid(4), ALU.mult)
        def conv(tile, rows, wc, bc, out, out2=None):
            a_ = acc[0:rows, :]
            P.ts(a_, tile[:, 0:512], pp[0:rows, wc:wc + 1], None, ALU.mult)
            for k in range(1, 4):
                P.stt(a_, tile[:, k:k + 512], pp[0:rows, wc + k:wc + k + 1], a_, ALU.mult, ALU.add)
            P.act(out, a_, AF.Silu, bias=pp[0:rows, bc:bc + 1])
            if out2 is not None:
                P.copy(out2, out, eng="pool")
            P.copy(tile[:, 0:3], tile[:, 512:515], eng="pool")
        for e in range(2):
            conv(self.xh[e], 64, PP_CW + 4 * e, PP_CB + e, xs[e][:], xsb[e][:])
        conv(self.Bh, 128, PP_CW + 8, PP_CB + 2, Bc[:])
        conv(self.Ch, 128, PP_CW + 12, PP_CB + 3, Cc[:])
        Ccb = f("Ccb", dt=BF16)
        P.copy(Ccb[:], Cc[:], eng="pool")
        for j in range(4):
            cs = slice(j * 128, (j + 1) * 128)
            P.matmul(pb[2][:, 0:128], Bc[:, cs], Ccb[:, cs], start=True, stop=True)
            P.tt(CBm[:], pb[2][:, 0:128], mk[:, MK_TRI, :], ALU.mult)
            P.transpose(st.pt_bf[1][:, 0:128], Bc[:, cs], cn.ident_bf[:])
            for e in range(2):
                acol = at[:, j, e:e + 1]
                P.ts(l1[e][:], mk[:, MK_SLF, :], acol, None, ALU.mult)
                P.ts(l2[e][:], cn.ones_f[:], acol, None, ALU.mult)
                P.matmul(pb[3][:, e * 256:e * 256 + 128], l1[e][:], mk[:, MK_TRI, :], start=True, stop=True)
                P.matmul(pb[3][:, e * 256 + 128:e * 256 + 256], l2[e][:], mk[:, MK_TRI, :], start=True, stop=True)
                P.matmul(pb[2][:, 128 + 2 * e:129 + 2 * e], mk[:, MK_SLF, :], acol, start=True, stop=True)
                P.matmul(pb[2][:, 129 + 2 * e:130 + 2 * e], cn.ones_f[:], acol, start=True, stop=True)
            P.act(E[:], pb[3][:, :], AF.Exp)
            P.act(dq[:], pb[2][:, 128:132], AF.Exp)
            for e in range(2):
                P.tt(Gt[e][:], E[:, e * 256:e * 256 + 128], CBm[:], ALU.mult)
                P.tt(Cst[e][:], E[:, e * 256 + 128:e * 256 + 256], Cc[:, cs], ALU.mult)
                P.transpose(st.pt_bf[0][:, e * 64:(e + 1) * 64], xsb[e][:, cs], cn.ident_bf[0:64, 0:64])
                P.ts(xdt[e][:], st.pt_bf[0][:, e * 64:(e + 1) * 64], dtt[:, j, e:e + 1], None, ALU.mult)
                P.ts(Bw[e][:], st.pt_bf[1][:, 0:128], dq[:, 2 * e:2 * e + 1], None, ALU.mult)
                yreg = pb[4 + e][0:64, cs]
                P.matmul(yreg, xdt[e][:], Gt[e][:], start=True, stop=False)
                P.matmul(yreg, self.Sb[e][:], Cst[e][:], start=False, stop=True)
                sreg = pb[2][:, 256 + e * 64:256 + (e + 1) * 64]
                P.matmul(sreg, Bw[e][:], xdt[e][:], start=True, stop=True)
                P.stt(self.S[e][:], self.S[e][:], dq[:, 2 * e + 1:2 * e + 2], sreg, ALU.mult, ALU.add)
                P.copy(self.Sb[e][:], self.S[e][:], eng="act")
        for e in range(2):
            P.act(z[e][:], z[e][:], AF.Silu)
            P.stt(yv[:], xs[e][:], pp[0:64, PP_D + e:PP_D + e + 1], pb[4 + e][0:64, :], ALU.mult, ALU.add)
            P.tt(yo[e][:], yv[:], z[e][:], ALU.mult)
            P.dma(st.yT_d[64 + e * 64:64 + (e + 1) * 64, t0:t0 + 512], yo[e][:], q="pool")
        P.release(mrk)


class FoxMixer:
    def __init__(self, st):
        self.st = st
        P = st.P
        L = st.L
        NB = L // 128
        self.KT = P.sb([67, L], BF16, "fox_KT")
        P.memset(self.KT[64:67, :], 1.0)
        self.Vaug = P.sb([128, NB, 65], BF16, "fox_V")
        P.memset(self.Vaug[:, :, 64:65], 1.0)
        self.negF = P.sb([128, NB], F32, "fox_negF")
        self.totb = P.sb([128, 1], F32, "fox_totb")
        P.memset(self.totb[:], 0.0)
        self.negfb = P.sb([128, 1], F32, "fox_negfb")
        P.ts(self.negfb[:], st.pbc[:, BC_FB:BC_FB + 1], -1.0, None, ALU.mult)
        self.qg = P.sb([64, 1], F32, "fox_qg")
        P.ts(self.qg[:], st.pp[0:64, PP_QG:PP_QG + 1], 0.125, None, ALU.mult)
        self.nmb = P.sb([128, 128], BF16, "fox_nmb")
        P.copy(self.nmb[:], st.mk[:, MK_NEG, :])
        self.nlw = P.sb([128, 4, 67], F32, "fox_nlw")
        P.memset(self.nlw[:], 0.0)
        self.pT = [P.sb([128, 512], BF16, f"fox_pT{i}") for i in range(4)]
        self.QT = P.sb([67, 512], BF16, "fox_QT")
        self.o = P.sb([65, 512], F32, "fox_o")
        self.rec = P.sb([64, 512], F32, "fox_rec")
        self.yo = P.sb([64, 512], BF16, "fox_yo")

    def prep(self, sb):
        st = self.st
        P, pb, pp, mk, cn = st.P, st.pb, st.pp, st.mk, st.cn
        t0 = sb * 512
        mrk = P.mark()
        f = lambda n, r=64, c=512, dt=F32: P.sb([r, c], dt, "fox_" + n)
        q, k, v = f("q"), f("k"), f("v")
        sq = f("sq", dt=BF16)
        rs = f("rs")
        vb = f("vb", dt=BF16)
        QT = self.QT
        nl = P.sb([128, 4], F32, "fox_nl")
        totc = P.sb([128, 4], F32, "fox_totc")
        tt4 = P.sb([128, 8], F32, "fox_tt4")
        Fv = f("Fv", 67)
        r1 = f("r1", 67)
        hf = f("hf", 67)
        hb = f("hb", 67, dt=BF16)
        ta = f("ta", 67)
        st.project(C_FQ, 64, q[:], pb[2], evac="act")
        st.project(C_FK, 64, k[:], pb[3], evac="dve")
        st.project(C_FV, 64, v[:], pb[2], evac="act")
        for j in range(4):
            for kk in range(8):
                P.matmul(pb[3][:, j:j + 1], st.hT[:, kk, j * 128:(j + 1) * 128], st.wB[:, kk, C_F:C_F + 1], start=(kk == 0), stop=(kk == 7))
        P.act(nl[:], pb[3][:, 0:4], AF.Exp, scale=-1.0, bias=self.negfb[:])
        P.act(nl[:], nl[:], AF.Ln, bias=1.0)
        P.matmul(pb[3][:, 8:12], cn.ones_f[:], nl[:], start=True, stop=True)
        P.matmul(pb[3][:, 12:16], mk[:, MK_TRI, :], nl[:], start=True, stop=True)
        P.copy(tt4[:, 0:8], pb[3][:, 8:16])
        P.copy(totc[:, 0:1], self.totb[:])
        for j in range(1, 4):
            P.tt(totc[:, j:j + 1], totc[:, j - 1:j], tt4[:, j - 1:j], ALU.add)
        P.tt(self.totb[:], totc[:, 3:4], tt4[:, 3:4], ALU.add)
        P.tt(self.negF[:, 4 * sb:4 * sb + 4], tt4[:, 4:8], totc[:], ALU.add)
        for j in range(4):
            P.copy(self.nlw[:, j, 64:67], nl[:, j:j + 1].bcast([128, 3]))
        for j in range(4):
            P.matmul(pb[2][0:67, j * 128:(j + 1) * 128], self.nlw[:, j, :], mk[:, MK_TRI, :], start=True, stop=True)
        for j in range(4):
            cs = slice(j * 128, (j + 1) * 128)
            P.ts(Fv[64:67, cs], pb[2][64:67, cs], totc[64:67, j:j + 1], -1.0, ALU.add, ALU.mult)
        R = slice(64, 67)
        P.copy(hb[R, :], Fv[R, :])
        P.copy(hf[R, :], hb[R, :])
        P.ts(ta[R, :], hf[R, :], pp[R, PP_SEL:PP_SEL + 1], None, ALU.mult)
        P.tt(r1[R, :], Fv[R, :], hf[R, :], ALU.subtract)
        P.copy(hb[R, :], r1[R, :])
        P.copy(hf[R, :], hb[R, :])
        P.stt(ta[R, :], hf[R, :], pp[R, PP_SEL + 1:PP_SEL + 2], ta[R, :], ALU.mult, ALU.add)
        P.tt(r1[R, :], r1[R, :], hf[R, :], ALU.subtract)
        P.copy(hb[R, :], r1[R, :])
        P.copy(hf[R, :], hb[R, :])
        P.stt(QT[R, :], hf[R, :], pp[R, PP_SEL + 2:PP_SEL + 3], ta[R, :], ALU.mult, ALU.add)
        for src, gcol, dst in ((q, self.qg[:], QT[0:64, :]), (k, pp[0:64, PP_KG:PP_KG + 1], self.KT[0:64, t0:t0 + 512])):
            P.tt(sq[:], src[:], src[:], ALU.mult)
            P.matmul(pb[3][0:64, :], cn.ones_bf[0:64, 0:64], sq[:], start=True, stop=True)
            P.ts(rs[:], pb[3][0:64, :], 1.0 / 64, EPS, ALU.mult, ALU.add)
            rsqrt_inplace(P, rs[:])
            P.stt(dst, src[:], gcol, rs[:], ALU.mult, ALU.mult)
        P.copy(vb[:], v[:], eng="pool")
        for j in range(4):
            P.transpose(st.pt_bf[0][:, j * 64:(j + 1) * 64], vb[:, j * 128:(j + 1) * 128], cn.ident_bf[0:64, 0:64])
        P.copy(self.Vaug[:, 4 * sb:4 * sb + 4, 0:64], st.pt_bf[0][:, 0:256].c3())
        P.release(mrk)

    def loop(self, sb):
        st = self.st
        P, pb, mk, cn = st.P, st.pb, st.mk, st.cn
        t0 = sb * 512
        QT, o, rec, yo = self.QT, self.o, self.rec, self.yo
        po = pb[4]
        nkb = 4 * sb + 4
        NSC = 4
        AHEAD = 3

        def score(kb):
            ps = pb[kb % NSC]
            Kb = self.KT[0:67, kb * 128:(kb + 1) * 128]
            d = kb - 4 * sb
            if d < 0:
                P.matmul(ps[:, :], Kb, QT[:, :], start=True, stop=True)
            else:
                c0 = d * 128
                P.matmul(ps[:, c0:c0 + 128], Kb, QT[:, c0:c0 + 128], start=True, stop=False)
                P.matmul(ps[:, c0:c0 + 128], cn.ident_bf[:], self.nmb[:], start=False, stop=True)
                if c0 + 128 < 512:
                    P.matmul(ps[:, c0 + 128:512], Kb, QT[:, c0 + 128:512], start=True, stop=True)

        def expo(kb):
            c0 = max(kb - 4 * sb, 0) * 128
            pT = self.pT[kb % NSC]
            P.act(pT[:, c0:512], pb[kb % NSC][:, c0:512], AF.Exp, bias=self.negF[:, kb:kb + 1])
            if c0 > 0:
                P.memset(pT[:, 0:c0], 0.0)

        def pv(kb):
            P.matmul(po[0:65, :], self.Vaug[:, kb, :], self.pT[kb % NSC][:, :], start=(kb == 0), stop=(kb == nkb - 1))

        for kb in range(min(AHEAD, nkb)):
            score(kb)
        for kb in range(nkb):
            expo(kb)
            if kb + AHEAD < nkb:
                score(kb + AHEAD)
            pv(kb)
            yield
        P.copy(o[:], po[0:65, :])
        P.matmul(pb[5][0:64, :], mk[0:65, MK_SEL, 0:64], o[:], start=True, stop=True)
        P.recip(rec[:], pb[5][0:64, :])
        P.tt(yo[:], o[0:64, :], rec[:], ALU.mult)
        P.dma(st.yT_d[192:256, t0:t0 + 512], yo[:], q="pool")
        yield


def build_B(L, layer, parts=("rw", "ssd", "fox")):
    nc = bass.Bass("TRN2", target_bir_lowering=False)
    P = Prog(nc)
    NSB = L // 512
    NWc = NW1 if layer == 1 else NW0
    x_d = P.dram("x", [L, D], F32, "ExternalInput")
    c_d = P.dram("c", [1, D], F32, "ExternalInput")
    ada_w_d = P.dram("ada_w", [D, 6 * D], F32, "ExternalInput")
    ada_b_d = P.dram("ada_b", [1, 6 * D], F32, "ExternalInput")
    nmix_d = P.dram("nmix", [1, D], F32, "ExternalInput")
    wB_d = P.dram("wB", [D, NWc], F32, "ExternalInput")
    pp_d = P.dram("pp", [128, NPP], F32, "ExternalInput")
    pbc_d = P.dram("pbc", [1, NBC], F32, "ExternalInput")
    mask_d = P.dram("masks", [128, NMASK, 128], F32, "ExternalInput")
    wup_d = P.dram("wup", [64, 64], F32, "ExternalInput")
    aup_d = P.dram("aup", [64, 64], F32, "ExternalInput")
    gup_d = P.dram("gup", [160, 64], F32, "ExternalInput")
    if layer == 1:
        vup_d = P.dram("vup", [32, 64], F32, "ExternalInput")
        vf_d = P.dram("vfT", [64, L], F32, "ExternalInput")
    else:
        vfo_d = P.dram("vfT_out", [64, L], F32, "ExternalOutput")
    yT_d = P.dram("yT", [256, L], BF16, "ExternalOutput")

    cn = mk_consts(P)
    pb = [P.ps([128, 512], F32, f"pb{i}") for i in range(6)]
    pt_bf = [P.ps([128, 1024], BF16, "ptA"), P.ps([128, 1024], BF16, "ptB")]
    ht = HT(P, cn, pt_bf)

    pp = P.sb([128, NPP], F32, "pp")
    P.dma(pp[:], pp_d[:, :])
    pbc = P.sb([128, NBC], F32, "pbc")
    P.dma(pbc[:], pbc_d[0:1, :].bcast([128, NBC]))
    mk = P.sb([128, NMASK, 128], F32, "mk")
    P.dma(mk[:], mask_d[:, :, :], q="act")
    A_m = P.sb([128, 8], F32, "A_m")
    sh_m = P.sb([128, 8], F32, "sh_m")
    m0 = P.mark()
    cols, _ = emit_mod(P, cn, c_d, ada_w_d, ada_b_d, [0, 1], [], pb[0], pb[1])
    nmix_c = load_col(P, nmix_d[0, :], 8, "nmix_c")
    P.ts(A_m[:], cols[1][:], 1.0, None, ALU.add)
    P.tt(A_m[:], A_m[:], nmix_c[:], ALU.mult)
    P.copy(sh_m[:], cols[0][:])
    P.release(m0)
    wB = P.sb([128, 8, NWc], BF16, "wB")
    wup = P.sb([64, 64], BF16, "wup")
    aup = P.sb([64, 64], BF16, "aup")
    gupa = P.sb([128, 64], BF16, "gupa")
    gupb = P.sb([32, 64], BF16, "gupb")
    if layer == 1:
        vup = P.sb([32, 64], BF16, "vup")
    m_ld = P.mark()
    ld = Loader(P)
    for k in range(8):
        ld.load(wB[:, k, :], wB_d[k * 128:(k + 1) * 128, :])
    ld.load(wup[:, :], wup_d[:, :], np_=64)
    ld.load(aup[:, :], aup_d[:, :], np_=64)
    ld.load(gupa[:, :], gup_d[0:128, :])
    ld.load(gupb[:, :], gup_d[128:160, :], np_=32)
    if layer == 1:
        ld.load(vup[:, :], vup_d[:, :], np_=32)
    P.release(m_ld)

    xt = [P.sb([128, D], F32, f"xt{i}") for i in range(4)]
    hTs = [P.sb([128, 8, 512], BF16, f"hT{i}") for i in range(2)]

    def project(c0, ncol, dst, psb, evac="act"):
        hT = st.hT
        for k in range(8):
            P.matmul(psb[0:ncol, :], wB[:, k, c0:c0 + ncol], hT[:, k, :], start=(k == 0), stop=(k == 7))
        if evac == "act":
            P.act(dst, psb[0:ncol, :], AF.Copy)
        else:
            P.copy(dst, psb[0:ncol, :])

    st = Ctx()
    st.P, st.cn, st.pb, st.pp, st.pbc, st.mk, st.hT, st.wB, st.project = P, cn, pb, pp, pbc, mk, hTs[0], wB, project
    st.L, st.layer, st.NSB, st.yT_d, st.pt_bf = L, layer, NSB, yT_d, pt_bf
    st.wup, st.aup, st.gupa, st.gupb = wup, aup, gupa, gupb
    if layer == 1:
        st.vup, st.vf_d = vup, vf_d
    else:
        st.vfo_d = vfo_d
    rw = RWMixer(st) if "rw" in parts else None
    ssd = SSDMixer(st) if "ssd" in parts else None
    fox = FoxMixer(st) if "fox" in parts else None

    pb5bf = pb[5][:, :].bitcast(BF16)

    def load_x(sb):
        t0 = sb * 512
        for i in range(4):
            P.dma(xt[i][:], x_d[t0 + i * 128:t0 + (i + 1) * 128, :], q="sp" if i % 2 == 0 else "act")

    def gen_hT(sb, banks):
        hT = hTs[sb % 2]
        for i in range(4):
            ht.emit(xt[i][:], A_m, sh_m, lambda k, i=i: hT[:, k, i * 128:(i + 1) * 128], banks=banks)
            yield

    load_x(0)
    for _ in gen_hT(0, None):
        pass
    pending = None
    for sb in range(NSB):
        st.hT = hTs[sb % 2]
        items = []
        if pending is not None:
            items.append([pending[0], pending[1], 0, False])
        if sb + 1 < NSB:
            load_x(sb + 1)
            items.append([gen_hT(sb + 1, [pb5bf[:, 0:512], pb5bf[:, 512:1024]]), 4, 0, False])
        calls = [0]
        NCALLS = 26

        def filler(frac, items=items, calls=calls):
            calls[0] += 1
            target = 1.0 if frac >= 1.0 else min(1.0, calls[0] / NCALLS)
            for it in items:
                gen, nsteps = it[0], it[1]
                while not it[3] and (frac >= 1.0 or it[2] < nsteps * target):
                    try:
                        next(gen)
                        it[2] += 1
                    except StopIteration:
                        it[3] = True
        if rw:
            rw.block(sb, filler if items else None)
        filler(1.0)
        pending = None
        if ssd:
            ssd.block(sb)
        if fox:
            fox.prep(sb)
            pending = (fox.loop(sb), 4 * sb + 5)
    if pending is not None:
        for _ in pending[0]:
            pass
    print("B peak sbuf", P.peak, "ops", len(P.ops))
    return P.emit()


RW_W, SSD_W, FOX_W = 512, 1024, 512
RW_COLS_, SSD_COLS_, FOX_COLS_ = 1824, 3088, 1544


def core_inputs_B(inp, l, i, x, vfT=None):
    w_in = inp['w_in'][l]
    o_rw, o_ssd, o_fox = 0, RW_COLS_, RW_COLS_ + SSD_COLS_
    hs = slice(i * 64, (i + 1) * 64)
    W, SW, FW = RW_W, SSD_W, FOX_W
    g = i // 2
    ar = np.arange
    cols = [ar(o_rw + i * 64, o_rw + (i + 1) * 64), ar(o_rw + W + i * 64, o_rw + W + (i + 1) * 64),
            ar(o_rw + 2 * W + i * 64, o_rw + 2 * W + (i + 1) * 64),
            ar(o_rw + 3 * W, o_rw + 3 * W + 64), ar(o_rw + 3 * W + 64, o_rw + 3 * W + 128), ar(o_rw + 3 * W + 128, o_rw + 3 * W + 288),
            ar(o_ssd + i * 128, o_ssd + (i + 1) * 128), ar(o_ssd + SW + i * 128, o_ssd + SW + (i + 1) * 128),
            ar(o_ssd + 2 * SW + g * 128, o_ssd + 2 * SW + (g + 1) * 128), ar(o_ssd + 2 * SW + 512 + g * 128, o_ssd + 2 * SW + 512 + (g + 1) * 128),
            ar(o_fox + i * 64, o_fox + (i + 1) * 64), ar(o_fox + FW + i * 64, o_fox + FW + (i + 1) * 64), ar(o_fox + 2 * FW + i * 64, o_fox + 2 * FW + (i + 1) * 64),
            ar(o_ssd + 2 * SW + 1024 + 2 * i, o_ssd + 2 * SW + 1024 + 2 * i + 2), np.array([o_fox + 3 * FW + i])]
    cols = np.concatenate(cols)
    wB = w_in[:, cols]
    if l == 1:
        wB = np.concatenate([wB, inp['rw_v_down'][0]], axis=1)
    pp = np.zeros((128, NPP), np.float32)
    mu = inp['rw_mu'][l]
    pp[0:64, PP_MU + 0] = mu[0 * W + i * 64:0 * W + (i + 1) * 64]
    pp[0:64, PP_MU + 1] = mu[1 * W + i * 64:1 * W + (i + 1) * 64]
    pp[0:64, PP_MU + 2] = mu[2 * W + i * 64:2 * W + (i + 1) * 64]
    pp[0:64, PP_MU + 3] = mu[3 * W:3 * W + 64]
    pp[0:64, PP_MU + 4] = mu[3 * W + 64:3 * W + 128]
    pp[0:128, PP_MU + 5] = mu[3 * W + 128:3 * W + 256]
    pp[0:32, PP_MU + 6] = mu[3 * W + 256:3 * W + 288]
    pp[0:64, PP_W0] = inp['rw_w0'][l][hs]
    pp[0:64, PP_A0] = inp['rw_a0'][l][hs]
    pp[0:64, PP_KK] = inp['rw_k_k'][l][hs]
    pp[0:64, PP_KA] = inp['rw_k_a'][l][hs]
    pp[0:64, PP_RK] = inp['rw_r_k'][l][i]
    if l == 1:
        pp[0:64, PP_V0] = inp['rw_v0'][0][hs]
    cw, cb = inp['ssd_conv_w'][l], inp['ssd_conv_b'][l]
    xs_ = slice(i * 128, (i + 1) * 128)
    bs_ = slice(SW + g * 128, SW + (g + 1) * 128)
    cs_ = slice(SW + 512 + g * 128, SW + 512 + (g + 1) * 128)
    for t in range(4):
        pp[0:64, PP_CW + t] = cw[t, xs_][0:64]
        pp[0:64, PP_CW + 4 + t] = cw[t, xs_][64:128]
        pp[:, PP_CW + 8 + t] = cw[t, bs_]
        pp[:, PP_CW + 12 + t] = cw[t, cs_]
    pp[0:64, PP_CB] = cb[xs_][0:64]
    pp[0:64, PP_CB + 1] = cb[xs_][64:128]
    pp[:, PP_CB + 2] = cb[bs_]
    pp[:, PP_CB + 3] = cb[cs_]
    pp[0:64, PP_QG] = inp['fox_q_gain'][l]
    pp[0:64, PP_KG] = inp['fox_k_gain'][l]
    pp[:, PP_D] = inp['ssd_d'][l][2 * i]
    pp[:, PP_D + 1] = inp['ssd_d'][l][2 * i + 1]
    pp[64, PP_SEL] = 1.0
    pp[65, PP_SEL + 1] = 1.0
    pp[66, PP_SEL + 2] = 1.0
    pbc = np.zeros((1, NBC), np.float32)
    pbc[0, BC_LNW:BC_LNW + 64] = inp['rw_lnx_w'][l][hs]
    pbc[0, BC_LNB:BC_LNB + 64] = inp['rw_lnx_b'][l][hs]
    pbc[0, BC_DTB:BC_DTB + 2] = inp['ssd_dt_bias'][l][2 * i:2 * i + 2]
    pbc[0, BC_ALOG:BC_ALOG + 2] = inp['ssd_a_log'][l][2 * i:2 * i + 2]
    pbc[0, BC_FB] = inp['fox_f_bias'][l][i]
    m = dict(x=x, c=inp['c'], ada_w=inp['ada_w'][l], ada_b=inp['ada_b'][l][None], nmix=inp['norm_mix'][l][None],
             wB=np.ascontiguousarray(wB), pp=pp, pbc=pbc, masks=make_masks(),
             wup=np.ascontiguousarray(inp['rw_w_up'][l][:, hs]), aup=np.ascontiguousarray(inp['rw_a_up'][l][:, hs]),
             gup=np.ascontiguousarray(inp['rw_g_up'][l][:, hs]))
    if l == 1:
        m['vup'] = np.ascontiguousarray(inp['rw_v_up'][0][:, hs])
        m['vfT'] = vfT
    return m


def common_inputs_C(inp, l):
    off_gate = RW_COLS_ + SSD_COLS_ + FOX_COLS_
    common = dict(
        c=inp['c'], ada_w=inp['ada_w'][l], ada_b=inp['ada_b'][l][None], nmix=inp['norm_mix'][l][None], nffn=inp['norm_ffn'][l][None],
        wgate=np.ascontiguousarray(inp['w_in'][l][:, off_gate:]), gateb=inp['gate_b'][l][None],
        proj=np.concatenate([inp['proj_rw'][l], inp['proj_ssd'][l], inp['proj_fox'][l]], axis=0), wout=inp['w_out'][l], ssdn=inp['ssd_norm'][l][None])
    if l % 2 == 1:
        common.update(routT=np.ascontiguousarray(inp['moe_router'][l // 2].T), wgu=inp['moe_w_gu'][l // 2], wdn=inp['moe_w_down'][l // 2])
    else:
        common.update(wgu=inp['ffn_w_gu'][l // 2][None], wdn=inp['ffn_w_down'][l // 2][None])
    return common


def assemble_yT(resB, L):
    yT = np.empty((2048, L), dtype=resB[0]['yT'].dtype)
    for i, r in enumerate(resB):
        y = r['yT']
        yT[i * 64:(i + 1) * 64] = y[0:64]
        yT[512 + i * 128:512 + (i + 1) * 128] = y[64:192]
        yT[1536 + i * 64:1536 + (i + 1) * 64] = y[192:256]
    return yT


NCORES = 8


def kernel(**inputs):
    inp = {k: np.asarray(v) for k, v in inputs.items()}
    L = inp['x'].shape[1]
    T_c = L // NCORES
    x = np.ascontiguousarray(inp['x'][0])
    cores = list(range(NCORES))
    vfT = [None] * NCORES
    for l in range(2):
        ncB = build_B(L, l)
        mapsB = [core_inputs_B(inp, l, i, x, vfT[i]) for i in cores]
        resB = run_bass_kernel_spmd(ncB, mapsB, core_ids=cores).results
        if l == 0:
            vfT = [np.ascontiguousarray(r['vfT_out']) for r in resB]
        yT = assemble_yT(resB, L)
        ncC = build_C(T_c, moe=(l % 2 == 1))
        common = common_inputs_C(inp, l)
        mapsC = []
        for j in cores:
            m = dict(common)
            m['x'] = np.ascontiguousarray(x[j * T_c:(j + 1) * T_c])
            m['yT'] = np.ascontiguousarray(yT[:, j * T_c:(j + 1) * T_c])
            mapsC.append(m)
        resC = run_bass_kernel_spmd(ncC, mapsC, core_ids=cores).results
        x = np.concatenate([r['out'] for r in resC], axis=0)
    return x[None].astype(np.float32)
```

```python
from contextlib import ExitStack
import numpy as np
import concourse.bass as bass
import concourse.mybir as mybir
from concourse.bass_utils import run_bass_kernel_spmd

F32 = mybir.dt.float32
BF16 = mybir.dt.bfloat16
ALU = mybir.AluOpType
AF = mybir.ActivationFunctionType
AX = mybir.AxisListType


class Tl:
    def __init__(self, t, name):
        self.t = t
        self.name = name
        self.last_w = None
        self.readers = []
        self.alias = None

    def __getitem__(self, key):
        return V(self, self.t[key])


class V:
    def __init__(self, tl, ap):
        self.tl = tl
        self.ap = ap

    def __getitem__(self, key):
        return V(self.tl, self.ap[key])

    def bitcast(self, dt):
        return V(self.tl, self.ap.bitcast(dt))

    def rearrange(self, s, **kw):
        return V(self.tl, self.ap.rearrange(s, **kw))

    def bcast(self, shape):
        return V(self.tl, self.ap.to_broadcast(shape))

    def bmid(self, n):
        p, m = self.ap.shape
        return V(self.tl, self.ap.unsqueeze(1).to_broadcast([p, n, m]))

    def binner(self, m):
        p, n = self.ap.shape
        return V(self.tl, self.ap.unsqueeze(2).to_broadcast([p, n, m]))

    def c3(self, t=64):
        return V(self.tl, self.ap.rearrange("p (c t) -> p c t", t=t))


class Op:
    __slots__ = ("eng", "fn", "reads", "writes", "dma", "deps", "need_inc", "cnt", "slot", "slot_prev")

    def __init__(self, eng, fn, reads, writes, dma):
        self.eng = eng
        self.fn = fn
        self.reads = reads
        self.writes = writes
        self.dma = dma
        self.deps = []
        self.need_inc = False
        self.cnt = 0
        self.slot = None
        self.slot_prev = 0


NDMA_SLOTS = 8
ARENA_BYTES = 212736
ENGS = ["pe", "act", "dve", "pool", "sp"]


class Prog:
    def __init__(self, nc):
        self.nc = nc
        self.ops = []
        self.stack = ExitStack()
        self.n_t = 0
        self.arena_t = self.stack.enter_context(nc.sbuf_tensor("arena", [128, ARENA_BYTES // 2], BF16))
        self.bump = 0
        self.live = []
        self.freed = []
        self.peak = 0

    def sb(self, shape, dt, name=None):
        self.n_t += 1
        name = name or f"t{self.n_t}"
        esz = 4 if dt == F32 else 2
        n = int(np.prod(shape[1:]))
        nbytes = n * esz
        nal = (nbytes + 63) // 64 * 64
        off = self.bump
        assert off + nal <= ARENA_BYTES, f"SBUF arena overflow allocating {name}: {off}+{nal}"
        self.bump += nal
        self.peak = max(self.peak, self.bump)
        ap = self.arena_t[0:shape[0], off // 2:(off + nbytes) // 2]
        if dt == F32:
            ap = ap.bitcast(F32)
        if len(shape) == 3:
            ap = ap.rearrange("p (a b) -> p a b", b=shape[2])
        tl = Tl(ap, name)
        al = [f for (o, e, f) in self.freed if o < off + nal and e > off]
        tl.alias = al if al else None
        self.live.append((off, off + nal, tl))
        return tl

    def mark(self):
        return (self.bump, len(self.live))

    def release(self, m):
        b, n = m
        self.freed.extend(self.live[n:])
        del self.live[n:]
        self.bump = b

    def ps(self, shape, dt, name=None):
        self.n_t += 1
        name = name or f"p{self.n_t}"
        t = self.stack.enter_context(self.nc.psum_tensor(name, list(shape), dt))
        return Tl(t, name)

    def dram(self, name, shape, dt, kind):
        t = self.nc.dram_tensor(name, list(shape), dt, kind=kind).ap()
        return Tl(t, name)

    def _add(self, eng, fn, reads, writes, dma=False):
        r = []
        for v in reads:
            if isinstance(v, V) and v.tl not in r:
                r.append(v.tl)
        w = []
        for v in writes:
            if isinstance(v, V) and v.tl not in w:
                w.append(v.tl)
        self.ops.append(Op(eng, fn, r, w, 16 if dma is True else (dma or 0)))

    @staticmethod
    def _a(v):
        return v.ap if isinstance(v, V) else v

    def dma(self, out, in_, q="sp", **kw):
        a = self._a
        self._add(q, lambda e: e.dma_start(out=a(out), in_=a(in_), **kw), [in_], [out], dma=True)

    def matmul(self, out, lhsT, rhs, start=True, stop=True, **kw):
        a = self._a
        self._add("pe", lambda e: e.matmul(a(out), a(lhsT), a(rhs), start=start, stop=stop, **kw), [lhsT, rhs], [out])

    def transpose(self, out, in_, ident):
        a = self._a
        self._add("pe", lambda e: e.transpose(a(out), a(in_), a(ident)), [in_, ident], [out])

    def act(self, out, in_, func, bias=None, scale=None, accum_out=None, eng="act"):
        a = self._a
        kw = {}
        if bias is not None:
            kw["bias"] = a(bias)
        if scale is not None:
            kw["scale"] = a(scale)
        if accum_out is not None:
            kw["accum_out"] = a(accum_out)
        self._add(eng, lambda e: e.activation(a(out), a(in_), func, **kw), [in_, bias, scale], [out, accum_out])

    def ts(self, out, in0, s1, s2, op0, op1=None, accum_out=None, eng="dve"):
        a = self._a
        kw = {}
        if accum_out is not None:
            kw["accum_out"] = a(accum_out)
        if op1 is None:
            self._add(eng, lambda e: e.tensor_single_scalar(a(out), a(in0), a(s1), op0), [in0, s1], [out])
        else:
            self._add(eng, lambda e: e.tensor_scalar(a(out), a(in0), a(s1), a(s2), op0, op1, **kw), [in0, s1, s2], [out, accum_out])

    def tt(self, out, in0, in1, op, eng="dve"):
        a = self._a
        self._add(eng, lambda e: e.tensor_tensor(a(out), a(in0), a(in1), op), [in0, in1], [out])

    def stt(self, out, in0, scalar, in1, op0, op1, accum_out=None, eng="dve"):
        a = self._a
        kw = {}
        if accum_out is not None:
            kw["accum_out"] = a(accum_out)
        self._add(eng, lambda e: e.scalar_tensor_tensor(a(out), a(in0), a(scalar), a(in1), op0, op1, **kw), [in0, scalar, in1], [out, accum_out])

    def copy(self, out, in_, eng="dve"):
        a = self._a
        if eng == "act":
            self._add(eng, lambda e: e.activation(a(out), a(in_), AF.Copy), [in_], [out])
        else:
            self._add(eng, lambda e: e.tensor_copy(a(out), a(in_)), [in_], [out])

    def memset(self, out, val, eng="pool"):
        a = self._a
        self._add(eng, lambda e: e.memset(a(out), val), [], [out])

    def recip(self, out, in_):
        a = self._a
        self._add("dve", lambda e: e.reciprocal(a(out), a(in_)), [in_], [out])

    def reduce(self, out, in_, op, axis=AX.X, eng="dve"):
        a = self._a
        self._add(eng, lambda e: e.tensor_reduce(a(out), a(in_), axis, op), [in_], [out])

    def affine_select(self, out, in_, pattern, compare_op, fill, base, channel_multiplier):
        a = self._a
        self._add("pool", lambda e: e.affine_select(out=a(out), in_=a(in_), pattern=pattern, compare_op=compare_op, fill=fill, base=base, channel_multiplier=channel_multiplier), [in_], [out])

    def generic(self, eng, fn, reads, writes, dma=False):
        self._add(eng, fn, reads, writes, dma)

    def emit(self):
        nc = self.nc
        ops = self.ops
        for i, op in enumerate(ops):
            deps = {}
            for tl in op.reads + op.writes:
                if tl.alias:
                    for par in tl.alias:
                        tl.readers.extend(par.readers)
                        if par.last_w is not None:
                            tl.readers.append(par.last_w)
                    tl.alias = None
            for tl in op.reads:
                if tl.last_w is not None:
                    deps[tl.last_w] = "raw"
            for tl in op.writes:
                if tl.last_w is not None and tl.last_w not in deps:
                    deps[tl.last_w] = "waw"
                for r in tl.readers:
                    if r not in deps:
                        deps[r] = "war"
            for p, kind in deps.items():
                if p == i:
                    continue
                po = ops[p]
                if (not po.dma) and (not op.dma) and po.eng == op.eng and op.eng == "pe":
                    continue
                op.deps.append(p)
                po.need_inc = True
            for tl in op.reads:
                tl.readers.append(i)
            for tl in op.writes:
                tl.last_w = i
                tl.readers = []
        cnt = {e: 0 for e in ENGS}
        slot_n = {e: 0 for e in ENGS}
        slot_cnt = {}
        for op in ops:
            if op.dma:
                sk = op.eng if op.dma == 16 else op.eng + "_cc"
                slot_n.setdefault(sk, 0)
                k = slot_n[sk] % NDMA_SLOTS
                slot_n[sk] += 1
                key = (sk, k)
                op.slot = key
                op.slot_prev = slot_cnt.get(key, 0)
                slot_cnt[key] = op.slot_prev + 1
                op.cnt = op.slot_prev + 1
            elif op.need_inc:
                cnt[op.eng] += 1
                op.cnt = cnt[op.eng]
        st = self.stack
        sem_e = {e: st.enter_context(nc.semaphore(f"s_{e}")) for e in ENGS}
        sem_d = {}
        for e in list(slot_n.keys()):
            if slot_n[e] > 0:
                for k in range(min(NDMA_SLOTS, slot_n[e])):
                    sem_d[(e, k)] = st.enter_context(nc.semaphore(f"d_{e}{k}"))
        block = st.enter_context(nc.Block())

        def run_engine(ename):
            def body(eng):
                waited = {}

                def wait(sem, key, val):
                    if waited.get(key, 0) >= val:
                        return
                    waited[key] = val
                    eng.wait_ge(sem, val)

                for op in ops:
                    if op.eng != ename:
                        continue
                    need = {}
                    for p in op.deps:
                        po = ops[p]
                        if po.dma:
                            key = ("d",) + po.slot
                            val = po.dma * po.cnt
                            sem = sem_d[po.slot]
                        else:
                            key = ("e", po.eng)
                            val = po.cnt
                            sem = sem_e[po.eng]
                        if need.get(key, (None, 0))[1] < val:
                            need[key] = (sem, val)
                    if op.dma and op.slot_prev > 0:
                        key = ("d",) + op.slot
                        val = op.dma * op.slot_prev
                        if need.get(key, (None, 0))[1] < val:
                            need[key] = (sem_d[op.slot], val)
                    for key, (sem, val) in need.items():
                        wait(sem, key, val)
                    ins = op.fn(eng)
                    if op.dma == 16:
                        ins.then_inc(sem_d[op.slot], 16)
                    elif op.dma:
                        ins.then_inc(sem_d[op.slot])
                    elif op.need_inc:
                        ins.then_inc(sem_e[ename], 1)
                for (e, k), sem in sem_d.items():
                    if e == ename or e == ename + "_cc":
                        wait(sem, ("d", e, k), (16 if e == ename else 1) * slot_cnt[(e, k)])
            return body

        used = set(op.eng for op in ops)
        if "pe" in used:
            block.tensor(run_engine("pe"))
        if "act" in used:
            block.scalar(run_engine("act"))
        if "dve" in used:
            block.vector(run_engine("dve"))
        if "pool" in used:
            block.gpsimd(run_engine("pool"))
        if "sp" in used:
            block.sync(run_engine("sp"))
        st.close()
        return nc


D = 1024
EPS = 1e-6


class Ctx:
    pass


def mk_consts(P):
    c = Ctx()
    c.ones_bf = P.sb([128, 128], BF16, "ones_bf")
    c.ones_f = P.sb([128, 128], F32, "ones_f")
    c.ident_bf = P.sb([128, 128], BF16, "ident_bf")
    c.ident_f = P.sb([128, 128], F32, "ident_f")
    P.memset(c.ones_bf[:], 1.0)
    P.memset(c.ones_f[:], 1.0)
    P.affine_select(c.ident_bf[:], c.ones_bf[:], [[-1, 128]], ALU.is_equal, 0.0, 0, 1)
    P.affine_select(c.ident_f[:], c.ones_f[:], [[-1, 128]], ALU.is_equal, 0.0, 0, 1)
    return c


def rsqrt_inplace(P, v, eps=None):
    if eps is not None:
        P.ts(v, v, eps, None, ALU.add)
    P.act(v, v, AF.Ln)
    P.act(v, v, AF.Exp, scale=-0.5)


def emit_mod(P, cn, c_d, ada_w_d, ada_b_d, col_blocks, row_blocks, ps_col, ps_row):
    siluc = P.sb([128, 8], F32, "siluc")
    P.dma(siluc[:], c_d[0, :].rearrange("(k p) -> p k", p=128), allow_slow_non_contiguous=True)
    P.act(siluc[:], siluc[:], AF.Silu)
    aw = P.sb([128, 8, 1024], F32, "aw_stage")
    cols, rows = {}, {}
    sbc = None
    if row_blocks:
        sbc = P.sb([128, 8, 128], F32, "siluc_bc")
        for kc in range(8):
            P.ts(sbc[:, kc, :], cn.ones_f[:], siluc[:, kc:kc + 1], None, ALU.mult)
    for j in sorted(set(col_blocks) | set(row_blocks)):
        P.dma(aw[:], ada_w_d[:, j * 1024:(j + 1) * 1024].rearrange("(kc p) n -> p kc n", p=128))
        if j in col_blocks:
            ab = P.sb([128, 8], F32, f"adab_c{j}")
            P.dma(ab[:], ada_b_d[0, j * 1024:(j + 1) * 1024].rearrange("(k p) -> p k", p=128), allow_slow_non_contiguous=True)
            for k in range(8):
                for kc in range(8):
                    P.matmul(ps_col[:, k:k + 1], aw[:, kc, k * 128:(k + 1) * 128], siluc[:, kc:kc + 1], start=(kc == 0), stop=(kc == 7))
            t = P.sb([128, 8], F32, f"modc{j}")
            P.tt(t[:], ps_col[:, 0:8], ab[:], ALU.add)
            cols[j] = t
        if j in row_blocks:
            t = P.sb([128, 1024], F32, f"modr{j}")
            P.dma(t[:], ada_b_d[0:1, j * 1024:(j + 1) * 1024].bcast([128, 1024]))
            for half in range(2):
                for kc in range(8):
                    P.matmul(ps_row[:], sbc[:, kc, :], aw[:, kc, half * 512:(half + 1) * 512], start=(kc == 0), stop=(kc == 7))
                P.tt(t[:, half * 512:(half + 1) * 512], t[:, half * 512:(half + 1) * 512], ps_row[:], ALU.add)
            rows[j] = t
    return cols, rows


def load_col(P, src_d_row, n, name):
    t = P.sb([128, n], F32, name)
    P.dma(t[:], src_d_row.rearrange("(k p) -> p k", p=128), allow_slow_non_contiguous=True)
    return t


def mk_Acol(P, normw_col, sc_col, name):
    t = P.sb([128, 8], F32, name)
    P.ts(t[:], sc_col[:], 1.0, None, ALU.add)
    P.tt(t[:], t[:], normw_col[:], ALU.mult)
    return t


class HT:
    def __init__(self, P, cn, ps_t):
        self.P, self.cn, self.ps_t = P, cn, ps_t
        self.ss = P.sb([128, 1], F32, "ht_ss")
        self.xs = P.sb([128, 1024], BF16, "ht_xs")
        self.junk = self.xs

    def emit(self, xt, A_col, sh_col, dst, banks=None):
        P = self.P
        ssv = self.ss[:]
        P.act(self.xs[:], xt, AF.Square, accum_out=ssv)
        P.ts(ssv, ssv, 1.0 / 1024, EPS, ALU.mult, ALU.add)
        rsqrt_inplace(P, ssv)
        P.act(self.xs[:], xt, AF.Copy, scale=ssv)
        for half in range(2):
            pt = (banks or self.ps_t)[half]
            for j in range(4):
                k = half * 4 + j
                P.transpose(pt[:, j * 128:(j + 1) * 128], self.xs[:, k * 128:(k + 1) * 128], self.cn.ident_bf[:])
            for j in range(4):
                k = half * 4 + j
                P.ts(dst(k), pt[:, j * 128:(j + 1) * 128], A_col[:, k:k + 1], sh_col[:, k:k + 1], ALU.mult, ALU.add)


def load_convert(P, dst, src, stage, scale_bc=None, q="sp", eng="pool"):
    P.dma(stage, src, q=q)
    if scale_bc is None:
        P.copy(dst, stage, eng=eng)
    else:
        P.tt(dst, stage, scale_bc, ALU.mult, eng=eng)


class Loader:
    def __init__(self, P, nstage=3):
        self.P = P
        self.stg = [P.sb([128, 1024], F32, f"stg{i}") for i in range(nstage)]
        self.i = 0

    def load(self, dst, src, scale_bc=None, np_=128):
        n = dst.ap.shape[1]
        for c0 in range(0, n, 1024):
            c1 = min(n, c0 + 1024)
            st = self.stg[self.i % len(self.stg)]
            q = "sp" if self.i % 2 == 0 else "act"
            self.i += 1
            self.P.dma(st[0:np_, 0:c1 - c0], src[:, c0:c1], q=q)
            if scale_bc is None:
                self.P.copy(dst[:, c0:c1], st[0:np_, 0:c1 - c0], eng="pool")
            else:
                self.P.tt(dst[:, c0:c1], st[0:np_, 0:c1 - c0], scale_bc[:, c0:c1], ALU.mult, eng="pool")


def build_C(T_c, moe):
    nc = bass.Bass("TRN2", target_bir_lowering=False)
    P = Prog(nc)
    NBLK = T_c // 512
    NT = T_c // 128
    x_d = P.dram("x", [T_c, D], F32, "ExternalInput")
    yT_d = P.dram("yT", [2048, T_c], BF16, "ExternalInput")
    c_d = P.dram("c", [1, D], F32, "ExternalInput")
    ada_w_d = P.dram("ada_w", [D, 6 * D], F32, "ExternalInput")
    ada_b_d = P.dram("ada_b", [1, 6 * D], F32, "ExternalInput")
    nmix_d = P.dram("nmix", [1, D], F32, "ExternalInput")
    nffn_d = P.dram("nffn", [1, D], F32, "ExternalInput")
    wgate_d = P.dram("wgate", [D, 3 * D], F32, "ExternalInput")
    gateb_d = P.dram("gateb", [1, 3 * D], F32, "ExternalInput")
    proj_d = P.dram("proj", [2048, D], F32, "ExternalInput")
    wout_d = P.dram("wout", [D, D], F32, "ExternalInput")
    ssdn_d = P.dram("ssdn", [1, D], F32, "ExternalInput")
    if moe:
        NE, FF, FG = 8, 3584, 4
        routT_d = P.dram("routT", [8, D], F32, "ExternalInput")
    else:
        NE, FF, FG = 1, 2816, 2
    wgu_d = P.dram("wgu", [NE, D, 2 * FF], F32, "ExternalInput")
    wdn_d = P.dram("wdn", [NE, FF, D], F32, "ExternalInput")
    out_d = P.dram("out", [T_c, D], F32, "ExternalOutput")
    xmid_d = P.dram("xmid", [T_c, D], F32, "Internal")
    xmid_r = [Tl(xmid_d.t[i * 128:(i + 1) * 128, :], f"xmid{i}") for i in range(NT)]
    out_r = [Tl(out_d.t[i * 128:(i + 1) * 128, :], f"out{i}") for i in range(NT)]

    cn = mk_consts(P)
    pb = [P.ps([128, 512], F32, f"pb{i}") for i in range(6)]
    pt_bf = [P.ps([128, 1024], BF16, "ptA"), P.ps([128, 1024], BF16, "ptB")]
    ht = HT(P, cn, pt_bf)
    ld = Loader(P)

    gm_bc = P.sb([128, D], F32, "gm_bc")
    gf_bc = P.sb([128, D], F32, "gf_bc")
    if moe:
        Abc = P.sb([128, D], F32, "Abc")
        shbc = P.sb([128, D], F32, "shbc")
    A_m = P.sb([128, 8], F32, "A_m")
    A_f = P.sb([128, 8], F32, "A_f")
    sh_m = P.sb([128, 8], F32, "sh_m")
    sh_f = P.sb([128, 8], F32, "sh_f")
    gateb_c = load_col(P, gateb_d[0, :], 24, "gateb_c")
    ssdn_c = load_col(P, ssdn_d[0, :], 8, "ssdn_c")
    m0 = P.mark()
    cols, rows = emit_mod(P, cn, c_d, ada_w_d, ada_b_d, [0, 1, 3, 4], [2, 5] + ([3, 4] if moe else []), pb[0], pb[1])
    nmix_c = load_col(P, nmix_d[0, :], 8, "nmix_c")
    nffn_c = load_col(P, nffn_d[0, :], 8, "nffn_c")
    P.ts(A_m[:], cols[1][:], 1.0, None, ALU.add)
    P.tt(A_m[:], A_m[:], nmix_c[:], ALU.mult)
    P.ts(A_f[:], cols[4][:], 1.0, None, ALU.add)
    P.tt(A_f[:], A_f[:], nffn_c[:], ALU.mult)
    P.copy(sh_m[:], cols[0][:])
    P.copy(sh_f[:], cols[3][:])
    P.copy(gm_bc[:], rows[2][:])
    P.copy(gf_bc[:], rows[5][:])
    if moe:
        P.dma(Abc[:], nffn_d[0:1, :].bcast([128, D]))
        P.ts(rows[4][:], rows[4][:], 1.0, None, ALU.add)
        P.tt(Abc[:], Abc[:], rows[4][:], ALU.mult)
        P.copy(shbc[:], rows[3][:])
    P.release(m0)

    m1_ = P.mark()
    wg = P.sb([128, 8, 3072], BF16, "wg")
    wp = P.sb([128, 16, 1024], BF16, "wp")
    wo = P.sb([128, 8, 1024], BF16, "wo")
    for k in range(8):
        ld.load(wg[:, k, :], wgate_d[k * 128:(k + 1) * 128, :])
    for k in range(16):
        ld.load(wp[:, k, :], proj_d[k * 128:(k + 1) * 128, :])
    for k in range(8):
        ld.load(wo[:, k, :], wout_d[k * 128:(k + 1) * 128, :], scale_bc=gm_bc)

    xt = [P.sb([128, D], F32, f"xt{i}") for i in range(4)]
    hT = P.sb([128, 8, 512], BF16, "hT")
    ysb = P.sb([128, 16, 512], BF16, "ysb")
    ysq = P.sb([128, 2, 512], BF16, "ysq")
    rs = P.sb([128, 512], F32, "rs")
    sg = [P.sb([128, 512], BF16, f"sg{i}") for i in range(3)]
    tm = [P.sb([128, 512], F32, f"tm{i}") for i in range(2)]
    mT = P.sb([128, 8, 512], BF16, "mT")

    for b in range(NBLK):
        t0 = b * 512
        for i in range(4):
            P.dma(xt[i][:], x_d[t0 + i * 128:t0 + (i + 1) * 128, :], q="sp" if i % 2 == 0 else "act")
        P.dma(ysb[:], yT_d[:, t0:t0 + 512].rearrange("(k p) t -> p k t", p=128), q="pool")
        for i in range(4):
            ht.emit(xt[i][:], A_m, sh_m, lambda k, i=i: hT[:, k, i * 128:(i + 1) * 128])
        for g in range(4):
            for j in range(2):
                ch = 4 + 2 * g + j
                P.tt(ysq[:, j, :], ysb[:, ch, :], ysb[:, ch, :], ALU.mult)
            for j in range(2):
                P.matmul(pb[g % 2][:], cn.ones_bf[:], ysq[:, j, :], start=(j == 0), stop=(j == 1))
            P.ts(rs[:], pb[g % 2][:], 1.0 / 256, EPS, ALU.mult, ALU.add)
            rsqrt_inplace(P, rs[:])
            for j in range(2):
                ch = 4 + 2 * g + j
                P.stt(ysb[:, ch, :], ysb[:, ch, :], ssdn_c[:, 2 * g + j:2 * g + j + 1], rs[:], ALU.mult, ALU.mult)
        for dc in range(8):
            for br in range(3):
                for k in range(8):
                    P.matmul(pb[br][:], wg[:, k, br * 1024 + dc * 128: br * 1024 + (dc + 1) * 128], hT[:, k, :], start=(k == 0), stop=(k == 7))
                P.act(sg[br][:], pb[br][:], AF.Sigmoid, bias=gateb_c[:, br * 8 + dc: br * 8 + dc + 1])
            for br, (k0, k1) in enumerate([(0, 4), (4, 12), (12, 16)]):
                for k in range(k0, k1):
                    P.matmul(pb[3 + br][:], wp[:, k, dc * 128:(dc + 1) * 128], ysb[:, k, :], start=(k == k0), stop=(k == k1 - 1))
            P.tt(tm[0][:], sg[0][:], pb[3][:], ALU.mult)
            P.tt(tm[1][:], sg[1][:], pb[4][:], ALU.mult)
            P.tt(tm[0][:], tm[0][:], tm[1][:], ALU.add)
            P.tt(tm[1][:], sg[2][:], pb[5][:], ALU.mult)
            P.tt(mT[:, dc, :], tm[0][:], tm[1][:], ALU.add)
        for i in range(4):
            for h in range(2):
                pq = pb[(i * 2 + h) % 4]
                for k in range(8):
                    P.matmul(pq[:], mT[:, k, i * 128:(i + 1) * 128], wo[:, k, h * 512:(h + 1) * 512], start=(k == 0), stop=(k == 7))
                P.tt(xt[i][:, h * 512:(h + 1) * 512], xt[i][:, h * 512:(h + 1) * 512], pq[:], ALU.add)
            P.dma(xmid_r[b * 4 + i][:, :], xt[i][:], q="sp" if i % 2 == 0 else "act")
    P.release(m1_)

    h2T = P.sb([128, 8, T_c], BF16, "h2T")
    xall = [P.sb([128, D], F32, f"xall{i}") for i in range(NT)]
    if moe:
        comb = P.sb([128, NT, 8], F32, "comb")
        m2_ = P.mark()
        routbc = [P.sb([128, D], F32, f"routbc{e}") for e in range(8)]
        for e in range(8):
            P.dma(routbc[e][:], routT_d[e:e + 1, :].bcast([128, D]), q="pool")
        h2f = P.sb([128, D], F32, "h2f")
        rjunk = P.sb([128, D], F32, "rjunk")
        lg = P.sb([128, 8], F32, "lg")
        l2 = P.sb([128, 8], F32, "l2")
        mx1 = P.sb([128, 1], F32, "mx1")
        mx2 = P.sb([128, 1], F32, "mx2")
        mk1 = P.sb([128, 8], F32, "mk1")
        mk2 = P.sb([128, 8], F32, "mk2")
        w1 = P.sb([128, 1], F32, "w1")
        w2 = P.sb([128, 1], F32, "w2")
    for i in range(NT):
        xv = xall[i][:]
        P.dma(xv, xmid_r[i][:, :], q="sp" if i % 2 == 0 else "act")
        ht.emit(xv, A_f, sh_f, lambda k, i=i: h2T[:, k, i * 128:(i + 1) * 128])
        if moe:
            P.stt(h2f[:], xv, ht.ss[:], Abc[:], ALU.mult, ALU.mult)
            P.tt(h2f[:], h2f[:], shbc[:], ALU.add)
            for e in range(8):
                P.tt(rjunk[:], h2f[:], routbc[e][:], ALU.mult)
                P.reduce(lg[:, e:e + 1], rjunk[:], ALU.add)
            P.reduce(mx1[:], lg[:], ALU.max)
            P.ts(mk1[:], lg[:], mx1[:], None, ALU.is_equal)
            P.stt(l2[:], mk1[:], -1e30, lg[:], ALU.mult, ALU.add)
            P.reduce(mx2[:], l2[:], ALU.max)
            P.ts(mk2[:], l2[:], mx2[:], None, ALU.is_equal)
            P.tt(w1[:], mx2[:], mx1[:], ALU.subtract)
            P.act(w1[:], w1[:], AF.Exp)
            P.ts(w1[:], w1[:], 1.0, None, ALU.add)
            P.recip(w1[:], w1[:])
            P.ts(w2[:], w1[:], -1.0, 1.0, ALU.mult, ALU.add)
            P.ts(mk1[:], mk1[:], w1[:], None, ALU.mult)
            P.stt(comb[:, i, :], mk2[:], w2[:], mk1[:], ALU.mult, ALU.add)
    if moe:
        P.release(m2_)
    NCH = FF // 128
    NG = NCH // FG
    wgu_a = P.sb([128, 8, 2 * FG * 128], BF16, "wgu_a")
    wdn_a = P.sb([128, FG, 1024], BF16, "wdn_a")
    actT = P.sb([128, FG, 512], BF16, "actT")
    sl = P.sb([128, 512], F32, "sl")
    for e in range(NE):
        for gi in range(NG):
            f0 = gi * FG * 128
            for k in range(8):
                ld.load(wgu_a[:, k, 0:FG * 128], wgu_d[e, k * 128:(k + 1) * 128, f0:f0 + FG * 128])
                ld.load(wgu_a[:, k, FG * 128:2 * FG * 128], wgu_d[e, k * 128:(k + 1) * 128, FF + f0:FF + f0 + FG * 128])
            for j in range(FG):
                ld.load(wdn_a[:, j, :], wdn_d[e, f0 + j * 128:f0 + (j + 1) * 128, :], scale_bc=gf_bc)
            for b in range(NBLK):
                t0 = b * 512
                for j in range(FG):
                    for k in range(8):
                        P.matmul(pb[0][:], wgu_a[:, k, j * 128:(j + 1) * 128], h2T[:, k, t0:t0 + 512], start=(k == 0), stop=(k == 7))
                    for k in range(8):
                        P.matmul(pb[1][:], wgu_a[:, k, FG * 128 + j * 128:FG * 128 + (j + 1) * 128], h2T[:, k, t0:t0 + 512], start=(k == 0), stop=(k == 7))
                    P.act(sl[:], pb[0][:], AF.Silu)
                    P.tt(actT[:, j, :], sl[:], pb[1][:], ALU.mult)
                for i in range(4):
                    ti = b * 4 + i
                    for h in range(2):
                        pp = pb[2 + (i * 2 + h) % 4]
                        for j in range(FG):
                            P.matmul(pp[:], actT[:, j, i * 128:(i + 1) * 128], wdn_a[:, j, h * 512:(h + 1) * 512], start=(j == 0), stop=(j == FG - 1))
                        xs_ = xall[ti][:, h * 512:(h + 1) * 512]
                        if moe:
                            P.stt(xs_, pp[:], comb[:, ti, e:e + 1], xs_, ALU.mult, ALU.add)
                        else:
                            P.tt(xs_, xs_, pp[:], ALU.add)
    for i in range(NT):
        P.dma(out_r[i][:, :], xall[i][:], q="sp" if i % 2 == 0 else "act")
    print("C peak sbuf", P.peak, "ops", len(P.ops))
    return P.emit()


GN_EPS = 64e-5
C_R, C_K, C_V, C_WLO, C_ALO, C_GLO = 0, 64, 128, 192, 256, 320
C_Z, C_X, C_B, C_C = 480, 608, 736, 864
C_FQ, C_FK, C_FV = 992, 1056, 1120
C_DT, C_F = 1184, 1186
C_VD = 1187
NW0, NW1 = 1187, 1219
PP_MU = 0
PP_W0, PP_A0, PP_KK, PP_KA, PP_RK, PP_V0 = 7, 8, 9, 10, 11, 12
PP_CW = 13
PP_CB = 29
PP_QG, PP_KG = 33, 34
PP_D = 35
PP_SEL = 37
NPP = 40
BC_LNW, BC_LNB, BC_DTB, BC_ALOG, BC_FB = 0, 64, 128, 130, 132
NBC = 133
MK_BIG, MK_SL64, MK_TRI, MK_SLF, MK_CHUNK, MK_NEG, MK_SEL = 0, 1, 2, 3, 4, 5, 6
NMASK = 7


def make_masks():
    p = np.arange(128)[:, None]
    j = np.arange(128)[None, :]
    m = np.zeros((128, NMASK, 128), np.float32)
    pl, jl = p % 64, j % 64
    m[:, MK_BIG, :] = np.where(j < 64, jl > pl, jl >= pl)
    m[:, MK_SL64, :] = (j < p) & (p < 64) & (j < 64)
    m[:, MK_TRI, :] = j >= p
    m[:, MK_SLF, :] = p > j
    m[:, MK_CHUNK, :] = (j >= p) & ((j // 64) == (p // 64))
    m[:, MK_NEG, :] = np.where(p > j, -30000.0, 0.0)
    m[64, MK_SEL, :] = 1.0
    return m


import os
RW_STOP = int(os.environ.get('RW_STOP', '99'))


class RWMixer:
    def __init__(self, st):
        self.st = st
        P = st.P
        self.cur = {}
        for name, rows in [("r", 64), ("k", 64), ("v", 64), ("wlo", 64), ("alo", 64), ("gloa", 128), ("glob", 32)]:
            t = P.sb([rows, 513], F32, f"rw_{name}")
            P.memset(t[:, 0:1], 0.0)
            self.cur[name] = t
        self.S = P.sb([64, 64], F32, "rw_S")
        self.Sb = P.sb([64, 64], BF16, "rw_Sb")
        P.memset(self.S[:], 0.0)
        P.memset(self.Sb[:], 0.0)
        self.negw0 = P.sb([64, 1], F32, "rw_negw0")
        self.omka = P.sb([64, 1], F32, "rw_omka")
        pp = st.pp
        P.ts(self.negw0[:], pp[0:64, PP_W0:PP_W0 + 1], -1.0, None, ALU.mult)
        P.ts(self.omka[:], pp[0:64, PP_KA:PP_KA + 1], -1.0, 1.0, ALU.mult, ALU.add)

    def alloc(self):
        st = self.st
        P = st.P
        f = lambda n, r=64, c=512, dt=F32: P.sb([r, c], dt, "rw_" + n)
        self.tmp128 = f("tmp128", 128)
        self.tmp = self.tmp128[0:64, :]
        self.r = f("sr")
        self.k = f("sk")
        self.v = f("sv")
        self.twl = f("twl", dt=BF16)
        self.alo = f("salo", dt=BF16)
        self.sga = f("sga", 128, dt=BF16)
        self.sgb = f("sgb", 32, dt=BF16)
        self.logw = f("logw")
        self.a = f("a")
        self.kk = f("kk")
        self.sq = f("sq", dt=BF16)
        self.k2 = f("k2")
        self.bv = f("bv")
        self.rk = f("rk", dt=BF16)
        self.cl = f("cl")
        self.G = f("G")
        self.E2 = f("E2")
        self.lwT = P.sb([128, 64], F32, "rw_lwT")
        self.AR = P.sb([64, 8, 128], BF16, "rw_AR")
        self.BK = P.sb([64, 8, 128], BF16, "rw_BK")
        self.BKh = P.sb([64, 8, 128], BF16, "rw_BKh")
        self.VV = P.sb([64, 8, 64], BF16, "rw_VV")
        self.MBb = P.sb([64, 8, 64], BF16, "rw_MBb")
        self.MKb = P.sb([64, 8, 128], BF16, "rw_MKb")
        self.Pm = [P.sb([64, 8, 128], F32, f"rw_Pm{i}") for i in range(2)]
        self.TT = P.sb([64, 8, 128], F32, "rw_TT")
        self.Utok = P.sb([64, 8, 64], BF16, "rw_Utok")
        self.Vtok = P.sb([64, 8, 64], BF16, "rw_Vtok")
        self.BKhT = P.sb([64, 8, 128], BF16, "rw_BKhT")
        self.Wf = P.sb([64, 64], F32, "rw_Wf")
        self.yc = P.sb([64, 8, 64], F32, "rw_yc")
        self.yq = P.sb([64, 8, 64], F32, "rw_yq")
        self.st8 = [P.sb([64, 8], F32, f"rw_st{i}") for i in range(3)]
        self.yo = P.sb([64, 8, 64], BF16, "rw_yo")
        self.yTs = P.sb([64, 512], BF16, "rw_yTs")
        if st.layer == 1:
            self.hv = self.sq[0:32, :]
            self.vf = self.E2[:, :]

    def block(self, sb, filler=None):
        st = self.st
        P, pb, pp, mk, cn = st.P, st.pb, st.pp, st.mk, st.cn
        t0 = sb * 512
        cur = self.cur
        mrk = P.mark()
        self.alloc()
        groups = [("r", C_R, 64), ("k", C_K, 64), ("v", C_V, 64), ("wlo", C_WLO, 64), ("alo", C_ALO, 64),
                  ("gloa", C_GLO, 128), ("glob", C_GLO + 128, 32)]
        for gi, (name, c0, n) in enumerate(groups):
            st.project(c0, n, cur[name][:, 1:513], pb[gi % 2], evac="act" if gi % 2 == 0 else "dve")
        if st.layer == 1:
            st.project(C_VD, 32, self.hv[:], pb[1], evac="act")
            P.dma(self.vf[:], st.vf_d[:, t0:t0 + 512], q="pool")

        def shift(name, mucol, rows, out, func=None):
            c = cur[name]
            tmpv = self.tmp128[0:rows, :]
            P.tt(tmpv, c[:, 0:512], c[:, 1:513], ALU.subtract)
            if func is None:
                P.stt(out, tmpv, pp[0:rows, mucol:mucol + 1], c[:, 1:513], ALU.mult, ALU.add)
            else:
                P.stt(tmpv, tmpv, pp[0:rows, mucol:mucol + 1], c[:, 1:513], ALU.mult, ALU.add)
                P.act(out, tmpv, func)
            P.copy(c[:, 0:1], c[:, 512:513], eng="pool")

        shift("r", PP_MU + 0, 64, self.r[:])
        shift("k", PP_MU + 1, 64, self.k[:])
        shift("v", PP_MU + 2, 64, self.v[:])
        shift("wlo", PP_MU + 3, 64, self.twl[:], AF.Tanh)
        shift("alo", PP_MU + 4, 64, self.alo[:], AF.Copy)
        shift("gloa", PP_MU + 5, 128, self.sga[:], AF.Sigmoid)
        shift("glob", PP_MU + 6, 32, self.sgb[:], AF.Sigmoid)
        if RW_STOP == 1:
            P.release(mrk)
            return
        P.matmul(pb[0][0:64, :], st.wup[:, :], self.twl[:], start=True, stop=True)
        P.act(self.logw[:], pb[0][0:64, :], AF.Exp, scale=-1.0, bias=self.negw0[:])
        P.ts(self.logw[:], self.logw[:], 1.0, None, ALU.add)
        P.recip(self.logw[:], self.logw[:])
        P.ts(self.logw[:], self.logw[:], -float(np.exp(-0.5)), None, ALU.mult)
        P.matmul(pb[1][0:64, :], st.aup[:, :], self.alo[:], start=True, stop=True)
        P.act(self.a[:], pb[1][0:64, :], AF.Sigmoid, bias=pp[0:64, PP_A0:PP_A0 + 1])
        if st.layer == 1:
            P.matmul(pb[0][0:64, :], st.vup[:, :], self.hv[:], start=True, stop=True)
            P.act(self.tmp[:], pb[0][0:64, :], AF.Sigmoid, bias=pp[0:64, PP_V0:PP_V0 + 1])
            P.tt(self.vf[:], self.vf[:], self.v[:], ALU.subtract)
            P.tt(self.vf[:], self.vf[:], self.tmp[:], ALU.mult)
            P.tt(self.v[:], self.v[:], self.vf[:], ALU.add)
        else:
            P.dma(st.vfo_d[:, t0:t0 + 512], self.v[:], q="pool")
        P.ts(self.kk[:], self.k[:], pp[0:64, PP_KK:PP_KK + 1], None, ALU.mult)
        P.tt(self.sq[:], self.kk[:], self.kk[:], ALU.mult)
        P.matmul(pb[1][0:64, :], cn.ones_bf[0:64, 0:64], self.sq[:], start=True, stop=True)
        P.ts(self.tmp[:], pb[1][0:64, :], 1e-24, None, ALU.add)
        rsqrt_inplace(P, self.tmp[:])
        P.tt(self.kk[:], self.kk[:], self.tmp[:], ALU.mult)
        P.ts(self.tmp[:], self.a[:], pp[0:64, PP_KA:PP_KA + 1], self.omka[:], ALU.mult, ALU.add)
        P.tt(self.k2[:], self.k[:], self.tmp[:], ALU.mult)
        P.tt(self.bv[:], self.kk[:], self.a[:], ALU.mult)
        P.stt(self.rk[:], self.r[:], pp[0:64, PP_RK:PP_RK + 1], self.k2[:], ALU.mult, ALU.mult)
        if RW_STOP == 2:
            P.release(mrk)
            return
        for j in range(4):
            P.transpose(pb[0][:, 0:64], self.logw[:, j * 128:(j + 1) * 128], cn.ident_f[0:64, 0:64])
            P.copy(self.lwT[:], pb[0][:, 0:64])
            P.matmul(pb[1][0:64, j * 128:(j + 1) * 128], self.lwT[:], mk[:, MK_CHUNK, :], start=True, stop=True)
        P.copy(self.cl[:], pb[1][0:64, :])
        if RW_STOP == 3:
            P.release(mrk)
            return
        P.act(self.G[:], self.cl[:], AF.Exp)
        P.tt(self.AR[:, :, 64:128], self.r[:].c3(), self.G[:].c3(), ALU.mult)
        P.tt(self.tmp[:], self.cl[:], self.logw[:], ALU.subtract)
        P.act(self.E2[:], self.tmp[:], AF.Exp)
        P.stt(self.AR[:, :, 0:64], self.kk[:].c3(), -1.0, self.E2[:].c3(), ALU.mult, ALU.mult)
        P.act(self.E2[:], self.cl[:], AF.Exp, scale=-1.0)
        P.tt(self.BK[:, :, 0:64], self.bv[:].c3(), self.E2[:].c3(), ALU.mult)
        P.tt(self.BK[:, :, 64:128], self.k2[:].c3(), self.E2[:].c3(), ALU.mult)
        for c in range(8):
            P.act(self.E2[:, c * 64:(c + 1) * 64], self.cl[:, c * 64:(c + 1) * 64], AF.Exp, scale=-1.0, bias=self.cl[:, c * 64 + 63:c * 64 + 64])
        P.tt(self.BKh[:, :, 0:64], self.bv[:].c3(), self.E2[:].c3(), ALU.mult)
        P.tt(self.BKh[:, :, 64:128], self.k2[:].c3(), self.E2[:].c3(), ALU.mult)
        P.copy(self.VV[:, :, 0:64], self.v[:].c3(), eng="pool")
        if RW_STOP == 4:
            P.release(mrk)
            return
        for c in range(8):
            o = (c % 4) * 128
            P.matmul(pb[2 + c // 4][0:64, o:o + 128], self.BK[:, c, 0:64], self.AR[:, c, :], start=True, stop=True)
        for c in range(8):
            o = (c % 4) * 128
            P.matmul(pb[4 + c // 4][0:64, o:o + 128], self.BK[:, c, 64:128], self.AR[:, c, :], start=True, stop=True)
        Pm, TT = self.Pm, self.TT
        mbig = mk[0:64, MK_BIG, :]
        for hb in range(2):
            src = pb[2 + hb][0:64, :].c3(128)
            P.tt(Pm[0][:, hb * 4:(hb + 1) * 4, 0:64], src[:, :, 0:64], mbig[:, 0:64].bmid(4), ALU.mult)
            P.tt(self.MBb[:, hb * 4:(hb + 1) * 4, :], src[:, :, 64:128], mbig[:, 64:128].bmid(4), ALU.mult)
            P.tt(self.MKb[:, hb * 4:(hb + 1) * 4, :], pb[4 + hb][0:64, :].c3(128), mbig.bmid(4), ALU.mult)
        for c in range(8):
            P.matmul(pb[2][0:64, c * 64:(c + 1) * 64], self.AR[:, c, 0:64], self.BK[:, c, 0:64], start=True, stop=True)
        P.tt(Pm[0][:, :, 64:128], pb[2][0:64, :].c3(), mk[0:64, MK_SL64, 0:64].bmid(8), ALU.mult)
        for hf in range(2):
            P.tt(TT[:, :, hf * 64:(hf + 1) * 64], Pm[0][:, :, hf * 64:(hf + 1) * 64], cn.ident_f[0:64, 0:64].bmid(8), ALU.add)
        if RW_STOP == 5:
            P.release(mrk)
            return
        for c in range(8):
            P.transpose(st.pt_bf[0][0:64, c * 64:(c + 1) * 64], self.VV[:, c, 0:64], cn.ident_bf[0:64, 0:64])
        P.copy(self.Vtok[:], st.pt_bf[0][0:64, 0:512].c3())
        for c in range(8):
            for hf in range(2):
                P.transpose(st.pt_bf[1][0:64, c * 128 + hf * 64:c * 128 + (hf + 1) * 64], self.BKh[:, c, hf * 64:(hf + 1) * 64], cn.ident_bf[0:64, 0:64])
        P.copy(self.BKhT[:], st.pt_bf[1][0:64, :].c3(128))
        if RW_STOP == 6:
            P.release(mrk)
            return
        cu = 0
        for rnd in range(5):
            last = rnd == 4
            Pc, Pn = Pm[cu], Pm[1 - cu]
            for c in range(8):
                pbk = pb[2 + c // 4]
                o = (c % 4) * 128
                P.matmul(pbk[0:64, o:o + 64], Pc[:, c, 64:128], Pc[:, c, 0:64], start=True, stop=True)
                if not last:
                    P.matmul(pbk[0:64, o + 64:o + 128], Pc[:, c, 0:64], Pc[:, c, 64:128], start=True, stop=True)
            for hb in range(2):
                if last:
                    P.copy(Pn[:, hb * 4:(hb + 1) * 4, 0:64], pb[2 + hb][0:64, :].c3(128)[:, :, 0:64])
                else:
                    P.copy(Pn[:, hb * 4:(hb + 1) * 4, :], pb[2 + hb][0:64, :].c3(128))
            for c in range(8):
                pbk = pb[4 + c // 4]
                o = (c % 4) * 128
                P.matmul(pbk[0:64, o:o + 64], TT[:, c, 64:128], Pn[:, c, 0:64], start=True, stop=True)
                if not last:
                    P.matmul(pbk[0:64, o + 64:o + 128], TT[:, c, 0:64], Pn[:, c, 64:128], start=True, stop=True)
            for hb in range(2):
                if last:
                    P.tt(TT[:, hb * 4:(hb + 1) * 4, 0:64], TT[:, hb * 4:(hb + 1) * 4, 0:64], pb[4 + hb][0:64, :].c3(128)[:, :, 0:64], ALU.add)
                else:
                    P.tt(TT[:, hb * 4:(hb + 1) * 4, :], TT[:, hb * 4:(hb + 1) * 4, :], pb[4 + hb][0:64, :].c3(128), ALU.add)
            cu = 1 - cu
        if RW_STOP == 7:
            P.release(mrk)
            return
        pw = st.pt_bf[1][:, :].bitcast(F32)
        py = st.pt_bf[0][:, :].bitcast(F32)
        fill = (lambda: filler(0.0)) if filler is not None else (lambda: None)
        for c in range(8):
            P.matmul(pw[0:64, 0:64], self.AR[:, c, 0:64], self.Sb[:, :], start=True, stop=False)
            P.matmul(pw[0:64, 0:64], self.MKb[:, c, 0:64], self.Vtok[:, c, :], start=False, stop=True)
            fill()
            P.copy(self.Wf[:], pw[0:64, 0:64])
            P.matmul(pw[0:64, 64:128], TT[:, c, 0:64], self.Wf[:], start=True, stop=True)
            fill()
            P.copy(self.Utok[:, c, :], pw[0:64, 64:128])
            yreg = py[0:64, c * 64:(c + 1) * 64]
            P.matmul(yreg, self.AR[:, c, 64:128], self.Sb[:, :], start=True, stop=False)
            P.matmul(yreg, self.MBb[:, c, :], self.Utok[:, c, :], start=False, stop=False)
            P.matmul(yreg, self.MKb[:, c, 64:128], self.Vtok[:, c, :], start=False, stop=True)
            P.matmul(pw[0:64, 128:192], self.BKhT[:, c, 0:64], self.Utok[:, c, :], start=True, stop=False)
            P.matmul(pw[0:64, 128:192], self.BKhT[:, c, 64:128], self.Vtok[:, c, :], start=False, stop=True)
            fill()
            P.stt(self.S[:], self.S[:], self.G[:, c * 64 + 63:c * 64 + 64], pw[0:64, 128:192], ALU.mult, ALU.add)
            P.copy(self.Sb[:], self.S[:], eng="pool")
        if RW_STOP == 8:
            P.release(mrk)
            return
        yall = py[0:64, :].c3()
        s1, s2, bon = self.st8
        P.reduce(s1[:], yall, ALU.add)
        P.ts(s1[:], s1[:], -1.0 / 64, None, ALU.mult)
        P.tt(self.yc[:], yall, s1[:].binner(64), ALU.add)
        P.tt(self.yq[:], self.yc[:], self.yc[:], ALU.mult)
        P.reduce(s2[:], self.yq[:], ALU.add)
        P.ts(s2[:], s2[:], 1.0 / 64, GN_EPS, ALU.mult, ALU.add)
        rsqrt_inplace(P, s2[:])
        P.tt(self.yc[:], self.yc[:], s2[:].binner(64), ALU.mult)
        P.tt(self.yc[:], self.yc[:], st.pbc[0:64, BC_LNW:BC_LNW + 64].bmid(8), ALU.mult)
        P.tt(self.yc[:], self.yc[:], st.pbc[0:64, BC_LNB:BC_LNB + 64].bmid(8), ALU.add)
        for c in range(8):
            P.matmul(pw[0:64, 256 + c:257 + c], self.rk[:, c * 64:(c + 1) * 64], cn.ones_bf[0:64, 0:1], start=True, stop=True)
        P.copy(bon[:], pw[0:64, 256:264])
        P.tt(self.yq[:], self.Vtok[:], bon[:].binner(64), ALU.mult)
        P.tt(self.yc[:], self.yc[:], self.yq[:], ALU.add)
        for c in range(8):
            P.matmul(pw[0:64, c * 64:(c + 1) * 64], self.sga[:, c * 64:(c + 1) * 64], st.gupa[:, :], start=True, stop=False)
            P.matmul(pw[0:64, c * 64:(c + 1) * 64], self.sgb[:, c * 64:(c + 1) * 64], st.gupb[:, :], start=False, stop=True)
        fill()
        P.tt(self.yo[:], self.yc[:], pw[0:64, :].c3(), ALU.mult)
        for c in range(8):
            P.transpose(st.pt_bf[0][0:64, c * 64:(c + 1) * 64], self.yo[:, c, :], cn.ident_bf[0:64, 0:64])
        P.copy(self.yTs[:], st.pt_bf[0][0:64, 0:512])
        P.dma(st.yT_d[0:64, t0:t0 + 512], self.yTs[:], q="pool")
        P.release(mrk)


class SSDMixer:
    def __init__(self, st):
        self.st = st
        P = st.P
        self.xh = [P.sb([64, 515], F32, f"ssd_x{e}") for e in range(2)]
        self.Bh = P.sb([128, 515], F32, "ssd_B")
        self.Ch = P.sb([128, 515], F32, "ssd_C")
        for t in self.xh + [self.Bh, self.Ch]:
            P.memset(t[:, 0:3], 0.0)
        self.S = [P.sb([128, 64], F32, f"ssd_S{e}") for e in range(2)]
        self.Sb = [P.sb([128, 64], BF16, f"ssd_Sb{e}") for e in range(2)]
        for t in self.S + self.Sb:
            P.memset(t[:], 0.0)
        self.Arow = P.sb([128, 2], F32, "ssd_Arow")
        P.act(self.Arow[:], st.pbc[:, BC_ALOG:BC_ALOG + 2], AF.Exp)
        P.ts(self.Arow[:], self.Arow[:], -1.0, None, ALU.mult)

    def block(self, sb):
        st = self.st
        P, pb, pp, mk, cn = st.P, st.pb, st.pp, st.mk, st.cn
        t0 = sb * 512
        mrk = P.mark()
        f = lambda n, r=128, c=512, dt=F32: P.sb([r, c], dt, "ssd_" + n)
        z = [f(f"z{e}", 64) for e in range(2)]
        acc = f("acc")
        xs = [f(f"xs{e}", 64) for e in range(2)]
        xsb = [f(f"xsb{e}", 64, dt=BF16) for e in range(2)]
        Bc = f("Bc", dt=BF16)
        Cc = f("Cc")
        dtt = P.sb([128, 4, 4], F32, "ssd_dtt")
        at = P.sb([128, 4, 2], F32, "ssd_at")
        E = f("E", c=512)
        l1 = [f(f"l1{e}", c=128) for e in range(2)]
        l2 = [f(f"l2{e}", c=128) for e in range(2)]
        CBm = f("CBm", c=128)
        Gt = [f(f"Gt{e}", c=128, dt=BF16) for e in range(2)]
        Cst = [f(f"Cst{e}", c=128, dt=BF16) for e in range(2)]
        dq = P.sb([128, 4], F32, "ssd_dq")
        xdt = [P.sb([128, 64], BF16, f"ssd_xdt{e}") for e in range(2)]
        Bw = [f(f"Bw{e}", c=128, dt=BF16) for e in range(2)]
        yo = [f(f"yo{e}", 64, dt=BF16) for e in range(2)]
        yv = f("yv", 64)
        for e in range(2):
            st.project(C_Z + e * 64, 64, z[e][:], pb[e], evac="act")
        for e in range(2):
            st.project(C_X + e * 64, 64, self.xh[e][:, 3:515], pb[e], evac="dve")
        st.project(C_B, 128, self.Bh[:, 3:515], pb[0], evac="act")
        st.project(C_C, 128, self.Ch[:, 3:515], pb[1], evac="dve")
        for j in range(4):
            for k in range(8):
                P.matmul(pb[2][:, j * 4:j * 4 + 3], st.hT[:, k, j * 128:(j + 1) * 128], st.wB[:, k, C_DT:C_DT + 3], start=(k == 0), stop=(k == 7))
        P.copy(dtt[:, :, 0:3], pb[2][:, 0:16].c3(4)[:, :, 0:3])
        st.fraw = dtt
        P.tt(at[:], dtt[:, :, 0:2], st.pbc[:, BC_DTB:BC_DTB + 2].bmid(4), ALU.add)
        P.act(at[:], at[:], AF.Exp)
        P.act(dtt[:, :, 0:2], at[:], AF.Ln, bias=1.0)
        P.tt(at[:], dtt[:, :, 0:2], self.Arow[:].bmid(4), ALU.mult)
        def conv(tile, rows, wc, bc, out, out2=None):
            a_ = acc[0:rows, :]
            P.ts(a_, tile[:, 0:512], pp[0:rows, wc:wc + 1], None, ALU.mult)
            for k in range(1, 4):
                P.stt(a_, tile[:, k:k + 512], pp[0:rows, wc + k:wc + k + 1], a_, ALU.mult, ALU.add)
            P.act(out, a_, AF.Silu, bias=pp[0:rows, bc:bc + 1])
            if out2 is not None:
                P.copy(out2, out, eng="pool")
            P.copy(tile[:, 0:3], tile[:, 512:515], eng="pool")
        for e in range(2):
            conv(self.xh[e], 64, PP_CW + 4 * e, PP_CB + e, xs[e][:], xsb[e][:])
        conv(self.Bh, 128, PP_CW + 8, PP_CB + 2, Bc[:])
        conv(self.Ch, 128, PP_CW + 12, PP_CB + 3, Cc[:])
        Ccb = f("Ccb", dt=BF16)
        P.copy(Ccb[:], Cc[:], eng="pool")
        Gt2 = [[P.sb([128, 128], BF16, f"ssd_Gt{p}{e}") for e in range(2)] for p in range(2)]
        Cst2 = [[P.sb([128, 128], BF16, f"ssd_Cst{p}{e}") for e in range(2)] for p in range(2)]
        xdt2 = [[P.sb([128, 64], BF16, f"ssd_xdt{p}{e}") for e in range(2)] for p in range(2)]
        Bw2 = [[P.sb([128, 128], BF16, f"ssd_Bw{p}{e}") for e in range(2)] for p in range(2)]
        dq2 = [P.sb([128, 4], F32, f"ssd_dq{p}") for p in range(2)]

        def front(j):
            p = j % 2
            cs = slice(j * 128, (j + 1) * 128)
            P.matmul(pb[2][:, 0:128], Bc[:, cs], Ccb[:, cs], start=True, stop=True)
            P.tt(CBm[:], pb[2][:, 0:128], mk[:, MK_TRI, :], ALU.mult)
            P.transpose(st.pt_bf[1][:, 0:128], Bc[:, cs], cn.ident_bf[:])
            for e in range(2):
                acol = at[:, j, e:e + 1]
                P.ts(l1[e][:], mk[:, MK_SLF, :], acol, None, ALU.mult)
                P.ts(l2[e][:], cn.ones_f[:], acol, None, ALU.mult)
                P.matmul(pb[3][:, e * 256:e * 256 + 128], l1[e][:], mk[:, MK_TRI, :], start=True, stop=True)
                P.matmul(pb[3][:, e * 256 + 128:e * 256 + 256], l2[e][:], mk[:, MK_TRI, :], start=True, stop=True)
                P.matmul(pb[2][:, 128 + 2 * e:129 + 2 * e], mk[:, MK_SLF, :], acol, start=True, stop=True)
                P.matmul(pb[2][:, 129 + 2 * e:130 + 2 * e], cn.ones_f[:], acol, start=True, stop=True)
            P.act(E[:], pb[3][:, :], AF.Exp)
            P.act(dq2[p][:], pb[2][:, 128:132], AF.Exp)
            for e in range(2):
                P.tt(Gt2[p][e][:], E[:, e * 256:e * 256 + 128], CBm[:], ALU.mult)
                P.tt(Cst2[p][e][:], E[:, e * 256 + 128:e * 256 + 256], Cc[:, cs], ALU.mult)
                P.transpose(st.pt_bf[0][:, e * 64:(e + 1) * 64], xsb[e][:, cs], cn.ident_bf[0:64, 0:64])
                P.ts(xdt2[p][e][:], st.pt_bf[0][:, e * 64:(e + 1) * 64], dtt[:, j, e:e + 1], None, ALU.mult)
                P.ts(Bw2[p][e][:], st.pt_bf[1][:, 0:128], dq2[p][:, 2 * e:2 * e + 1], None, ALU.mult)

        def back(j):
            p = j % 2
            cs = slice(j * 128, (j + 1) * 128)
            for e in range(2):
                yreg = pb[4 + e][0:64, cs]
                P.matmul(yreg, xdt2[p][e][:], Gt2[p][e][:], start=True, stop=False)
                P.matmul(yreg, self.Sb[e][:], Cst2[p][e][:], start=False, stop=True)
                sreg = pb[e][:, 0:64]
                P.matmul(sreg, Bw2[p][e][:], xdt2[p][e][:], start=True, stop=True)
                P.stt(self.S[e][:], self.S[e][:], dq2[p][:, 2 * e + 1:2 * e + 2], sreg, ALU.mult, ALU.add)
                P.copy(self.Sb[e][:], self.S[e][:], eng="act")

        front(0)
        for j in range(4):
            if j + 1 < 4:
                front(j + 1)
            back(j)
        for e in range(2):
            P.act(z[e][:], z[e][:], AF.Silu)
            P.stt(yv[:], xs[e][:], pp[0:64, PP_D + e:PP_D + e + 1], pb[4 + e][0:64, :], ALU.mult, ALU.add)
            P.tt(yo[e][:], yv[:], z[e][:], ALU.mult)
            P.dma(st.yT_d[64 + e * 64:64 + (e + 1) * 64, t0:t0 + 512], yo[e][:], q="pool")
        P.release(mrk)


class FoxMixer:
    def __init__(self, st):
        self.st = st
        P = st.P
        L = st.L
        NB = L // 128
        self.KT = P.sb([67, L], BF16, "fox_KT")
        P.memset(self.KT[64:67, :], 1.0)
        self.Vaug = P.sb([128, NB, 65], BF16, "fox_V")
        P.memset(self.Vaug[:, :, 64:65], 1.0)
        self.negF = P.sb([128, NB], F32, "fox_negF")
        self.totb = P.sb([128, 1], F32, "fox_totb")
        P.memset(self.totb[:], 0.0)
        self.negfb = P.sb([128, 1], F32, "fox_negfb")
        P.ts(self.negfb[:], st.pbc[:, BC_FB:BC_FB + 1], -1.0, None, ALU.mult)
        self.qg = P.sb([64, 1], F32, "fox_qg")
        P.ts(self.qg[:], st.pp[0:64, PP_QG:PP_QG + 1], 0.125, None, ALU.mult)
        self.nmb = P.sb([128, 128], BF16, "fox_nmb")
        P.copy(self.nmb[:], st.mk[:, MK_NEG, :])
        self.nlw = P.sb([128, 4, 67], F32, "fox_nlw")
        P.memset(self.nlw[:], 0.0)
        self.pT = [P.sb([128, 512], BF16, f"fox_pT{i}") for i in range(4)]
        self.QT = P.sb([67, 512], BF16, "fox_QT")
        self.o = P.sb([65, 512], F32, "fox_o")
        self.rec = P.sb([64, 512], F32, "fox_rec")
        self.yo = P.sb([64, 512], BF16, "fox_yo")

    def prep(self, sb):
        st = self.st
        P, pb, pp, mk, cn = st.P, st.pb, st.pp, st.mk, st.cn
        t0 = sb * 512
        mrk = P.mark()
        f = lambda n, r=64, c=512, dt=F32: P.sb([r, c], dt, "fox_" + n)
        q, k, v = f("q"), f("k"), f("v")
        sq = f("sq", dt=BF16)
        rs = f("rs")
        vb = f("vb", dt=BF16)
        QT = self.QT
        nl = P.sb([128, 4], F32, "fox_nl")
        totc = P.sb([128, 4], F32, "fox_totc")
        tt4 = P.sb([128, 8], F32, "fox_tt4")
        Fv = f("Fv", 67)
        r1 = f("r1", 67)
        hf = f("hf", 67)
        hb = f("hb", 67, dt=BF16)
        ta = f("ta", 67)
        st.project(C_FQ, 64, q[:], pb[2], evac="act")
        st.project(C_FK, 64, k[:], pb[3], evac="dve")
        st.project(C_FV, 64, v[:], pb[2], evac="act")
        for j in range(4):
            for kk in range(8):
                P.matmul(pb[3][:, j:j + 1], st.hT[:, kk, j * 128:(j + 1) * 128], st.wB[:, kk, C_F:C_F + 1], start=(kk == 0), stop=(kk == 7))
        P.act(nl[:], pb[3][:, 0:4], AF.Exp, scale=-1.0, bias=self.negfb[:])
        P.act(nl[:], nl[:], AF.Ln, bias=1.0)
        P.matmul(pb[3][:, 8:12], cn.ones_f[:], nl[:], start=True, stop=True)
        P.matmul(pb[3][:, 12:16], mk[:, MK_TRI, :], nl[:], start=True, stop=True)
        P.copy(tt4[:, 0:8], pb[3][:, 8:16])
        P.copy(totc[:, 0:1], self.totb[:])
        for j in range(1, 4):
            P.tt(totc[:, j:j + 1], totc[:, j - 1:j], tt4[:, j - 1:j], ALU.add)
        P.tt(self.totb[:], totc[:, 3:4], tt4[:, 3:4], ALU.add)
        P.tt(self.negF[:, 4 * sb:4 * sb + 4], tt4[:, 4:8], totc[:], ALU.add)
        for j in range(4):
            P.copy(self.nlw[:, j, 64:67], nl[:, j:j + 1].bcast([128, 3]))
        for j in range(4):
            P.matmul(pb[2][0:67, j * 128:(j + 1) * 128], self.nlw[:, j, :], mk[:, MK_TRI, :], start=True, stop=True)
        for j in range(4):
            cs = slice(j * 128, (j + 1) * 128)
            P.ts(Fv[64:67, cs], pb[2][64:67, cs], totc[64:67, j:j + 1], -1.0, ALU.add, ALU.mult)
        R = slice(64, 67)
        P.copy(hb[R, :], Fv[R, :])
        P.copy(hf[R, :], hb[R, :])
        P.ts(ta[R, :], hf[R, :], pp[R, PP_SEL:PP_SEL + 1], None, ALU.mult)
        P.tt(r1[R, :], Fv[R, :], hf[R, :], ALU.subtract)
        P.copy(hb[R, :], r1[R, :])
        P.copy(hf[R, :], hb[R, :])
        P.stt(ta[R, :], hf[R, :], pp[R, PP_SEL + 1:PP_SEL + 2], ta[R, :], ALU.mult, ALU.add)
        P.tt(r1[R, :], r1[R, :], hf[R, :], ALU.subtract)
        P.copy(hb[R, :], r1[R, :])
        P.copy(hf[R, :], hb[R, :])
        P.stt(QT[R, :], hf[R, :], pp[R, PP_SEL + 2:PP_SEL + 3], ta[R, :], ALU.mult, ALU.add)
        for src, gcol, dst in ((q, self.qg[:], QT[0:64, :]), (k, pp[0:64, PP_KG:PP_KG + 1], self.KT[0:64, t0:t0 + 512])):
            P.tt(sq[:], src[:], src[:], ALU.mult)
            P.matmul(pb[3][0:64, :], cn.ones_bf[0:64, 0:64], sq[:], start=True, stop=True)
            P.ts(rs[:], pb[3][0:64, :], 1.0 / 64, EPS, ALU.mult, ALU.add)
            rsqrt_inplace(P, rs[:])
            P.stt(dst, src[:], gcol, rs[:], ALU.mult, ALU.mult)
        P.copy(vb[:], v[:], eng="pool")
        for j in range(4):
            P.transpose(st.pt_bf[0][:, j * 64:(j + 1) * 64], vb[:, j * 128:(j + 1) * 128], cn.ident_bf[0:64, 0:64])
        P.copy(self.Vaug[:, 4 * sb:4 * sb + 4, 0:64], st.pt_bf[0][:, 0:256].c3())
        P.release(mrk)

    def loop(self, sb):
        st = self.st
        P, pb, mk, cn = st.P, st.pb, st.mk, st.cn
        t0 = sb * 512
        QT, o, rec, yo = self.QT, self.o, self.rec, self.yo
        po = pb[4]
        nkb = 4 * sb + 4
        NSC = 4
        AHEAD = 3

        def score(kb):
            ps = pb[kb % NSC]
            Kb = self.KT[0:67, kb * 128:(kb + 1) * 128]
            d = kb - 4 * sb
            if d < 0:
                P.matmul(ps[:, :], Kb, QT[:, :], start=True, stop=True)
            else:
                c0 = d * 128
                P.matmul(ps[:, c0:c0 + 128], Kb, QT[:, c0:c0 + 128], start=True, stop=False)
                P.matmul(ps[:, c0:c0 + 128], cn.ident_bf[:], self.nmb[:], start=False, stop=True)
                if c0 + 128 < 512:
                    P.matmul(ps[:, c0 + 128:512], Kb, QT[:, c0 + 128:512], start=True, stop=True)

        def expo(kb):
            c0 = max(kb - 4 * sb, 0) * 128
            pT = self.pT[kb % NSC]
            P.act(pT[:, c0:512], pb[kb % NSC][:, c0:512], AF.Exp, bias=self.negF[:, kb:kb + 1])
            if c0 > 0:
                P.memset(pT[:, 0:c0], 0.0)

        def pv(kb):
            P.matmul(po[0:65, :], self.Vaug[:, kb, :], self.pT[kb % NSC][:, :], start=(kb == 0), stop=(kb == nkb - 1))

        for kb in range(min(AHEAD, nkb)):
            score(kb)
        for kb in range(nkb):
            expo(kb)
            if kb + AHEAD < nkb:
                score(kb + AHEAD)
            pv(kb)
            yield
        P.copy(o[:], po[0:65, :])
        P.matmul(pb[5][0:64, :], mk[0:65, MK_SEL, 0:64], o[:], start=True, stop=True)
        P.recip(rec[:], pb[5][0:64, :])
        P.tt(yo[:], o[0:64, :], rec[:], ALU.mult)
        P.dma(st.yT_d[192:256, t0:t0 + 512], yo[:], q="pool")
        yield


def build_B(L, layer, parts=("rw", "ssd", "fox")):
    nc = bass.Bass("TRN2", target_bir_lowering=False)
    P = Prog(nc)
    NSB = L // 512
    NWc = NW1 if layer == 1 else NW0
    x_d = P.dram("x", [L, D], F32, "ExternalInput")
    c_d = P.dram("c", [1, D], F32, "ExternalInput")
    ada_w_d = P.dram("ada_w", [D, 6 * D], F32, "ExternalInput")
    ada_b_d = P.dram("ada_b", [1, 6 * D], F32, "ExternalInput")
    nmix_d = P.dram("nmix", [1, D], F32, "ExternalInput")
    wB_d = P.dram("wB", [D, NWc], F32, "ExternalInput")
    pp_d = P.dram("pp", [128, NPP], F32, "ExternalInput")
    pbc_d = P.dram("pbc", [1, NBC], F32, "ExternalInput")
    mask_d = P.dram("masks", [128, NMASK, 128], F32, "ExternalInput")
    wup_d = P.dram("wup", [64, 64], F32, "ExternalInput")
    aup_d = P.dram("aup", [64, 64], F32, "ExternalInput")
    gup_d = P.dram("gup", [160, 64], F32, "ExternalInput")
    if layer == 1:
        vup_d = P.dram("vup", [32, 64], F32, "ExternalInput")
        vf_d = P.dram("vfT", [64, L], F32, "ExternalInput")
    else:
        vfo_d = P.dram("vfT_out", [64, L], F32, "ExternalOutput")
    yT_d = P.dram("yT", [256, L], BF16, "ExternalOutput")

    cn = mk_consts(P)
    pb = [P.ps([128, 512], F32, f"pb{i}") for i in range(6)]
    pt_bf = [P.ps([128, 1024], BF16, "ptA"), P.ps([128, 1024], BF16, "ptB")]
    ht = HT(P, cn, pt_bf)

    pp = P.sb([128, NPP], F32, "pp")
    P.dma(pp[:], pp_d[:, :])
    pbc = P.sb([128, NBC], F32, "pbc")
    P.dma(pbc[:], pbc_d[0:1, :].bcast([128, NBC]))
    mk = P.sb([128, NMASK, 128], F32, "mk")
    P.dma(mk[:], mask_d[:, :, :], q="act")
    A_m = P.sb([128, 8], F32, "A_m")
    sh_m = P.sb([128, 8], F32, "sh_m")
    m0 = P.mark()
    cols, _ = emit_mod(P, cn, c_d, ada_w_d, ada_b_d, [0, 1], [], pb[0], pb[1])
    nmix_c = load_col(P, nmix_d[0, :], 8, "nmix_c")
    P.ts(A_m[:], cols[1][:], 1.0, None, ALU.add)
    P.tt(A_m[:], A_m[:], nmix_c[:], ALU.mult)
    P.copy(sh_m[:], cols[0][:])
    P.release(m0)
    wB = P.sb([128, 8, NWc], BF16, "wB")
    wup = P.sb([64, 64], BF16, "wup")
    aup = P.sb([64, 64], BF16, "aup")
    gupa = P.sb([128, 64], BF16, "gupa")
    gupb = P.sb([32, 64], BF16, "gupb")
    if layer == 1:
        vup = P.sb([32, 64], BF16, "vup")
    m_ld = P.mark()
    ld = Loader(P)
    for k in range(8):
        ld.load(wB[:, k, :], wB_d[k * 128:(k + 1) * 128, :])
    ld.load(wup[:, :], wup_d[:, :], np_=64)
    ld.load(aup[:, :], aup_d[:, :], np_=64)
    ld.load(gupa[:, :], gup_d[0:128, :])
    ld.load(gupb[:, :], gup_d[128:160, :], np_=32)
    if layer == 1:
        ld.load(vup[:, :], vup_d[:, :], np_=32)
    P.release(m_ld)

    xt = [P.sb([128, D], F32, f"xt{i}") for i in range(4)]
    hTs = [P.sb([128, 8, 512], BF16, f"hT{i}") for i in range(2)]

    def project(c0, ncol, dst, psb, evac="act"):
        hT = st.hT
        for k in range(8):
            P.matmul(psb[0:ncol, :], wB[:, k, c0:c0 + ncol], hT[:, k, :], start=(k == 0), stop=(k == 7))
        if evac == "act":
            P.act(dst, psb[0:ncol, :], AF.Copy)
        else:
            P.copy(dst, psb[0:ncol, :])

    st = Ctx()
    st.P, st.cn, st.pb, st.pp, st.pbc, st.mk, st.hT, st.wB, st.project = P, cn, pb, pp, pbc, mk, hTs[0], wB, project
    st.L, st.layer, st.NSB, st.yT_d, st.pt_bf = L, layer, NSB, yT_d, pt_bf
    st.wup, st.aup, st.gupa, st.gupb = wup, aup, gupa, gupb
    if layer == 1:
        st.vup, st.vf_d = vup, vf_d
    else:
        st.vfo_d = vfo_d
    rw = RWMixer(st) if "rw" in parts else None
    ssd = SSDMixer(st) if "ssd" in parts else None
    fox = FoxMixer(st) if "fox" in parts else None

    pb5bf = pb[5][:, :].bitcast(BF16)

    def load_x(sb):
        t0 = sb * 512
        for i in range(4):
            P.dma(xt[i][:], x_d[t0 + i * 128:t0 + (i + 1) * 128, :], q="sp" if i % 2 == 0 else "act")

    def gen_hT(sb, banks):
        hT = hTs[sb % 2]
        for i in range(4):
            ht.emit(xt[i][:], A_m, sh_m, lambda k, i=i: hT[:, k, i * 128:(i + 1) * 128], banks=banks)
            yield

    load_x(0)
    for _ in gen_hT(0, None):
        pass
    pending = None
    for sb in range(NSB):
        st.hT = hTs[sb % 2]
        items = []
        if pending is not None:
            items.append([pending[0], pending[1], 0, False])
        if sb + 1 < NSB:
            load_x(sb + 1)
            items.append([gen_hT(sb + 1, [pb5bf[:, 0:512], pb5bf[:, 512:1024]]), 4, 0, False])
        calls = [0]
        NCALLS = 26

        def filler(frac, items=items, calls=calls):
            calls[0] += 1
            target = 1.0 if frac >= 1.0 else min(1.0, calls[0] / NCALLS)
            for it in items:
                gen, nsteps = it[0], it[1]
                while not it[3] and (frac >= 1.0 or it[2] < nsteps * target):
                    try:
                        next(gen)
                        it[2] += 1
                    except StopIteration:
                        it[3] = True
        if rw:
            rw.block(sb, filler if items else None)
        filler(1.0)
        pending = None
        if ssd:
            ssd.block(sb)
        if fox:
            fox.prep(sb)
            pending = (fox.loop(sb), 4 * sb + 5)
    if pending is not None:
        for _ in pending[0]:
            pass
    print("B peak sbuf", P.peak, "ops", len(P.ops))
    return P.emit()


RW_W, SSD_W, FOX_W = 512, 1024, 512
RW_COLS_, SSD_COLS_, FOX_COLS_ = 1824, 3088, 1544


def core_inputs_B(inp, l, i, x, vfT=None):
    w_in = inp['w_in'][l]
    o_rw, o_ssd, o_fox = 0, RW_COLS_, RW_COLS_ + SSD_COLS_
    hs = slice(i * 64, (i + 1) * 64)
    W, SW, FW = RW_W, SSD_W, FOX_W
    g = i // 2
    ar = np.arange
    cols = [ar(o_rw + i * 64, o_rw + (i + 1) * 64), ar(o_rw + W + i * 64, o_rw + W + (i + 1) * 64),
            ar(o_rw + 2 * W + i * 64, o_rw + 2 * W + (i + 1) * 64),
            ar(o_rw + 3 * W, o_rw + 3 * W + 64), ar(o_rw + 3 * W + 64, o_rw + 3 * W + 128), ar(o_rw + 3 * W + 128, o_rw + 3 * W + 288),
            ar(o_ssd + i * 128, o_ssd + (i + 1) * 128), ar(o_ssd + SW + i * 128, o_ssd + SW + (i + 1) * 128),
            ar(o_ssd + 2 * SW + g * 128, o_ssd + 2 * SW + (g + 1) * 128), ar(o_ssd + 2 * SW + 512 + g * 128, o_ssd + 2 * SW + 512 + (g + 1) * 128),
            ar(o_fox + i * 64, o_fox + (i + 1) * 64), ar(o_fox + FW + i * 64, o_fox + FW + (i + 1) * 64), ar(o_fox + 2 * FW + i * 64, o_fox + 2 * FW + (i + 1) * 64),
            ar(o_ssd + 2 * SW + 1024 + 2 * i, o_ssd + 2 * SW + 1024 + 2 * i + 2), np.array([o_fox + 3 * FW + i])]
    cols = np.concatenate(cols)
    wB = w_in[:, cols]
    if l == 1:
        wB = np.concatenate([wB, inp['rw_v_down'][0]], axis=1)
    pp = np.zeros((128, NPP), np.float32)
    mu = inp['rw_mu'][l]
    pp[0:64, PP_MU + 0] = mu[0 * W + i * 64:0 * W + (i + 1) * 64]
    pp[0:64, PP_MU + 1] = mu[1 * W + i * 64:1 * W + (i + 1) * 64]
    pp[0:64, PP_MU + 2] = mu[2 * W + i * 64:2 * W + (i + 1) * 64]
    pp[0:64, PP_MU + 3] = mu[3 * W:3 * W + 64]
    pp[0:64, PP_MU + 4] = mu[3 * W + 64:3 * W + 128]
    pp[0:128, PP_MU + 5] = mu[3 * W + 128:3 * W + 256]
    pp[0:32, PP_MU + 6] = mu[3 * W + 256:3 * W + 288]
    pp[0:64, PP_W0] = inp['rw_w0'][l][hs]
    pp[0:64, PP_A0] = inp['rw_a0'][l][hs]
    pp[0:64, PP_KK] = inp['rw_k_k'][l][hs]
    pp[0:64, PP_KA] = inp['rw_k_a'][l][hs]
    pp[0:64, PP_RK] = inp['rw_r_k'][l][i]
    if l == 1:
        pp[0:64, PP_V0] = inp['rw_v0'][0][hs]
    cw, cb = inp['ssd_conv_w'][l], inp['ssd_conv_b'][l]
    xs_ = slice(i * 128, (i + 1) * 128)
    bs_ = slice(SW + g * 128, SW + (g + 1) * 128)
    cs_ = slice(SW + 512 + g * 128, SW + 512 + (g + 1) * 128)
    for t in range(4):
        pp[0:64, PP_CW + t] = cw[t, xs_][0:64]
        pp[0:64, PP_CW + 4 + t] = cw[t, xs_][64:128]
        pp[:, PP_CW + 8 + t] = cw[t, bs_]
        pp[:, PP_CW + 12 + t] = cw[t, cs_]
    pp[0:64, PP_CB] = cb[xs_][0:64]
    pp[0:64, PP_CB + 1] = cb[xs_][64:128]
    pp[:, PP_CB + 2] = cb[bs_]
    pp[:, PP_CB + 3] = cb[cs_]
    pp[0:64, PP_QG] = inp['fox_q_gain'][l]
    pp[0:64, PP_KG] = inp['fox_k_gain'][l]
    pp[:, PP_D] = inp['ssd_d'][l][2 * i]
    pp[:, PP_D + 1] = inp['ssd_d'][l][2 * i + 1]
    pp[64, PP_SEL] = 1.0
    pp[65, PP_SEL + 1] = 1.0
    pp[66, PP_SEL + 2] = 1.0
    pbc = np.zeros((1, NBC), np.float32)
    pbc[0, BC_LNW:BC_LNW + 64] = inp['rw_lnx_w'][l][hs]
    pbc[0, BC_LNB:BC_LNB + 64] = inp['rw_lnx_b'][l][hs]
    pbc[0, BC_DTB:BC_DTB + 2] = inp['ssd_dt_bias'][l][2 * i:2 * i + 2]
    pbc[0, BC_ALOG:BC_ALOG + 2] = inp['ssd_a_log'][l][2 * i:2 * i + 2]
    pbc[0, BC_FB] = inp['fox_f_bias'][l][i]
    m = dict(x=x, c=inp['c'], ada_w=inp['ada_w'][l], ada_b=inp['ada_b'][l][None], nmix=inp['norm_mix'][l][None],
             wB=np.ascontiguousarray(wB), pp=pp, pbc=pbc, masks=make_masks(),
             wup=np.ascontiguousarray(inp['rw_w_up'][l][:, hs]), aup=np.ascontiguousarray(inp['rw_a_up'][l][:, hs]),
             gup=np.ascontiguousarray(inp['rw_g_up'][l][:, hs]))
    if l == 1:
        m['vup'] = np.ascontiguousarray(inp['rw_v_up'][0][:, hs])
        m['vfT'] = vfT
    return m


def common_inputs_C(inp, l):
    off_gate = RW_COLS_ + SSD_COLS_ + FOX_COLS_
    common = dict(
        c=inp['c'], ada_w=inp['ada_w'][l], ada_b=inp['ada_b'][l][None], nmix=inp['norm_mix'][l][None], nffn=inp['norm_ffn'][l][None],
        wgate=np.ascontiguousarray(inp['w_in'][l][:, off_gate:]), gateb=inp['gate_b'][l][None],
        proj=np.concatenate([inp['proj_rw'][l], inp['proj_ssd'][l], inp['proj_fox'][l]], axis=0), wout=inp['w_out'][l], ssdn=inp['ssd_norm'][l][None])
    if l % 2 == 1:
        common.update(routT=np.ascontiguousarray(inp['moe_router'][l // 2].T), wgu=inp['moe_w_gu'][l // 2], wdn=inp['moe_w_down'][l // 2])
    else:
        common.update(wgu=inp['ffn_w_gu'][l // 2][None], wdn=inp['ffn_w_down'][l // 2][None])
    return common


def assemble_yT(resB, L):
    yT = np.empty((2048, L), dtype=resB[0]['yT'].dtype)
    for i, r in enumerate(resB):
        y = r['yT']
        yT[i * 64:(i + 1) * 64] = y[0:64]
        yT[512 + i * 128:512 + (i + 1) * 128] = y[64:192]
        yT[1536 + i * 64:1536 + (i + 1) * 64] = y[192:256]
    return yT


NCORES = 8


def kernel(**inputs):
    inp = {k: np.asarray(v) for k, v in inputs.items()}
    L = inp['x'].shape[1]
    T_c = L // NCORES
    x = np.ascontiguousarray(inp['x'][0])
    cores = list(range(NCORES))
    vfT = [None] * NCORES
    for l in range(2):
        ncB = build_B(L, l)
        mapsB = [core_inputs_B(inp, l, i, x, vfT[i]) for i in cores]
        resB = run_bass_kernel_spmd(ncB, mapsB, core_ids=cores).results
        if l == 0:
            vfT = [np.ascontiguousarray(r['vfT_out']) for r in resB]
        yT = assemble_yT(resB, L)
        ncC = build_C(T_c, moe=(l % 2 == 1))
        common = common_inputs_C(inp, l)
        mapsC = []
        for j in cores:
            m = dict(common)
            m['x'] = np.ascontiguousarray(x[j * T_c:(j + 1) * T_c])
            m['yT'] = np.ascontiguousarray(yT[:, j * T_c:(j + 1) * T_c])
            mapsC.append(m)
        resC = run_bass_kernel_spmd(ncC, mapsC, core_ids=cores).results
        x = np.concatenate([r['out'] for r in resC], axis=0)
    return x[None].astype(np.float32)
```
